# Optimizing a Trainium2 kernel written in Bass

```python
import math
import jax, jax.numpy as jnp
from jax import lax
import numpy as np

D_MODEL = 1024
BATCH = 2
SEQ = 8192
DEPTH = 1

CHUNK = 64
Q_BLOCK = 128
MEM_LEN = 256

N_DIFF_HEADS = 4
DIFF_DK = 64
DIFF_DV = 128
DIFF_WIDTH = N_DIFF_HEADS * DIFF_DV

POOL_WINDOWS = (2, 4, 8, 16)
POOL_GROUPS = len(POOL_WINDOWS)
POOL_DIM = 128
POOL_WIDTH = POOL_GROUPS * POOL_DIM

MIX_WIDTH = DIFF_WIDTH + POOL_WIDTH
IN_WIDTH = 3 * DIFF_WIDTH + POOL_WIDTH

REL_BUCKETS = 32
REL_MAX_DISTANCE = 128

N_CROSS_HEADS = 4
CROSS_HEAD_DIM = D_MODEL // N_CROSS_HEADS

N_EXPERT_GROUPS = 4
EXPERTS_PER_GROUP = 4
N_EXPERTS = N_EXPERT_GROUPS * EXPERTS_PER_GROUP
TOP_K_INNER = 2
EXPERT_FF = D_MODEL // 4

EPS = 1e-6

kernel_name = "hybrid_diffattn_pool_hiermoe_block"


def rmsnorm(x, g):
    xf = x.astype(jnp.float32)
    y = xf * lax.rsqrt(jnp.mean(xf * xf, axis=-1, keepdims=True) + EPS)
    return (y * g.astype(jnp.float32)).astype(x.dtype)


def rel_bucket(rel):
    nb = REL_BUCKETS // 2
    max_exact = nb // 2
    ret = (rel > 0).astype(jnp.int32) * nb
    n = jnp.abs(rel)
    nf = jnp.maximum(n, 1).astype(jnp.float32)
    large = max_exact + (jnp.log(nf / max_exact) / math.log(REL_MAX_DISTANCE / max_exact)
                         * (nb - max_exact)).astype(jnp.int32)
    large = jnp.minimum(large, nb - 1)
    return ret + jnp.where(n < max_exact, n, large)


def diff_attention(q, k, v, lam, rel_bias):
    B, S = q.shape[0], q.shape[1]
    nb = S // Q_BLOCK
    scale = DIFF_DK ** -0.5
    qb_all = q.reshape(B, nb, Q_BLOCK, N_DIFF_HEADS, 2, DIFF_DK).transpose(1, 0, 3, 4, 2, 5)
    kt = k.transpose(0, 2, 3, 1, 4)
    vt = v.transpose(0, 2, 1, 3)
    kpos = jnp.arange(S, dtype=jnp.int32)
    kchunk = kpos // CHUNK
    neg = jnp.finfo(jnp.float32).min

    def block(args):
        qb, bi = args
        qpos = bi * Q_BLOCK + jnp.arange(Q_BLOCK, dtype=jnp.int32)
        bias = rel_bias[rel_bucket(kpos[None, :] - qpos[:, None])]
        bias = bias.transpose(2, 0, 1).astype(jnp.float32)
        allowed = kchunk[None, :] <= (qpos // CHUNK)[:, None]
        s = jnp.einsum("bhmqd,bhmkd->bhmqk", qb, kt).astype(jnp.float32) * scale
        s = s + bias[None, :, None]
        s = jnp.where(allowed[None, None, None], s, neg)
        p = jax.nn.softmax(s, axis=-1)
        a = p[:, :, 0] - lam.astype(jnp.float32) * p[:, :, 1]
        return jnp.einsum("bhqk,bhkd->bhqd", a.astype(vt.dtype), vt)

    o = lax.map(block, (qb_all, jnp.arange(nb, dtype=jnp.int32)))
    return o.transpose(1, 0, 3, 2, 4).reshape(B, S, N_DIFF_HEADS, DIFF_DV)


def multiscale_pool(u, w_pool, scale):
    B, S, _ = u.shape
    uf = u.astype(jnp.float32).reshape(B, S, POOL_GROUPS, POOL_DIM)
    cum = jnp.cumsum(uf, axis=1)
    t1 = jnp.arange(1, S + 1, dtype=jnp.float32)
    outs = []
    for g, w in enumerate(POOL_WINDOWS):
        c = cum[:, :, g]
        lag = jnp.pad(c, ((0, 0), (w, 0), (0, 0)))[:, :S]
        mean = (c - lag) / jnp.minimum(t1, float(w))[None, :, None]
        outs.append(mean - uf[:, :, g])
    d = jnp.stack(outs, axis=2).astype(u.dtype)
    y = jnp.einsum("bsgc,gcd->bsgd", d, w_pool) * scale.reshape(POOL_GROUPS, POOL_DIM)
    return y.reshape(B, S, POOL_WIDTH)


def cross_attention(h, m, wq, wkv, wo):
    B, S, _ = h.shape
    q = (h @ wq).reshape(B, S, N_CROSS_HEADS, CROSS_HEAD_DIM)
    kv = (m @ wkv).reshape(B, m.shape[1], 2, N_CROSS_HEADS, CROSS_HEAD_DIM)
    k, v = kv[:, :, 0], kv[:, :, 1]
    s = jnp.einsum("bqhd,bkhd->bhqk", q, k).astype(jnp.float32) * (CROSS_HEAD_DIM ** -0.5)
    p = jax.nn.softmax(s, axis=-1).astype(v.dtype)
    o = jnp.einsum("bhqk,bkhd->bqhd", p, v).reshape(B, S, D_MODEL)
    return o @ wo


def hier_moe(h, wr_g, br_g, wr_e, br_e, w_gate, w_up, w_down):
    B, S, D = h.shape
    t = h.reshape(B * S, D)
    g_logits = (t @ wr_g + br_g).astype(jnp.float32)
    g_prob = jax.nn.softmax(g_logits, axis=-1)
    g_sel = jnp.argmax(g_logits, axis=-1)
    g_w = jnp.take_along_axis(g_prob, g_sel[:, None], axis=-1)[:, 0]
    e_logits = (jnp.einsum("td,gde->tge", t, wr_e) + br_e).astype(jnp.float32)
    e_sel_logits = jnp.take_along_axis(e_logits, g_sel[:, None, None], axis=1)[:, 0]
    top_v, top_i = lax.top_k(e_sel_logits, TOP_K_INNER)
    top_w = jax.nn.softmax(top_v, axis=-1)
    inner = jnp.einsum("tk,tke->te", top_w, jax.nn.one_hot(top_i, EXPERTS_PER_GROUP, dtype=jnp.float32))
    combine = (g_w[:, None, None] * jax.nn.one_hot(g_sel, N_EXPERT_GROUPS, dtype=jnp.float32)[:, :, None]
               * inner[:, None, :]).reshape(B * S, N_EXPERTS)
    hg = jnp.einsum("td,edf->tef", t, w_gate)
    hu = jnp.einsum("td,edf->tef", t, w_up)
    act = jax.nn.silu(hg) * hu * combine.astype(t.dtype)[:, :, None]
    out = jnp.einsum("tef,efd->td", act, w_down)
    return out.reshape(B, S, D)


def setup_inputs(seed: int = 0) -> dict:
    key = jax.random.key(seed)
    ks = jax.random.split(key, 32)
    f32 = jnp.float32
    nrm = lambda k, shape, s: jax.random.normal(k, shape, f32) * s
    gain = lambda k, shape: 1.0 + 0.05 * jax.random.normal(k, shape, f32)
    L, D = DEPTH, D_MODEL
    return {
        "x": jax.random.normal(ks[0], (BATCH, SEQ, D), f32),
        "mem": jax.random.normal(ks[1], (BATCH, MEM_LEN, D), f32),
        "rel_bias": nrm(ks[2], (REL_BUCKETS, N_DIFF_HEADS), 0.5),
        "attn_norm": gain(ks[3], (L, D)),
        "w_in": nrm(ks[4], (L, D, IN_WIDTH), D ** -0.5),
        "lambda_q1": nrm(ks[5], (L, DIFF_DK), 0.1),
        "lambda_k1": nrm(ks[6], (L, DIFF_DK), 0.1),
        "lambda_q2": nrm(ks[7], (L, DIFF_DK), 0.1),
        "lambda_k2": nrm(ks[8], (L, DIFF_DK), 0.1),
        "diff_subln": gain(ks[9], (L, DIFF_DV)),
        "pool_w": nrm(ks[10], (L, POOL_GROUPS, POOL_DIM, POOL_DIM), POOL_DIM ** -0.5),
        "pool_scale": gain(ks[11], (L, POOL_WIDTH)),
        "w_out": nrm(ks[12], (L, MIX_WIDTH, D), MIX_WIDTH ** -0.5),
        "cross_norm": gain(ks[13], (L, D)),
        "mem_norm": gain(ks[14], (L, D)),
        "wq_cross": nrm(ks[15], (L, D, D), D ** -0.5),
        "wkv_cross": nrm(ks[16], (L, D, 2 * D), D ** -0.5),
        "wo_cross": nrm(ks[17], (L, D, D), D ** -0.5),
        "ffn_norm": gain(ks[18], (L, D)),
        "router_group": nrm(ks[19], (L, D, N_EXPERT_GROUPS), D ** -0.5),
        "router_group_bias": nrm(ks[20], (L, N_EXPERT_GROUPS), 0.01),
        "router_expert": nrm(ks[21], (L, N_EXPERT_GROUPS, D, EXPERTS_PER_GROUP), D ** -0.5),
        "router_expert_bias": nrm(ks[22], (L, N_EXPERT_GROUPS, EXPERTS_PER_GROUP), 0.01),
        "w_gate": nrm(ks[23], (L, N_EXPERTS, D, EXPERT_FF), D ** -0.5),
        "w_up": nrm(ks[24], (L, N_EXPERTS, D, EXPERT_FF), D ** -0.5),
        "w_down": nrm(ks[25], (L, N_EXPERTS, EXPERT_FF, D), EXPERT_FF ** -0.5),
        "final_norm": gain(ks[26], (D,)),
    }


def reference(x, mem, rel_bias, attn_norm, w_in, lambda_q1, lambda_k1, lambda_q2, lambda_k2,
              diff_subln, pool_w, pool_scale, w_out, cross_norm, mem_norm, wq_cross, wkv_cross,
              wo_cross, ffn_norm, router_group, router_group_bias, router_expert,
              router_expert_bias, w_gate, w_up, w_down, final_norm):
    B, S, _ = x.shape
    h = x
    for layer in range(DEPTH):
        lambda_init = 0.8 - 0.6 * math.exp(-0.3 * layer)
        hn = rmsnorm(h, attn_norm[layer])
        z = hn @ w_in[layer]
        q = z[..., :DIFF_WIDTH].reshape(B, S, N_DIFF_HEADS, 2, DIFF_DK)
        k = z[..., DIFF_WIDTH:2 * DIFF_WIDTH].reshape(B, S, N_DIFF_HEADS, 2, DIFF_DK)
        v = z[..., 2 * DIFF_WIDTH:3 * DIFF_WIDTH].reshape(B, S, N_DIFF_HEADS, DIFF_DV)
        u = z[..., 3 * DIFF_WIDTH:]
        lam = (jnp.exp(jnp.sum(lambda_q1[layer] * lambda_k1[layer]))
               - jnp.exp(jnp.sum(lambda_q2[layer] * lambda_k2[layer])) + lambda_init)
        a = diff_attention(q, k, v, lam, rel_bias)
        a = rmsnorm(a, diff_subln[layer]) * (1.0 - lambda_init)
        a = a.reshape(B, S, DIFF_WIDTH)
        p = multiscale_pool(u, pool_w[layer], pool_scale[layer])
        h = h + jnp.concatenate([a, p], axis=-1) @ w_out[layer]
        h = h + cross_attention(rmsnorm(h, cross_norm[layer]), rmsnorm(mem, mem_norm[layer]),
                                wq_cross[layer], wkv_cross[layer], wo_cross[layer])
        h = h + hier_moe(rmsnorm(h, ffn_norm[layer]), router_group[layer], router_group_bias[layer],
                         router_expert[layer], router_expert_bias[layer],
                         w_gate[layer], w_up[layer], w_down[layer])
    return rmsnorm(h, final_norm)
```

```python
import math
import numpy as np
from contextlib import ExitStack
import concourse.bass as bass
import concourse.mybir as mybir
from concourse.bass_utils import run_bass_kernel_spmd

F32 = mybir.dt.float32
BF16 = mybir.dt.bfloat16
AF = mybir.ActivationFunctionType
ALU = mybir.AluOpType
AX = mybir.AxisListType

COMPUTE = ("pe", "act", "dve", "pool")
ENGS = ("pe", "act", "dve", "pool", "sp")
NEG = -30000.0


class Buf:
    __slots__ = ("name", "writer", "rd_eng", "rd_dma")

    def __init__(self, name=""):
        self.name = name
        self.writer = None
        self.rd_eng = {}
        self.rd_dma = []


class Op:
    __slots__ = ("eng", "idx", "fn", "waits", "signal", "num", "dma", "dma_i", "clock", "slotwait")


class Sched:
    def __init__(self, K=8):
        self.ops = {e: [] for e in ENGS}
        self.known = {e: {c: -1 for c in COMPUTE} for e in ENGS}
        self.dma_known = {e: set() for e in ENGS}
        self.ndma = {e: 0 for e in ENGS}
        self.dma_ops = {e: [] for e in ENGS}
        self.bar = {e: [] for e in ENGS}
        self.K = K

    def barrier(self):
        lasts = []
        for e in COMPUTE:
            for op in reversed(self.ops[e]):
                if not op.dma:
                    lasts.append(op)
                    break
        for e in ENGS:
            self.bar[e] = list(lasts)

    def add(self, eng, fn, reads=(), writes=(), dma=False):
        op = Op()
        op.eng = eng
        op.idx = len(self.ops[eng])
        op.fn = fn
        op.dma = dma
        op.signal = False
        op.num = None
        op.dma_i = None
        op.slotwait = None
        deps = []
        if self.bar[eng]:
            deps.extend(d for d in self.bar[eng] if not (d.eng == eng and eng == "pe"))
            self.bar[eng] = []
        for b in reads:
            if b.writer is not None:
                deps.append(b.writer)
        for b in writes:
            if b.writer is not None:
                deps.append(b.writer)
            deps.extend(b.rd_eng.values())
            deps.extend(b.rd_dma)
        known = self.known[eng]
        best = {}
        dwaits = []
        for d in deps:
            if d.dma:
                if id(d) not in self.dma_known[eng]:
                    self.dma_known[eng].add(id(d))
                    dwaits.append(d)
            else:
                if d.eng == eng and eng == "pe":
                    continue
                if known[d.eng] >= d.idx:
                    continue
                if d.eng not in best or best[d.eng].idx < d.idx:
                    best[d.eng] = d
        waits = list(best.values()) + dwaits
        for d in waits:
            d.signal = True
            for c in COMPUTE:
                if d.clock[c] > known[c]:
                    known[c] = d.clock[c]
            if not d.dma and d.idx > known[d.eng]:
                known[d.eng] = d.idx
        if dma:
            i = self.ndma[eng]
            op.dma_i = i
            self.ndma[eng] = i + 1
            if i >= self.K:
                prev = self.dma_ops[eng][i - self.K]
                op.slotwait = prev
                self.dma_known[eng].add(id(prev))
            self.dma_ops[eng].append(op)
        op.waits = waits
        op.clock = dict(known)
        if not dma and eng in COMPUTE:
            op.clock[eng] = op.idx
        for b in reads:
            if dma:
                b.rd_dma.append(op)
            else:
                b.rd_eng[eng] = op
        for b in writes:
            b.writer = op
            b.rd_eng = {}
            b.rd_dma = []
        self.ops[eng].append(op)
        return op

    def emit(self, nc, stack):
        sem_eng = {e: stack.enter_context(nc.semaphore("s_" + e)) for e in COMPUTE}
        sem_dma = {e: [stack.enter_context(nc.semaphore("d_%s%d" % (e, k))) for k in range(self.K)]
                   for e in ENGS if self.ndma[e] > 0}
        for e in COMPUTE:
            n = 0
            for op in self.ops[e]:
                if op.signal and not op.dma:
                    n += 1
                    op.num = n
        K = self.K

        def dma_target(d):
            return sem_dma[d.eng][d.dma_i % K], 16 * (d.dma_i // K + 1)

        def run(ename, e):
            for op in self.ops[ename]:
                if op.slotwait is not None:
                    s, v = dma_target(op.slotwait)
                    e.wait_ge(s, v)
                for d in op.waits:
                    if d.dma:
                        s, v = dma_target(d)
                        e.wait_ge(s, v)
                    else:
                        e.wait_ge(sem_eng[d.eng], d.num)
                ins = op.fn(e)
                if op.dma:
                    s, v = dma_target(op)
                    ins.then_inc(s, 16)
                elif op.signal:
                    ins.then_inc(sem_eng[ename], 1)
            for d in self.dma_ops[ename][-K:]:
                s, v = dma_target(d)
                e.wait_ge(s, v)

        block = stack.enter_context(nc.Block())

        @block.tensor
        def _(e):
            run("pe", e)

        @block.scalar
        def _(e):
            run("act", e)

        @block.vector
        def _(e):
            run("dve", e)

        @block.gpsimd
        def _(e):
            run("pool", e)

        @block.sync
        def _(e):
            run("sp", e)


class Arena:
    def __init__(self, nc, st, name, nbytes):
        self.t32 = st.enter_context(nc.sbuf_tensor(name, [128, nbytes // 4], F32))
        self.t16 = self.t32.bitcast(BF16)
        self.nbytes = nbytes
        self.off = 0

    def reset(self, off=0):
        self.off = off

    def f32(self, n):
        o = self.off
        self.off += 4 * n
        assert self.off <= self.nbytes, (self.off, self.nbytes)
        return self.t32[:, o // 4:o // 4 + n]

    def bf16(self, n):
        o = self.off
        self.off += 2 * n
        self.off = (self.off + 3) // 4 * 4
        assert self.off <= self.nbytes, (self.off, self.nbytes)
        return self.t16[:, o // 2:o // 2 + n]


def build_nc(debug=0):
    nc = bass.Bass("TRN2", target_bir_lowering=False)

    def din(name, shape):
        return nc.dram_tensor(name, shape, F32, kind="ExternalInput")

    x_t = din("x", [8192, 1024]); x = x_t.ap()
    mem = din("mem", [256, 1024]).ap()
    w_in = din("w_in", [1024, 2048]).ap()
    w_out = din("w_out", [1024, 1024]).ap()
    wq = din("wq", [1024, 1024]).ap()
    wkv = din("wkv", [1024, 2048]).ap()
    wo = din("wo", [1024, 1024]).ap()
    w_gate = din("w_gate", [16, 1024, 256]).ap()
    w_up = din("w_up", [16, 1024, 256]).ap()
    w_down = din("w_down", [16, 256, 1024]).ap()
    pool_w = din("pool_w", [4, 128, 128]).ap()
    wr = din("wr", [1024, 20]).ap()
    rbias_t = din("rbias", [1, 20])
    rel_bias = din("rel_bias", [32, 4]).ap()
    lamv_t = din("lamv", [1, 256])
    gains_d = din("gains", [128, 40]).ap()
    fnorm_t = din("fnorm", [1, 1024])
    ident_d = din("ident", [128, 128]).ap()
    oh_d = din("oh", [32, 384]).ap()
    maskT_d = din("maskT", [128, 128]).ap()
    kvb_d = din("kvb", [128, 16]).ap()
    pinv_d = din("pinv", [128, 64]).ap()
    sel_d = din("sel", [16, 2048]).ap()
    y = nc.dram_tensor("y", [2048, 1024], F32, kind="ExternalOutput").ap()
    E_t = nc.dram_tensor("Escr", [4, 128, 384], F32, kind="Internal")
    dbg = {}
    if debug:
        for nm in ("dbg_h1", "dbg_h2", "dbg_h3"):
            dbg[nm] = nc.dram_tensor(nm, [2048, 1024], F32, kind="ExternalOutput").ap()
        dbg["dbg_mix"] = nc.dram_tensor("dbg_mix", [128, 8 * 2048], BF16, kind="ExternalOutput").ap()

    S = Sched()
    st = ExitStack()
    with st:
        G = Arena(nc, st, "G", 18 * 1024)
        ident = G.bf16(128)
        ones_bf = G.bf16(128)
        ones_f = G.f32(128)
        gains = G.f32(40)
        gains_t = G.t32
        gains_off = gains.offset
        identf = G.f32(128)
        fnorm_bc = G.f32(1024)
        rbias_bc = G.f32(20)
        lam_sb = G.f32(256)
        small = G.f32(64)
        relb = G.f32(4)
        oh_sb = G.f32(384)
        g_sb = G.f32(384)
        g_off = g_sb.offset
        maskT = G.f32(128)
        kvb = G.f32(16)
        pinv = G.f32(64)
        biasT = G.f32(1024).rearrange("p (h t q) -> p h t q", h=4, t=2)
        rstd_all = G.f32(64)
        ssq_all = G.f32(64)
        lnv = G.f32(8)
        FP8 = mybir.dt.float8e4
        g8 = G.t32.bitcast(FP8)
        junks = [g8[:, G.off + 1024 * k:G.off + 1024 * (k + 1)] for k in range(2)]
        G.off += 2048
        B_junk = [Buf() for _ in range(2)]
        jctr = [0]

        def SQ(src, Bsrc, accum, wr_):
            k = jctr[0] % 2
            jctr[0] += 1
            return S.add("act", lambda e: e.activation(junks[k], src, AF.Square, accum_out=accum, saturate=False),
                         Bsrc, wr_ + [B_junk[k]])
        lnv_ctr = [0]
        R1 = Arena(nc, st, "R1", 32 * 1024)
        R2 = Arena(nc, st, "R2", 64 * 1024)
        R3 = Arena(nc, st, "R3", 92 * 1024)
        ps_all = st.enter_context(nc.psum_tensor("ps_all", [128, 4096], F32))
        psb_all = ps_all.bitcast(BF16)
        ps = [ps_all[:, i * 512:(i + 1) * 512] for i in range(8)]
        psb = [psb_all[:, i * 1024:(i + 1) * 1024] for i in range(8)]
        Bp = [Buf("ps%d" % i) for i in range(8)]

        mixT = R1.t16[:, 0:16384].rearrange("p (c n) -> p c n", c=8)
        hT = mixT
        KT = R2.t16[:, 0:16384].rearrange("p (h n) -> p h n", h=2)
        Vv = R2.t16[:, 16384:32768].rearrange("p (k n) -> p k n", k=64)
        hres = R2.t32[:, 0:16384].rearrange("p (t n) -> p t n", t=16)

        def MM(out, lhsT, rhs, start, stop, rd, wr_):
            return S.add("pe", lambda e: e.matmul(out, lhsT, rhs, start=start, stop=stop), rd, wr_)

        def TR(out, in_, rd, wr_):
            return S.add("pe", lambda e: e.transpose(out, in_, ident), rd, wr_)

        def ACT(out, in_, func, rd, wr_, **kw):
            return S.add("act", lambda e: e.activation(out, in_, func, **kw), rd, wr_)

        def TT(eng, out, in0, in1, op, rd, wr_):
            return S.add(eng, lambda e: e.tensor_tensor(out, in0, in1, op), rd, wr_)

        def TS(eng, out, in0, s1, s2, op0, op1, rd, wr_):
            if op1 is None:
                return S.add(eng, lambda e: e.tensor_scalar(out, in0, s1, None, op0), rd, wr_)
            return S.add(eng, lambda e: e.tensor_scalar(out, in0, s1, s2, op0, op1), rd, wr_)

        def STT(out, in0, scalar, in1, op0, op1, rd, wr_):
            return S.add("dve", lambda e: e.scalar_tensor_tensor(out, in0, scalar, in1, op0, op1), rd, wr_)

        def CP(eng, out, in_, rd, wr_):
            if eng == "act":
                return S.add("act", lambda e: e.copy(out, in_), rd, wr_)
            return S.add(eng, lambda e: e.tensor_copy(out, in_), rd, wr_)

        def RECIP(out, in_, rd, wr_):
            return S.add("dve", lambda e: e.reciprocal(out, in_), rd, wr_)

        def DMA(q, out, in_, rd, wr_):
            return S.add(q, lambda e: e.dma_start(out=out, in_=in_), rd, wr_, dma=True)

        def gain_bc(c0):
            return bass.AP(gains_t, gains_off + c0, [[G.nbytes // 4, 128], [1, 8], [0, 128]])

        def gcol(c):
            return gains[:, c:c + 1]

        Bc = Buf("consts")
        B_g = Buf("g_sb")
        B_E = Buf("E")
        B_bias = Buf("biasT")
        DMA("pool", ident, ident_d, [], [Bc])
        DMA("sp", identf, ident_d, [], [Bc])
        DMA("sp", gains, gains_d, [], [Bc])
        DMA("sp", fnorm_bc, bass.AP(fnorm_t, 0, [[0, 128], [1, 1024]]), [], [Bc])
        DMA("sp", rbias_bc, bass.AP(rbias_t, 0, [[0, 128], [1, 20]]), [], [Bc])
        DMA("sp", lam_sb, bass.AP(lamv_t, 0, [[0, 128], [1, 256]]), [], [Bc])
        DMA("sp", relb[0:32, :], rel_bias, [], [Bc])
        DMA("sp", oh_sb[0:32, :], oh_d, [], [Bc])
        DMA("sp", maskT, maskT_d, [], [Bc])
        DMA("sp", kvb, kvb_d, [], [Bc])
        DMA("sp", pinv, pinv_d, [], [Bc])
        S.add("dve", lambda e: e.memset(ones_bf, 1.0), [], [Bc])
        S.add("dve", lambda e: e.memset(ones_f, 1.0 / 128.0), [], [Bc])
        prod = G.f32(128)
        TT("dve", prod[:, 0:64], lam_sb[:, 0:64], lam_sb[:, 64:128], ALU.mult, [Bc], [Bc])
        TT("dve", prod[:, 64:128], lam_sb[:, 128:192], lam_sb[:, 192:256], ALU.mult, [Bc], [Bc])
        S.add("dve", lambda e: e.reduce_sum(small[:, 0:1], prod[:, 0:64], axis=AX.X), [Bc], [Bc])
        S.add("dve", lambda e: e.reduce_sum(small[:, 1:2], prod[:, 64:128], axis=AX.X), [Bc], [Bc])
        ACT(small[:, 2:4], small[:, 0:2], AF.Exp, [Bc], [Bc])
        TT("dve", small[:, 4:5], small[:, 3:4], small[:, 2:3], ALU.subtract, [Bc], [Bc])
        TS("dve", small[:, 4:5], small[:, 4:5], -0.2, None, ALU.add, None, [Bc], [Bc])
        TS("dve", small[:, 5:6], gcol(36), 0.8, None, ALU.mult, None, [Bc], [Bc])
        neg_lam = small[:, 4:5]
        subcol = small[:, 5:6]
        def bias_part1():
            MM(ps[7][0:4, 0:384], relb[0:32, 0:4], oh_sb[0:32, 0:384], True, True, [Bc], [Bp[7]])
            CP("dve", g_sb[0:4, :], ps[7][0:4, 0:384], [Bp[7]], [B_g])

        def bias_part1b():
            DMA("sp", E_t.ap(), bass.AP(G.t32, g_off, [[G.nbytes // 4, 4], [0, 128], [1, 384]]), [B_g], [B_E])

        def bias_part2():
            DMA("sp", biasT[:, :, 0, :], bass.AP(E_t, 127, [[383, 128], [128 * 384, 4], [1, 128]]), [B_E], [B_bias])
            DMA("sp", biasT[:, :, 1, :], bass.AP(E_t, 255, [[383, 128], [128 * 384, 4], [1, 128]]), [B_E], [B_bias])

        def bias_part3():
            mask_bc = bass.AP(G.t32, maskT.offset, [[G.nbytes // 4, 128], [0, 4], [1, 128]])
            TT("dve", biasT[:, :, 0, :], biasT[:, :, 0, :], mask_bc, ALU.add, [B_bias, Bc], [B_bias])

        def norm_transpose(src, Bsrc, rstd_col, gain_c0, dst, Bdst, xs, Bxs, bank, Brs):
            ACT(xs, src, AF.Copy, [Bsrc, Brs], [Bxs], scale=rstd_col)
            for c in range(8):
                TR(psb[bank][:, c * 128:(c + 1) * 128], xs[:, c * 128:(c + 1) * 128], [Bxs, Bc], [Bp[bank]])
            TT("dve", dst, psb[bank][:, 0:1024].rearrange("p (c n) -> p c n", c=8), gain_bc(gain_c0), ALU.mult,
               [Bp[bank], Bc], [Bdst])

        B_lnc = [Buf() for _ in range(8)]

        def rms_cols(src, Bsrc, col):
            k = lnv_ctr[0] % 8
            lnv_ctr[0] += 1
            b1 = Buf()
            b3 = Buf()
            SQ(src, [Bsrc], ssq_all[:, col:col + 1], [b1])
            ACT(lnv[:, k:k + 1], ssq_all[:, col:col + 1], AF.Ln, [b1], [B_lnc[k]], scale=1.0 / 1024.0, bias=1e-6)
            ACT(rstd_all[:, col:col + 1], lnv[:, k:k + 1], AF.Exp, [B_lnc[k]], [b3], scale=-0.5)
            return b3
        brs_c = [None] * 16
        brs_d = [None] * 16
        brs_f = [None] * 16
        B_ssq = [Buf() for _ in range(64)]
        B_ln = [Buf() for _ in range(2)]
        B_rslot = [Buf() for _ in range(16)]
        B_mix = [[Buf("mix%d_%d" % (c, s)) for s in range(4)] for c in range(8)]

        for pr in range(2):
            S.barrier()
            R3.reset()
            wA = R3.bf16(8 * 768).rearrange("p (c n) -> p c n", c=8)
            B_wA = [Buf() for _ in range(3)]
            for part, c0 in enumerate((512 + 256 * pr, 1024 + 256 * pr, 256 * pr)):
                DMA("pool", wA[:, :, part * 256:(part + 1) * 256],
                    w_in[:, c0:c0 + 256].rearrange("(c p) n -> p c n", p=128), [], [B_wA[part]])
            if pr == 0:
                wU = R3.bf16(8 * 512).rearrange("p (c n) -> p c n", c=8)
                B_wU = Buf()
                DMA("pool", wU, w_in[:, 1536:2048].rearrange("(c p) n -> p c n", p=128), [], [B_wU])
                pw = R3.bf16(512).rearrange("p (g n) -> p g n", g=4)
                B_pw = Buf()
                DMA("pool", pw, pool_w.rearrange("g c d -> c g d"), [], [B_pw])
            QT = R3.bf16(2 * 2 * 2048).rearrange("p (h m n) -> p h m n", h=2, m=2)
            B_QT = [[Buf() for _ in range(4)] for _ in range(2)]
            B_QTz = Buf()
            S.add("pool", lambda e, QT=QT: e.memset(QT.rearrange("p h m n -> p (h m n)"), 0.0), [], [B_QTz])
            mark = R3.off
            xst = [R3.f32(1024) for _ in range(4)]
            if pr == 0:
                xst += [R1.t32[:, k * 1024:(k + 1) * 1024] for k in range(4)]
            else:
                xst += [R3.f32(1024) for _ in range(4)]
            B_xst = [Buf() for _ in range(8)]
            nxs = 3 if pr == 0 else 4
            xs_t = [R3.bf16(1024) for _ in range(nxs)]
            B_xs = [Buf() for _ in range(nxs)]
            hnT = [R3.bf16(8 * 512).rearrange("p (c n) -> p c n", c=8) for _ in range(2)]
            B_hnT = [[Buf() for _ in range(4)] for _ in range(2)]
            if pr == 0:
                uT = R3.f32(4 * 528).rearrange("p (g n) -> p g n", g=4)
                B_uT = [Buf() for _ in range(4)]
                B_uTp = [Buf() for _ in range(4)]
                ptmp = [R3.f32(528) for _ in range(2)]
                B_pt = [Buf() for _ in range(2)]
                dT = R3.bf16(4 * 512).rearrange("p (g n) -> p g n", g=4)
                B_dT = [Buf() for _ in range(4)]
            kq = [0]

            def kbank():
                b_ = 4 + kq[0] % 3
                kq[0] += 1
                return b_

            def emit_V(g, hb, tt):
                vb = 2 + g % 2
                for c in range(8):
                    MM(ps[vb][:, 0:256], hnT[hb][:, c, tt * 128:(tt + 1) * 128], wA[:, c, 256:512], c == 0, c == 7,
                       [B_hnT[hb][tt], B_wA[1]], [Bp[vb]])
                CP("act" if pr == 1 else "dve", Vv[:, g, :], ps[vb][:, 0:256], [Bp[vb]], [])

            def emit_slot(i, hb):
                own = (i % 4 == 3)
                s_own = i // 4
                allh = B_hnT[hb]
                for hc in range(2):
                    kb_ = kbank()
                    for c in range(8):
                        MM(ps[kb_][:, :], wA[:, c, hc * 128:(hc + 1) * 128], hnT[hb][:, c, :], c == 0, c == 7,
                           allh + [B_wA[0]], [Bp[kb_]])
                    CP("dve" if (hc or pr == 0) else "act", KT[:, hc, i * 512:(i + 1) * 512], ps[kb_][:, :], [Bp[kb_]], [])
                if own:
                    for hc in range(2):
                        kb_ = kbank()
                        for c in range(8):
                            MM(ps[kb_][:, :], wA[:, c, 512 + hc * 128:512 + (hc + 1) * 128], hnT[hb][:, c, :], c == 0, c == 7,
                               allh + [B_wA[2]], [Bp[kb_]])
                        TS("dve", QT[0:64, hc, 0, s_own * 512:(s_own + 1) * 512], ps[kb_][0:64, :], 0.125, None, ALU.mult, None,
                           [Bp[kb_], B_QTz], [B_QT[hc][s_own]])
                        TS("dve", QT[64:128, hc, 1, s_own * 512:(s_own + 1) * 512], ps[kb_][64:128, :], 0.125, None, ALU.mult, None,
                           [Bp[kb_], B_QTz, B_QT[hc][s_own]], [B_QT[hc][s_own]])
                if pr == 0 and i % 4 == 2:
                    for gg in range(4):
                        kb_ = kbank()
                        for c in range(8):
                            MM(ps[kb_][:, 0:16], wU[:, c, gg * 128:(gg + 1) * 128], hnT[hb][:, c, 496:512], c == 0, c == 7,
                               [B_hnT[hb][3], B_wU], [Bp[kb_]])
                        CP("dve", uT[:, gg, 0:16], ps[kb_][:, 0:16], [Bp[kb_]], [B_uTp[gg]])
                if pr == 0 and own:
                    for gg in range(4):
                        w = 2 ** (gg + 1)
                        kb_ = kbank()
                        for c in range(8):
                            MM(ps[kb_][:, :], wU[:, c, gg * 128:(gg + 1) * 128], hnT[hb][:, c, :], c == 0, c == 7,
                               allh + [B_wU], [Bp[kb_]])
                        CP("act", uT[:, gg, 16:528], ps[kb_][:, :], [Bp[kb_]], [B_uT[gg]])
                        U = uT[:, gg, :]
                        ru = [B_uT[gg], B_uTp[gg]]
                        TT("pool", ptmp[0][:, 1:528], U[:, 1:528], U[:, 0:527], ALU.add, ru, [B_pt[0]])
                        cur = 0
                        sh = 2
                        lo = 1
                        while sh < w:
                            lo += sh
                            TT("pool", ptmp[1 - cur][:, lo:528], ptmp[cur][:, lo:528], ptmp[cur][:, lo - sh:528 - sh], ALU.add,
                               [B_pt[cur]], [B_pt[1 - cur]])
                            cur = 1 - cur
                            sh *= 2
                        STT(dT[:, gg, :], ptmp[cur][:, 16:528], 1.0 / w, U[:, 16:528], ALU.mult, ALU.subtract,
                            [B_pt[cur]] + ru, [B_dT[gg]])
                        if s_own == 0:
                            tmp16 = small[:, 16:32]
                            TT("dve", tmp16, ptmp[cur][:, 16:32], pinv[:, gg * 16:(gg + 1) * 16], ALU.mult, [B_pt[cur], Bc], [Bc])
                            TT("dve", dT[:, gg, 0:16], tmp16, U[:, 16:32], ALU.subtract, [Bc] + ru + [B_dT[gg]], [B_dT[gg], Bc])

                    def stageB(s_own=s_own):
                        for gg in range(4):
                            kb2 = kbank()
                            MM(ps[kb2][:, :], pw[:, gg, :], dT[:, gg, :], True, True, [B_pw, B_dT[gg]], [Bp[kb2]])
                            TS("dve", mixT[:, 4 + gg, s_own * 512:(s_own + 1) * 512], ps[kb2][:, :], gcol(32 + gg), None, ALU.mult, None,
                               [Bp[kb2], Bc], [B_mix[4 + gg][s_own]])
                    late.append(stageB)

            def emit_load(g):
                xb = 4 * ((g // 4) % 2) + g % 4
                DMA("sp", xst[xb], x[g * 128:(g + 1) * 128, :], [], [B_xst[xb]])

            def emit_sq1(g):
                if pr == 0:
                    xb = 4 * ((g // 4) % 2) + g % 4
                    SQ(xst[xb], [B_xst[xb]], ssq_all[:, g:g + 1], [B_ssq[g]])

            def emit_stats(i, squares=True):
                for tt in range(4):
                    if squares:
                        emit_sq1(4 * i + tt)
                if pr == 0:
                    lv = lnv[:, 4 * (i % 2):4 * (i % 2) + 4]
                    ACT(lv, ssq_all[:, 4 * i:4 * i + 4], AF.Ln, B_ssq[4 * i:4 * i + 4], [B_ln[i % 2]], scale=1.0 / 1024.0, bias=1e-6)
                    ACT(rstd_all[:, 4 * i:4 * i + 4], lv, AF.Exp, [B_ln[i % 2]], [B_rslot[i]], scale=-0.5)

            pending = []
            late = []
            TB = (0, 1, 7)

            def partA(g):
                i_, tt_ = g // 4, g % 4
                xb = 4 * (i_ % 2) + tt_
                ACT(xs_t[g % nxs], xst[xb], AF.Copy, [B_xst[xb], B_rslot[i_]], [B_xs[g % nxs]], scale=rstd_all[:, g:g + 1])

            def partB(g):
                i_, tt_ = g // 4, g % 4
                hb_ = i_ % 2
                bank = TB[g % 3]
                xs = xs_t[g % nxs]
                for c in range(8):
                    TR(psb[bank][:, c * 128:(c + 1) * 128], xs[:, c * 128:(c + 1) * 128], [B_xs[g % nxs], Bc], [Bp[bank]])
                TT("dve", hnT[hb_][:, :, tt_ * 128:(tt_ + 1) * 128], psb[bank][:, 0:1024].rearrange("p (c n) -> p c n", c=8),
                   gain_bc(0), ALU.mult, [Bp[bank], Bc], [B_hnT[hb_][tt_]])

            for g in range(8):
                emit_load(g)
            emit_stats(0)
            emit_stats(1)
            partA(0)
            partA(1)
            emit_load(8)
            emit_load(9)
            for g in range(64):
                i, tt = g // 4, g % 4
                hb = i % 2
                if tt == 0:
                    run_late = late[:]
                    del late[:]
                if pr == 0 and g == 8:
                    bias_part1()
                if pr == 0 and g == 16:
                    bias_part1b()
                if pr == 0 and g == 24:
                    bias_part2()
                if pr == 0 and g == 40:
                    bias_part3()
                if g + 2 < 64:
                    partA(g + 2)
                if g + 10 < 64:
                    emit_load(g + 10)
                if g + 8 < 64:
                    emit_sq1(g + 8)
                    if tt == 3:
                        emit_stats(i + 2, squares=False)
                partB(g)
                cur_p = [lambda g=g, hb=hb, tt=tt: emit_V(g, hb, tt)]
                if tt == 3:
                    cur_p.append(lambda i=i, hb=hb: emit_slot(i, hb))
                if tt == 2:
                    cur_p.extend(run_late)
                pending.append(cur_p)
                if len(pending) > 2:
                    for f_ in pending.pop(0):
                        f_()
            for grp_ in pending:
                for f_ in grp_:
                    f_()
            for f_ in late:
                f_()
            pending = []
            late = []

            S.barrier()
            R3.reset(mark)
            NP = 3
            Pt = [R3.bf16(1024) for _ in range(NP)]
            B_P = [Buf() for _ in range(NP)]
            rs = [R3.f32(512) for _ in range(2)]
            tq = [R3.f32(512) for _ in range(2)]
            o_sb = R3.f32(512)
            sq_sb = R3.f32(512)
            r2_sb = R3.f32(512)
            B_fin = [Buf() for _ in range(8)]
            if pr == 1:
                TOP = R3.nbytes - 32 * 1024
                assert R3.off <= TOP, R3.off
                R3.reset(TOP)
                wO = R3.bf16(8 * 1024).rearrange("p (c n) -> p c n", c=8)
                wQ = R3.bf16(8 * 1024).rearrange("p (c n) -> p c n", c=8)
                B_wO = Buf()
                B_wQ = Buf()
                DMA("pool", wO, w_out.rearrange("(c p) n -> p c n", p=128), [], [B_wO])
                DMA("pool", wQ, wq.rearrange("(c p) n -> p c n", p=128), [], [B_wQ])
            flat = []
            for s in range(4):
                for hc in range(2):
                    nkb_ = 4 * (4 * s + 3 + 1)
                    for kb in range(nkb_):
                        flat.append((s, hc, kb, nkb_))
            n = len(flat)
            sbank = {}
            pbuf = {}
            rot = {"s": 0, "p": 0}

            def geom(t):
                s, hc, kb, nkb = flat[t]
                r = kb - 4 * (4 * s + 3)
                return s, hc, kb, nkb, r, 128 * max(r, 0)

            def QK(t):
                s, hc, kb, nkb, r, c0 = geom(t)
                b0 = 2 * (rot["s"] % 2)
                rot["s"] += 1
                sbank[t] = b0
                for m in range(2):
                    MM(ps[b0 + m][:, c0:512], KT[:, hc, kb * 128:(kb + 1) * 128],
                       QT[:, hc, m, s * 512 + c0:(s + 1) * 512], True, True,
                       [B_QT[hc][s]], [Bp[b0 + m]])

            def SOFT(t):
                s, hc, kb, nkb, r, c0 = geom(t)
                h = 2 * pr + hc
                b0 = sbank[t]
                for m in range(2):
                    b = b0 + m
                    if r >= 0:
                        TT("dve", ps[b][:, 128 * r:128 * r + 128], ps[b][:, 128 * r:128 * r + 128], biasT[:, h, 0, :], ALU.add,
                           [Bp[b], B_bias], [Bp[b]])
                    if r >= -1 and r + 1 <= 3:
                        cc = 128 * (r + 1)
                        TT("dve", ps[b][:, cc:cc + 128], ps[b][:, cc:cc + 128], biasT[:, h, 1, :], ALU.add,
                           [Bp[b], B_bias], [Bp[b]])
                pb = rot["p"] % NP
                rot["p"] += 1
                pbuf[t] = pb
                kw = {}
                if kb // 4 <= 2:
                    kw["bias"] = kvb[:, kb // 4:kb // 4 + 1]
                src = ps_all[:, b0 * 512:(b0 + 2) * 512].rearrange("p (b n) -> p b n", b=2)[:, :, c0:512]
                dst = Pt[pb].rearrange("p (b n) -> p b n", b=2)[:, :, c0:512]
                ACT(dst, src, AF.Exp, [Bp[b0], Bp[b0 + 1], Bc], [B_P[pb]], **kw)

            def PV(t):
                s, hc, kb, nkb, r, c0 = geom(t)
                pb = pbuf[t]
                first = (kb == 0)
                last = (kb == nkb - 1)
                for m in range(2):
                    Pm = Pt[pb][:, m * 512 + c0:(m + 1) * 512]
                    MM(ps[4 + m][:, c0:512], Vv[:, kb, hc * 128:(hc + 1) * 128], Pm, first, last,
                       [B_P[pb]], [Bp[4 + m]])
                    MM(ps[6 + m][:, c0:512], ones_bf, Pm, first, last, [B_P[pb], Bc], [Bp[6 + m]])

            def FIN(s, hc):
                h = 2 * pr + hc
                RECIP(rs[0], ps[6][:, :], [Bp[6]], [B_fin[0]])
                TT("dve", tq[0], ps[4][:, :], rs[0], ALU.mult, [Bp[4], B_fin[0]], [B_fin[2]])
                RECIP(rs[1], ps[7][:, :], [Bp[7]], [B_fin[1]])
                TT("dve", tq[1], ps[5][:, :], rs[1], ALU.mult, [Bp[5], B_fin[1]], [B_fin[3]])
                STT(o_sb, tq[1], neg_lam, tq[0], ALU.mult, ALU.add, [B_fin[2], B_fin[3], Bc], [B_fin[4]])
                ACT(sq_sb, o_sb, AF.Square, [B_fin[4]], [B_fin[5]])
                bm = 2 * (rot["s"] % 2)
                rot["s"] += 1
                MM(ps[bm][:, :], ones_f, sq_sb, True, True, [B_fin[5], Bc], [Bp[bm]])
                ACT(r2_sb, ps[bm][:, :], AF.Ln, [Bp[bm]], [B_fin[6]], bias=1e-6)
                ACT(r2_sb, r2_sb, AF.Exp, [B_fin[6]], [B_fin[6]], scale=-0.5)
                STT(mixT[:, h, s * 512:(s + 1) * 512], o_sb, subcol, r2_sb, ALU.mult, ALU.mult,
                    [B_fin[4], B_fin[6], Bc], [B_mix[h][s]])

            QK(0)
            for t in range(n):
                SOFT(t)
                if t + 1 < n:
                    QK(t + 1)
                PV(t)
                s_, hc_, kb_l, nkb_l = flat[t]
                if kb_l == nkb_l - 1:
                    FIN(s_, hc_)

        S.barrier()
        R3.reset()
        if debug:
            DMA("sp", dbg["dbg_mix"], R1.t16[:, 0:16384], [b for row in B_mix for b in row], [])
        wKV = R3.bf16(8 * 2048).rearrange("p (c n) -> p c n", c=8)
        mark_b = R3.off
        B_wKV = Buf()
        DMA("pool", wKV[:, :, 0:1024], wkv[:, 0:1024].rearrange("(c p) n -> p c n", p=128), [], [B_wKV])
        DMA("pool", wKV[:, :, 1024:2048], wkv[:, 1024:2048].rearrange("(c p) n -> p c n", p=128), [], [B_wKV])
        xo = [R3.f32(1024) for _ in range(2)]
        assert R3.off <= TOP
        B_xo = [Buf() for _ in range(2)]
        B_h = [Buf("h%d" % t) for t in range(16)]
        allmix = [b for row in B_mix for b in row]
        for tt in range(16):
            s_, t4 = tt // 4, tt % 4
            row0 = (4 * s_ + 3) * 512 + t4 * 128
            DMA("sp", xo[tt % 2], x[row0:row0 + 128, :], [], [B_xo[tt % 2]])
            for half in range(2):
                b = 2 * (tt % 2) + half
                for c in range(8):
                    MM(ps[b][:, :], mixT[:, c, tt * 128:(tt + 1) * 128], wO[:, c, half * 512:(half + 1) * 512], c == 0, c == 7,
                       [B_mix[c][s_], B_wO], [Bp[b]])
                TT("dve", hres[:, tt, half * 512:(half + 1) * 512], ps[b][:, :], xo[tt % 2][:, half * 512:(half + 1) * 512], ALU.add,
                   [Bp[b], B_xo[tt % 2]], [B_h[tt]])
            brs_c[tt] = rms_cols(hres[:, tt, :], B_h[tt], tt)

        def dump_h(name):
            if debug:
                for tt in range(16):
                    DMA("sp", dbg[name][tt * 128:(tt + 1) * 128, :], hres[:, tt, :], [B_h[tt]], [])

        dump_h("dbg_h1")

        S.barrier()
        R3.reset(mark_b)
        xs_t = [R3.bf16(1024) for _ in range(2)]
        B_xs = [Buf() for _ in range(2)]
        mnT = R3.bf16(8 * 256).rearrange("p (c n) -> p c n", c=8)
        B_mnT = [Buf() for _ in range(2)]
        KcT = R3.bf16(8 * 256).rearrange("p (c n) -> p c n", c=8)
        B_Kc = Buf()
        Vc = R3.bf16(2 * 1024).rearrange("p (k n) -> p k n", k=2)
        B_Vc = Buf()
        mark_c = R3.off
        mst = [R3.f32(1024) for _ in range(2)]
        B_mst = [Buf() for _ in range(2)]

        for mk in range(2):
            DMA("sp", mst[mk], mem[mk * 128:(mk + 1) * 128, :], [], [B_mst[mk]])
            brs = rms_cols(mst[mk], B_mst[mk], 32 + mk)
            norm_transpose(mst[mk], B_mst[mk], rstd_all[:, 32 + mk:33 + mk], 16, mnT[:, :, mk * 128:(mk + 1) * 128], B_mnT[mk],
                           xs_t[mk], B_xs[mk], mk, brs)
        for j8 in range(8):
            b = 4 + j8 % 4
            for c in range(8):
                MM(ps[b][:, 0:256], wKV[:, c, j8 * 128:(j8 + 1) * 128], mnT[:, c, :], c == 0, c == 7, B_mnT + [B_wKV], [Bp[b]])
            CP("dve" if j8 % 2 else "act", KcT[:, j8, :], ps[b][:, 0:256], [Bp[b]], [B_Kc])
        for mk in range(2):
            for half in range(2):
                b = 4 + (2 * mk + half) % 4
                for c in range(8):
                    MM(ps[b][:, :], mnT[:, c, mk * 128:(mk + 1) * 128], wKV[:, c, 1024 + half * 512:1024 + (half + 1) * 512], c == 0, c == 7,
                       B_mnT + [B_wKV], [Bp[b]])
                CP("dve" if half else "act", Vc[:, mk, half * 512:(half + 1) * 512], ps[b][:, :], [Bp[b]], [B_Vc])
        B_hT = [[Buf() for _ in range(4)] for _ in range(4)]
        for tt in range(16):
            norm_transpose(hres[:, tt, :], B_h[tt], rstd_all[:, tt:tt + 1], 8, hT[:, :, tt * 128:(tt + 1) * 128], B_hT[tt // 4][tt % 4],
                           xs_t[tt % 2], B_xs[tt % 2], tt % 2, brs_c[tt])
        S.barrier()
        wOc = wKV[:, :, 0:1024]
        B_wOc = Buf()
        DMA("pool", wOc, wo.rearrange("(c p) n -> p c n", p=128), [], [B_wOc])
        R3.reset(mark_c)
        qT = R3.bf16(8 * 512).rearrange("p (c n) -> p c n", c=8)
        B_qT = [Buf() for _ in range(8)]
        ocT = R3.bf16(8 * 512).rearrange("p (c n) -> p c n", c=8)
        B_oc = [Buf() for _ in range(8)]
        Pc = [R3.bf16(512) for _ in range(4)]
        B_Pc = [Buf() for _ in range(4)]
        rsc2 = [R3.f32(512) for _ in range(2)]
        B_rsc2 = [Buf() for _ in range(2)]
        pc_rot = 0
        for s in range(4):
            for j8 in range(8):
                b = j8 % 2
                for c in range(8):
                    MM(ps[b][:, :], wQ[:, c, j8 * 128:(j8 + 1) * 128], hT[:, c, s * 512:(s + 1) * 512], c == 0, c == 7,
                       B_hT[s] + [B_wQ], [Bp[b]])
                if j8 % 2:
                    TS("dve", qT[:, j8, :], ps[b][:, :], 1.0 / 16.0, None, ALU.mult, None, [Bp[b]], [B_qT[j8]])
                else:
                    ACT(qT[:, j8, :], ps[b][:, :], AF.Copy, [Bp[b]], [B_qT[j8]], scale=1.0 / 16.0)
            pcs_h = {}

            def c_scores(hh):
                nonlocal pc_rot
                pcs = []
                for mk in range(2):
                    b = 2 + mk
                    for e2 in range(2):
                        MM(ps[b][:, :], KcT[:, 2 * hh + e2, mk * 128:(mk + 1) * 128], qT[:, 2 * hh + e2, :], e2 == 0, e2 == 1,
                           [B_Kc, B_qT[2 * hh + e2]], [Bp[b]])
                    pi = pc_rot % 4
                    pc_rot += 1
                    pcs.append(pi)
                    ACT(Pc[pi], ps[b][:, :], AF.Exp, [Bp[b]], [B_Pc[pi]])
                pcs_h[hh] = pcs

            def c_pv(hh):
                pcs = pcs_h[hh]
                st_ = hh % 2
                ob = (4, 5) if st_ == 0 else (0, 1)
                sb_ = 6 if st_ == 0 else 7
                for e2 in range(2):
                    b = ob[e2]
                    for mk in range(2):
                        MM(ps[b][:, :], Vc[:, mk, (2 * hh + e2) * 128:(2 * hh + e2 + 1) * 128], Pc[pcs[mk]], mk == 0, mk == 1,
                           [B_Vc, B_Pc[pcs[mk]]], [Bp[b]])
                for mk in range(2):
                    MM(ps[sb_][:, :], ones_bf, Pc[pcs[mk]], mk == 0, mk == 1, [Bc, B_Pc[pcs[mk]]], [Bp[sb_]])
                RECIP(rsc2[st_], ps[sb_][:, :], [Bp[sb_]], [B_rsc2[st_]])
                for e2 in range(2):
                    TT("dve", ocT[:, 2 * hh + e2, :], ps[ob[e2]][:, :], rsc2[st_], ALU.mult, [Bp[ob[e2]], B_rsc2[st_]], [B_oc[2 * hh + e2]])

            c_scores(0)
            for hh in range(4):
                if hh + 1 < 4:
                    c_scores(hh + 1)
                c_pv(hh)
            for t4 in range(4):
                tt = 4 * s + t4
                for half in range(2):
                    b = half
                    for c in range(8):
                        MM(ps[b][:, :], ocT[:, c, t4 * 128:(t4 + 1) * 128], wOc[:, c, half * 512:(half + 1) * 512], c == 0, c == 7,
                           [B_oc[c], B_wOc], [Bp[b]])
                    TT("dve", hres[:, tt, half * 512:(half + 1) * 512], ps[b][:, :], hres[:, tt, half * 512:(half + 1) * 512], ALU.add,
                       [Bp[b], B_h[tt]], [B_h[tt]])
                brs_d[tt] = rms_cols(hres[:, tt, :], B_h[tt], 16 + tt)
        dump_h("dbg_h2")

        S.barrier()
        R3.reset()
        NU = 2
        ring = []
        for k in range(NU):
            unit = []
            for _e in range(2):
                wg_ = R3.bf16(8 * 256).rearrange("p (c n) -> p c n", c=8)
                wu_ = R3.bf16(8 * 256).rearrange("p (c n) -> p c n", c=8)
                wd_ = R3.bf16(2 * 1024).rearrange("p (f n) -> p f n", f=2)
                unit.append((wg_, wu_, wd_, Buf(), Buf(), Buf()))
            ring.append(unit)

        def load_unit(u):
            for e2 in range(2):
                e = 2 * u + e2
                wg_, wu_, wd_, b1, b2, b3 = ring[u % NU][e2]
                DMA("pool", wg_, w_gate[e].rearrange("(c p) n -> p c n", p=128), [], [b1])
                DMA("pool", wu_, w_up[e].rearrange("(c p) n -> p c n", p=128), [], [b2])
                DMA("pool", wd_, w_down[e].rearrange("(f p) n -> p f n", p=128), [], [b3])

        wR = R3.bf16(8 * 20).rearrange("p (c n) -> p c n", c=8)
        B_wR = Buf()
        DMA("pool", wR, wr.rearrange("(c p) n -> p c n", p=128), [], [B_wR])
        for u in range(NU):
            load_unit(u)
        selT = R3.bf16(2048)
        B_sel = Buf()
        DMA("pool", selT[0:16, :], sel_d, [], [B_sel])
        chi = R3.bf16(2048)
        clo = R3.bf16(2048)
        combT = R3.f32(2048)
        B_comb = [Buf() for _ in range(16)]
        rt_n = 16 * (20 + 16 + 4 * 8 + 16 + 9)
        rt = R3.f32(rt_n)
        B_rt = Buf()
        B_lg = [Buf() for _ in range(16)]
        _o = [0]

        def rtv(n_inner):
            o = _o[0]
            _o[0] += 16 * n_inner
            v = rt[:, o:o + 16 * n_inner]
            return v if n_inner == 1 else v.rearrange("p (t k) -> p t k", t=16)

        lg = rtv(20); comb = rtv(16); goh = rtv(4); gex = rtv(4); esel = rtv(4); oh1 = rtv(4); es2 = rtv(4); oh2 = rtv(4)
        inner = rtv(4); gsc = rtv(4); prod16 = rtv(16)
        gmax = rtv(1); gsum = rtv(1); gw = rtv(1); m1 = rtv(1); m2 = rtv(1); e21 = rtv(1); den = rtv(1); w1 = rtv(1); w2 = rtv(1)
        prod4 = prod16.rearrange("p t (g e) -> p t g e", g=4)
        comb4 = comb.rearrange("p t (g e) -> p t g e", g=4)

        def bcl(v, n):
            return bass.AP(v.tensor, v.offset, [list(d) for d in v.ap] + [[0, n]])

        def bcm(v, n):
            dd = [list(d) for d in v.ap]
            return bass.AP(v.tensor, v.offset, dd[:-1] + [[0, n]] + dd[-1:])

        actT = R3.bf16(2 * 2 * 512).rearrange("p (e f n) -> p e f n", e=2, f=2)
        B_act = [[Buf() for _ in range(2)] for _ in range(2)]
        sg = [R3.f32(512) for _ in range(2)]
        B_sg = [Buf() for _ in range(2)]
        tg = [R3.f32(512) for _ in range(2)]
        B_tg = [Buf() for _ in range(2)]
        bcs = [R3.f32(512) for _ in range(2)]
        B_bcs = [Buf() for _ in range(2)]
        xs_t = [tg[0].bitcast(BF16), tg[1].bitcast(BF16)]
        B_xs = [Buf() for _ in range(2)]
        B_h3T = [[Buf() for _ in range(4)] for _ in range(4)]
        for tt in range(16):
            norm_transpose(hres[:, tt, :], B_h[tt], rstd_all[:, 16 + tt:17 + tt], 24, hT[:, :, tt * 128:(tt + 1) * 128], B_h3T[tt // 4][tt % 4],
                           xs_t[tt % 2], B_xs[tt % 2], tt % 2, brs_d[tt])
            for c in range(8):
                MM(ps[2 + tt % 2][:, 0:20], hT[:, c, tt * 128:(tt + 1) * 128], wR[:, c, :], c == 0, c == 7,
                   [B_h3T[tt // 4][tt % 4], B_wR], [Bp[2 + tt % 2]])
            TT("dve", lg[:, tt, :], ps[2 + tt % 2][:, 0:20], rbias_bc, ALU.add, [Bp[2 + tt % 2], Bc], [B_lg[tt]])

        R_ = [B_rt]
        lgg = lg[:, :, 0:4]
        le4 = lg[:, :, 4:20].rearrange("p t (g e) -> p t g e", g=4)
        S.add("dve", lambda e: e.tensor_reduce(gmax, lgg, axis=AX.X, op=ALU.max), B_lg, R_)
        TT("dve", goh, lgg, bcl(gmax, 4), ALU.is_equal, B_lg + R_, R_)
        TT("dve", gex, lgg, bcl(gmax, 4), ALU.subtract, B_lg + R_, R_)
        ACT(gex, gex, AF.Exp, R_, R_)
        S.add("dve", lambda e: e.tensor_reduce(gsum, gex, axis=AX.X, op=ALU.add), R_, R_)
        RECIP(gw, gsum, R_, R_)
        TT("dve", prod4, le4, bcl(goh, 4), ALU.mult, B_lg + R_, R_)
        S.add("dve", lambda e: e.tensor_reduce(esel, prod4.rearrange("p t g e -> p t e g"), axis=AX.X, op=ALU.add), R_, R_)
        S.add("dve", lambda e: e.tensor_reduce(m1, esel, axis=AX.X, op=ALU.max), R_, R_)
        TT("dve", oh1, esel, bcl(m1, 4), ALU.is_equal, R_, R_)
        STT(es2, oh1, -1e30, esel, ALU.mult, ALU.add, R_, R_)
        S.add("dve", lambda e: e.tensor_reduce(m2, es2, axis=AX.X, op=ALU.max), R_, R_)
        TT("dve", oh2, es2, bcl(m2, 4), ALU.is_equal, R_, R_)
        TT("dve", e21, m2, m1, ALU.subtract, R_, R_)
        ACT(e21, e21, AF.Exp, R_, R_)
        TS("dve", den, e21, 1.0, None, ALU.add, None, R_, R_)
        RECIP(w1, den, R_, R_)
        TT("dve", w2, e21, w1, ALU.mult, R_, R_)
        TT("dve", inner, oh1, bcl(w1, 4), ALU.mult, R_, R_)
        TT("dve", oh2, oh2, bcl(w2, 4), ALU.mult, R_, R_)
        TT("dve", inner, inner, oh2, ALU.add, R_, R_)
        TT("dve", gsc, goh, bcl(gw, 4), ALU.mult, R_, R_)
        TT("dve", comb4, bcl(gsc, 4), bcm(inner, 4), ALU.mult, R_, R_)
        for q4 in range(4):
            pb_ = 2 + q4 % 2
            for t4 in range(4):
                tt = 4 * q4 + t4
                S.add("pe", lambda e, tt=tt, t4=t4, pb_=pb_: e.transpose(ps[pb_][0:16, t4 * 128:(t4 + 1) * 128], comb[:, tt, :], identf),
                      [B_rt, Bc], [Bp[pb_]])
            CP("dve", combT[0:16, q4 * 512:(q4 + 1) * 512], ps[pb_][0:16, :], [Bp[pb_]], B_comb[4 * q4:4 * q4 + 4])
            CP("dve", chi[0:16, q4 * 512:(q4 + 1) * 512], combT[0:16, q4 * 512:(q4 + 1) * 512], B_comb[4 * q4:4 * q4 + 4], B_comb[4 * q4:4 * q4 + 4])
            TT("dve", clo[0:16, q4 * 512:(q4 + 1) * 512], combT[0:16, q4 * 512:(q4 + 1) * 512], chi[0:16, q4 * 512:(q4 + 1) * 512], ALU.subtract,
               B_comb[4 * q4:4 * q4 + 4], B_comb[4 * q4:4 * q4 + 4])

        d_rot = [0]

        def gu_mm(u, s, e2, f):
            wg_, wu_, wd_, b1, b2, b3 = ring[u % NU][e2]
            bg = 2 * f
            bu = 2 * f + 1
            for c in range(8):
                MM(ps[bg][:, :], wg_[:, c, f * 128:(f + 1) * 128], hT[:, c, s * 512:(s + 1) * 512], c == 0, c == 7,
                   B_h3T[s] + [b1], [Bp[bg]])
            for c in range(8):
                MM(ps[bu][:, :], wu_[:, c, f * 128:(f + 1) * 128], hT[:, c, s * 512:(s + 1) * 512], c == 0, c == 7,
                   B_h3T[s] + [b2], [Bp[bu]])

        def gu_post(u, s, e2, f):
            e = 2 * u + e2
            bi = e2
            bg = 2 * f
            bu = 2 * f + 1
            if f == 0:
                MM(ps[6][:, :], selT[0:16, e * 128:(e + 1) * 128], chi[0:16, s * 512:(s + 1) * 512], True, False,
                   [B_sel] + B_comb[4 * s:4 * s + 4], [Bp[6]])
                MM(ps[6][:, :], selT[0:16, e * 128:(e + 1) * 128], clo[0:16, s * 512:(s + 1) * 512], False, True,
                   [B_sel] + B_comb[4 * s:4 * s + 4], [Bp[6]])
                CP("act", bcs[bi], ps[6][:, :], [Bp[6]], [B_bcs[bi]])
            ACT(sg[f], ps[bg][:, :], AF.Silu, [Bp[bg]], [B_sg[f]])
            TT("dve", tg[f], ps[bu][:, :], sg[f], ALU.mult, [Bp[bu], B_sg[f]], [B_tg[f]])
            TT("pool", actT[:, e2, f, :], tg[f], bcs[bi], ALU.mult, [B_tg[f], B_bcs[bi]], [B_act[e2][f]])

        def down(u, s):
            unit = ring[u % NU]
            for t4 in range(4):
                tt = 4 * s + t4
                for half in range(2):
                    b = 4 + d_rot[0] % 2
                    d_rot[0] += 1
                    k = 0
                    for e2 in range(2):
                        wd_, b3 = unit[e2][2], unit[e2][5]
                        for f in range(2):
                            MM(ps[b][:, :], actT[:, e2, f, t4 * 128:(t4 + 1) * 128], wd_[:, f, half * 512:(half + 1) * 512],
                               k == 0, k == 3, [B_act[e2][f], b3], [Bp[b]])
                            k += 1
                    TT("dve", hres[:, tt, half * 512:(half + 1) * 512], ps[b][:, :], hres[:, tt, half * 512:(half + 1) * 512], ALU.add,
                       [Bp[b], B_h[tt]], [B_h[tt]])
                if u == 7:
                    brs_f[tt] = rms_cols(hres[:, tt, :], B_h[tt], 34 + tt)

        steps = [(u, s) for u in range(8) for s in range(4)]
        gu_mm(0, 0, 0, 0)
        for k_, (u, s) in enumerate(steps):
            gu_post(u, s, 0, 0)
            gu_mm(u, s, 0, 1)
            gu_post(u, s, 0, 1)
            gu_mm(u, s, 1, 0)
            gu_post(u, s, 1, 0)
            gu_mm(u, s, 1, 1)
            gu_post(u, s, 1, 1)
            if k_ + 1 < len(steps):
                un, sn = steps[k_ + 1]
                gu_mm(un, sn, 0, 0)
            down(u, s)
            if s == 3 and u + NU < 8:
                load_unit(u + NU)
        dump_h("dbg_h3")

        S.barrier()
        R3.reset()
        yo = [R3.f32(1024) for _ in range(2)]
        B_yo = [Buf() for _ in range(2)]
        for tt in range(16):
            STT(yo[tt % 2], hres[:, tt, :], rstd_all[:, 34 + tt:35 + tt], fnorm_bc, ALU.mult, ALU.mult, [B_h[tt], brs_f[tt], Bc], [B_yo[tt % 2]])
            DMA("sp", y[tt * 128:(tt + 1) * 128, :], yo[tt % 2], [B_yo[tt % 2]], [])

        S.emit(nc, st)
    return nc


def _rel_bucket(rel):
    nb = 16
    max_exact = 8
    ret = (rel > 0).astype(np.int32) * nb
    n = np.abs(rel)
    nf = np.maximum(n, 1).astype(np.float32)
    large = max_exact + (np.log(nf / np.float32(max_exact)) / np.float32(math.log(128 / max_exact))
                         * np.float32(nb - max_exact)).astype(np.int32)
    large = np.minimum(large, nb - 1)
    return ret + np.where(n < max_exact, n, large)


_NC_CACHE = {}


def kernel(**inp):
    debug = int(inp.pop("_debug", 0)) if "_debug" in inp else 0
    f = lambda k: np.ascontiguousarray(np.asarray(inp[k], dtype=np.float32))
    x = f("x")
    mem = f("mem")
    i = np.arange(384)
    rel = 127 - i
    bk = _rel_bucket(rel.astype(np.int32))
    oh = np.zeros((32, 384), np.float32)
    oh[bk, i] = 1.0
    oh[15, :] -= 1.0
    kk = np.arange(128)[:, None]
    qq = np.arange(128)[None, :]
    maskT = np.where((kk < 64) | (qq >= 64), 0.0, NEG).astype(np.float32)
    ident = np.eye(128, dtype=np.float32)
    sel = np.zeros((16, 16, 128), np.float32)
    for e in range(16):
        sel[e, e, :] = 1.0
    sel = sel.reshape(16, 2048)
    gains = np.zeros((128, 40), np.float32)
    gains[:, 0:8] = f("attn_norm")[0].reshape(8, 128).T
    gains[:, 8:16] = f("cross_norm")[0].reshape(8, 128).T
    gains[:, 16:24] = f("mem_norm")[0].reshape(8, 128).T
    gains[:, 24:32] = f("ffn_norm")[0].reshape(8, 128).T
    gains[:, 32:36] = f("pool_scale")[0].reshape(4, 128).T
    gains[:, 36] = f("diff_subln")[0]
    wr = np.concatenate([f("router_group")[0], f("router_expert")[0].transpose(1, 0, 2).reshape(1024, 16)], axis=1)
    rbias = np.concatenate([f("router_group_bias")[0], f("router_expert_bias")[0].reshape(16)])[None, :]
    lamv = np.concatenate([f("lambda_q1")[0], f("lambda_k1")[0], f("lambda_q2")[0], f("lambda_k2")[0]])[None, :]
    shared = {
        "w_in": f("w_in")[0], "w_out": f("w_out")[0], "wq": f("wq_cross")[0], "wkv": f("wkv_cross")[0], "wo": f("wo_cross")[0],
        "w_gate": f("w_gate")[0], "w_up": f("w_up")[0], "w_down": f("w_down")[0], "pool_w": f("pool_w")[0],
        "wr": np.ascontiguousarray(wr), "rbias": np.ascontiguousarray(rbias), "rel_bias": f("rel_bias"),
        "lamv": np.ascontiguousarray(lamv), "gains": gains, "fnorm": f("final_norm")[None, :],
        "ident": ident, "oh": oh, "maskT": maskT, "sel": sel,
    }
    in_maps = []
    for c in range(8):
        b, j = c // 4, c % 4
        pad = 3 - j
        xs = np.zeros((8192, 1024), np.float32)
        xs[pad * 512:] = x[b, :(16 - pad) * 512]
        kvb = np.zeros((128, 16), np.float32)
        kvb[:, :pad] = NEG
        pinv = np.zeros((128, 4, 16), np.float32)
        for g in range(4):
            w = 2 ** (g + 1)
            if j == 0:
                pinv[:, g, :] = 1.0 / np.minimum(np.arange(1, 17), w)
            else:
                pinv[:, g, :] = 1.0 / w
        m = dict(shared)
        m.update({"x": xs, "mem": mem[b], "kvb": kvb, "pinv": pinv.reshape(128, 64)})
        in_maps.append(m)
    key = debug
    if key not in _NC_CACHE:
        _NC_CACHE[key] = build_nc(debug)
    nc = _NC_CACHE[key]
    res = run_bass_kernel_spmd(nc, in_maps, core_ids=list(range(8)))
    out = np.zeros((2, 8192, 1024), np.float32)
    extra = {}
    for c in range(8):
        b, j = c // 4, c % 4
        r = res.results[c]
        for s in range(4):
            t = 4 * s + j
            out[b, t * 512:(t + 1) * 512] = r["y"][s * 512:(s + 1) * 512]
        if debug:
            extra[c] = {k: v for k, v in r.items() if k.startswith("dbg")}
    if debug:
        return out, extra
    return out
```

```python
import math
import numpy as np
from contextlib import ExitStack
import concourse.bass as bass
import concourse.mybir as mybir
from concourse.bass_utils import run_bass_kernel_spmd

F32 = mybir.dt.float32
BF16 = mybir.dt.bfloat16
AF = mybir.ActivationFunctionType
ALU = mybir.AluOpType
AX = mybir.AxisListType

COMPUTE = ("pe", "act", "dve", "pool")
ENGS = ("pe", "act", "dve", "pool", "sp")
NEG = -30000.0


class Buf:
    __slots__ = ("name", "writer", "rd_eng", "rd_dma")

    def __init__(self, name=""):
        self.name = name
        self.writer = None
        self.rd_eng = {}
        self.rd_dma = []


class Op:
    __slots__ = ("eng", "idx", "fn", "waits", "signal", "num", "dma", "dma_i", "clock", "slotwait")


class Sched:
    def __init__(self, K=8):
        self.ops = {e: [] for e in ENGS}
        self.known = {e: {c: -1 for c in COMPUTE} for e in ENGS}
        self.dma_known = {e: set() for e in ENGS}
        self.ndma = {e: 0 for e in ENGS}
        self.dma_ops = {e: [] for e in ENGS}
        self.bar = {e: [] for e in ENGS}
        self.K = K

    def barrier(self):
        lasts = []
        for e in COMPUTE:
            for op in reversed(self.ops[e]):
                if not op.dma:
                    lasts.append(op)
                    break
        for e in ENGS:
            self.bar[e] = list(lasts)

    def add(self, eng, fn, reads=(), writes=(), dma=False):
        op = Op()
        op.eng = eng
        op.idx = len(self.ops[eng])
        op.fn = fn
        op.dma = dma
        op.signal = False
        op.num = None
        op.dma_i = None
        op.slotwait = None
        deps = []
        if self.bar[eng]:
            deps.extend(d for d in self.bar[eng] if not (d.eng == eng and eng == "pe"))
            self.bar[eng] = []
        for b in reads:
            if b.writer is not None:
                deps.append(b.writer)
        for b in writes:
            if b.writer is not None:
                deps.append(b.writer)
            deps.extend(b.rd_eng.values())
            deps.extend(b.rd_dma)
        known = self.known[eng]
        best = {}
        dwaits = []
        for d in deps:
            if d.dma:
                if id(d) not in self.dma_known[eng]:
                    self.dma_known[eng].add(id(d))
                    dwaits.append(d)
            else:
                if d.eng == eng and eng == "pe":
                    continue
                if known[d.eng] >= d.idx:
                    continue
                if d.eng not in best or best[d.eng].idx < d.idx:
                    best[d.eng] = d
        waits = list(best.values()) + dwaits
        for d in waits:
            d.signal = True
            for c in COMPUTE:
                if d.clock[c] > known[c]:
                    known[c] = d.clock[c]
            if not d.dma and d.idx > known[d.eng]:
                known[d.eng] = d.idx
        if dma:
            i = self.ndma[eng]
            op.dma_i = i
            self.ndma[eng] = i + 1
            if i >= self.K:
                prev = self.dma_ops[eng][i - self.K]
                op.slotwait = prev
                self.dma_known[eng].add(id(prev))
            self.dma_ops[eng].append(op)
        op.waits = waits
        op.clock = dict(known)
        if not dma and eng in COMPUTE:
            op.clock[eng] = op.idx
        for b in reads:
            if dma:
                b.rd_dma.append(op)
            else:
                b.rd_eng[eng] = op
        for b in writes:
            b.writer = op
            b.rd_eng = {}
            b.rd_dma = []
        self.ops[eng].append(op)
        return op

    def emit(self, nc, stack):
        sem_eng = {e: stack.enter_context(nc.semaphore("s_" + e)) for e in COMPUTE}
        sem_dma = {e: [stack.enter_context(nc.semaphore("d_%s%d" % (e, k))) for k in range(self.K)]
                   for e in ENGS if self.ndma[e] > 0}
        for e in COMPUTE:
            n = 0
            for op in self.ops[e]:
                if op.signal and not op.dma:
                    n += 1
                    op.num = n
        K = self.K

        def dma_target(d):
            return sem_dma[d.eng][d.dma_i % K], 16 * (d.dma_i // K + 1)

        def run(ename, e):
            for op in self.ops[ename]:
                if op.slotwait is not None:
                    s, v = dma_target(op.slotwait)
                    e.wait_ge(s, v)
                for d in op.waits:
                    if d.dma:
                        s, v = dma_target(d)
                        e.wait_ge(s, v)
                    else:
                        e.wait_ge(sem_eng[d.eng], d.num)
                ins = op.fn(e)
                if op.dma:
                    s, v = dma_target(op)
                    ins.then_inc(s, 16)
                elif op.signal:
                    ins.then_inc(sem_eng[ename], 1)
            for d in self.dma_ops[ename][-K:]:
                s, v = dma_target(d)
                e.wait_ge(s, v)

        block = stack.enter_context(nc.Block())

        @block.tensor
        def _(e):
            run("pe", e)

        @block.scalar
        def _(e):
            run("act", e)

        @block.vector
        def _(e):
            run("dve", e)

        @block.gpsimd
        def _(e):
            run("pool", e)

        @block.sync
        def _(e):
            run("sp", e)


class Arena:
    def __init__(self, nc, st, name, nbytes):
        self.t32 = st.enter_context(nc.sbuf_tensor(name, [128, nbytes // 4], F32))
        self.t16 = self.t32.bitcast(BF16)
        self.nbytes = nbytes
        self.off = 0

    def reset(self, off=0):
        self.off = off

    def f32(self, n):
        o = self.off
        self.off += 4 * n
        assert self.off <= self.nbytes, (self.off, self.nbytes)
        return self.t32[:, o // 4:o // 4 + n]

    def bf16(self, n):
        o = self.off
        self.off += 2 * n
        self.off = (self.off + 3) // 4 * 4
        assert self.off <= self.nbytes, (self.off, self.nbytes)
        return self.t16[:, o // 2:o // 2 + n]


def build_nc(debug=0):
    nc = bass.Bass("TRN2", target_bir_lowering=False)

    def din(name, shape):
        return nc.dram_tensor(name, shape, F32, kind="ExternalInput")

    x_t = din("x", [8192, 1024]); x = x_t.ap()
    mem = din("mem", [256, 1024]).ap()
    w_in = din("w_in", [1024, 2048]).ap()
    w_out = din("w_out", [1024, 1024]).ap()
    wq = din("wq", [1024, 1024]).ap()
    wkv = din("wkv", [1024, 2048]).ap()
    wo = din("wo", [1024, 1024]).ap()
    w_gate = din("w_gate", [16, 1024, 256]).ap()
    w_up = din("w_up", [16, 1024, 256]).ap()
    w_down = din("w_down", [16, 256, 1024]).ap()
    pool_w = din("pool_w", [4, 128, 128]).ap()
    wr = din("wr", [1024, 20]).ap()
    rbias_t = din("rbias", [1, 20])
    rel_bias = din("rel_bias", [32, 4]).ap()
    lamv_t = din("lamv", [1, 256])
    gains_d = din("gains", [128, 40]).ap()
    fnorm_t = din("fnorm", [1, 1024])
    ident_d = din("ident", [128, 128]).ap()
    oh_d = din("oh", [32, 384]).ap()
    maskT_d = din("maskT", [128, 128]).ap()
    kvb_d = din("kvb", [128, 16]).ap()
    pinv_d = din("pinv", [128, 64]).ap()
    sel_d = din("sel", [16, 2048]).ap()
    y = nc.dram_tensor("y", [2048, 1024], F32, kind="ExternalOutput").ap()
    E_t = nc.dram_tensor("Escr", [4, 128, 384], F32, kind="Internal")
    dbg = {}
    if debug:
        for nm in ("dbg_h1", "dbg_h2", "dbg_h3"):
            dbg[nm] = nc.dram_tensor(nm, [2048, 1024], F32, kind="ExternalOutput").ap()
        dbg["dbg_mix"] = nc.dram_tensor("dbg_mix", [128, 8 * 2048], BF16, kind="ExternalOutput").ap()

    S = Sched()
    st = ExitStack()
    with st:
        G = Arena(nc, st, "G", 18 * 1024)
        ident = G.bf16(128)
        ones_bf = G.bf16(128)
        ones_f = G.f32(128)
        gains = G.f32(40)
        gains_t = G.t32
        gains_off = gains.offset
        identf = G.f32(128)
        fnorm_bc = G.f32(1024)
        rbias_bc = G.f32(20)
        lam_sb = G.f32(256)
        small = G.f32(64)
        relb = G.f32(4)
        oh_sb = G.f32(384)
        g_sb = G.f32(384)
        g_off = g_sb.offset
        maskT = G.f32(128)
        kvb = G.f32(16)
        pinv = G.f32(64)
        biasT = G.f32(1024).rearrange("p (h t q) -> p h t q", h=4, t=2)
        rstd_all = G.f32(64)
        ssq_all = G.f32(64)
        lnv = G.f32(8)
        FP8 = mybir.dt.float8e4
        g8 = G.t32.bitcast(FP8)
        junks = [g8[:, G.off + 1024 * k:G.off + 1024 * (k + 1)] for k in range(2)]
        G.off += 2048
        B_junk = [Buf() for _ in range(2)]
        jctr = [0]

        def SQ(src, Bsrc, accum, wr_):
            k = jctr[0] % 2
            jctr[0] += 1
            return S.add("act", lambda e: e.activation(junks[k], src, AF.Square, accum_out=accum, saturate=False),
                         Bsrc, wr_ + [B_junk[k]])
        lnv_ctr = [0]
        R1 = Arena(nc, st, "R1", 32 * 1024)
        R2 = Arena(nc, st, "R2", 64 * 1024)
        R3 = Arena(nc, st, "R3", 92 * 1024)
        ps_all = st.enter_context(nc.psum_tensor("ps_all", [128, 4096], F32))
        psb_all = ps_all.bitcast(BF16)
        ps = [ps_all[:, i * 512:(i + 1) * 512] for i in range(8)]
        psb = [psb_all[:, i * 1024:(i + 1) * 1024] for i in range(8)]
        Bp = [Buf("ps%d" % i) for i in range(8)]

        mixT = R1.t16[:, 0:16384].rearrange("p (c n) -> p c n", c=8)
        hT = mixT
        KT = R2.t16[:, 0:16384].rearrange("p (h n) -> p h n", h=2)
        Vv = R2.t16[:, 16384:32768].rearrange("p (k n) -> p k n", k=64)
        hres = R2.t32[:, 0:16384].rearrange("p (t n) -> p t n", t=16)

        def MM(out, lhsT, rhs, start, stop, rd, wr_):
            return S.add("pe", lambda e: e.matmul(out, lhsT, rhs, start=start, stop=stop), rd, wr_)

        def TR(out, in_, rd, wr_):
            return S.add("pe", lambda e: e.transpose(out, in_, ident), rd, wr_)

        def ACT(out, in_, func, rd, wr_, **kw):
            return S.add("act", lambda e: e.activation(out, in_, func, **kw), rd, wr_)

        def TT(eng, out, in0, in1, op, rd, wr_):
            return S.add(eng, lambda e: e.tensor_tensor(out, in0, in1, op), rd, wr_)

        def TS(eng, out, in0, s1, s2, op0, op1, rd, wr_):
            if op1 is None:
                return S.add(eng, lambda e: e.tensor_scalar(out, in0, s1, None, op0), rd, wr_)
            return S.add(eng, lambda e: e.tensor_scalar(out, in0, s1, s2, op0, op1), rd, wr_)

        def STT(out, in0, scalar, in1, op0, op1, rd, wr_):
            return S.add("dve", lambda e: e.scalar_tensor_tensor(out, in0, scalar, in1, op0, op1), rd, wr_)

        def CP(eng, out, in_, rd, wr_):
            if eng == "act":
                return S.add("act", lambda e: e.copy(out, in_), rd, wr_)
            return S.add(eng, lambda e: e.tensor_copy(out, in_), rd, wr_)

        def RECIP(out, in_, rd, wr_):
            return S.add("dve", lambda e: e.reciprocal(out, in_), rd, wr_)

        def DMA(q, out, in_, rd, wr_):
            return S.add(q, lambda e: e.dma_start(out=out, in_=in_), rd, wr_, dma=True)

        def gain_bc(c0):
            return bass.AP(gains_t, gains_off + c0, [[G.nbytes // 4, 128], [1, 8], [0, 128]])

        def gcol(c):
            return gains[:, c:c + 1]

        Bc = Buf("consts")
        B_g = Buf("g_sb")
        B_E = Buf("E")
        B_bias = Buf("biasT")
        DMA("pool", ident, ident_d, [], [Bc])
        DMA("sp", identf, ident_d, [], [Bc])
        DMA("sp", gains, gains_d, [], [Bc])
        DMA("sp", fnorm_bc, bass.AP(fnorm_t, 0, [[0, 128], [1, 1024]]), [], [Bc])
        DMA("sp", rbias_bc, bass.AP(rbias_t, 0, [[0, 128], [1, 20]]), [], [Bc])
        DMA("sp", lam_sb, bass.AP(lamv_t, 0, [[0, 128], [1, 256]]), [], [Bc])
        DMA("sp", relb[0:32, :], rel_bias, [], [Bc])
        DMA("sp", oh_sb[0:32, :], oh_d, [], [Bc])
        DMA("sp", maskT, maskT_d, [], [Bc])
        DMA("sp", kvb, kvb_d, [], [Bc])
        DMA("sp", pinv, pinv_d, [], [Bc])
        S.add("dve", lambda e: e.memset(ones_bf, 1.0), [], [Bc])
        S.add("dve", lambda e: e.memset(ones_f, 1.0 / 128.0), [], [Bc])
        prod = G.f32(128)
        TT("dve", prod[:, 0:64], lam_sb[:, 0:64], lam_sb[:, 64:128], ALU.mult, [Bc], [Bc])
        TT("dve", prod[:, 64:128], lam_sb[:, 128:192], lam_sb[:, 192:256], ALU.mult, [Bc], [Bc])
        S.add("dve", lambda e: e.reduce_sum(small[:, 0:1], prod[:, 0:64], axis=AX.X), [Bc], [Bc])
        S.add("dve", lambda e: e.reduce_sum(small[:, 1:2], prod[:, 64:128], axis=AX.X), [Bc], [Bc])
        ACT(small[:, 2:4], small[:, 0:2], AF.Exp, [Bc], [Bc])
        TT("dve", small[:, 4:5], small[:, 3:4], small[:, 2:3], ALU.subtract, [Bc], [Bc])
        TS("dve", small[:, 4:5], small[:, 4:5], -0.2, None, ALU.add, None, [Bc], [Bc])
        TS("dve", small[:, 5:6], gcol(36), 0.8, None, ALU.mult, None, [Bc], [Bc])
        neg_lam = small[:, 4:5]
        subcol = small[:, 5:6]
        def bias_part1():
            MM(ps[7][0:4, 0:384], relb[0:32, 0:4], oh_sb[0:32, 0:384], True, True, [Bc], [Bp[7]])
            CP("dve", g_sb[0:4, :], ps[7][0:4, 0:384], [Bp[7]], [B_g])

        def bias_part1b():
            DMA("sp", E_t.ap(), bass.AP(G.t32, g_off, [[G.nbytes // 4, 4], [0, 128], [1, 384]]), [B_g], [B_E])

        def bias_part2():
            DMA("sp", biasT[:, :, 0, :], bass.AP(E_t, 127, [[383, 128], [128 * 384, 4], [1, 128]]), [B_E], [B_bias])
            DMA("sp", biasT[:, :, 1, :], bass.AP(E_t, 255, [[383, 128], [128 * 384, 4], [1, 128]]), [B_E], [B_bias])

        def bias_part3():
            mask_bc = bass.AP(G.t32, maskT.offset, [[G.nbytes // 4, 128], [0, 4], [1, 128]])
            TT("dve", biasT[:, :, 0, :], biasT[:, :, 0, :], mask_bc, ALU.add, [B_bias, Bc], [B_bias])

        def norm_transpose(src, Bsrc, rstd_col, gain_c0, dst, Bdst, xs, Bxs, bank, Brs):
            ACT(xs, src, AF.Copy, [Bsrc, Brs], [Bxs], scale=rstd_col)
            for c in range(8):
                TR(psb[bank][:, c * 128:(c + 1) * 128], xs[:, c * 128:(c + 1) * 128], [Bxs, Bc], [Bp[bank]])
            TT("dve", dst, psb[bank][:, 0:1024].rearrange("p (c n) -> p c n", c=8), gain_bc(gain_c0), ALU.mult,
               [Bp[bank], Bc], [Bdst])

        B_lnc = [Buf() for _ in range(8)]

        def rms_cols(src, Bsrc, col):
            k = lnv_ctr[0] % 8
            lnv_ctr[0] += 1
            b1 = Buf()
            b3 = Buf()
            SQ(src, [Bsrc], ssq_all[:, col:col + 1], [b1])
            ACT(lnv[:, k:k + 1], ssq_all[:, col:col + 1], AF.Ln, [b1], [B_lnc[k]], scale=1.0 / 1024.0, bias=1e-6)
            ACT(rstd_all[:, col:col + 1], lnv[:, k:k + 1], AF.Exp, [B_lnc[k]], [b3], scale=-0.5)
            return b3
        brs_c = [None] * 16
        brs_d = [None] * 16
        brs_f = [None] * 16
        B_ssq = [Buf() for _ in range(64)]
        B_ln = [Buf() for _ in range(2)]
        B_rslot = [Buf() for _ in range(16)]
        B_mix = [[Buf("mix%d_%d" % (c, s)) for s in range(4)] for c in range(8)]

        for pr in range(2):
            S.barrier()
            R3.reset()
            wA = R3.bf16(8 * 768).rearrange("p (c n) -> p c n", c=8)
            B_wA = [Buf() for _ in range(3)]
            for part, c0 in enumerate((512 + 256 * pr, 1024 + 256 * pr, 256 * pr)):
                DMA("pool", wA[:, :, part * 256:(part + 1) * 256],
                    w_in[:, c0:c0 + 256].rearrange("(c p) n -> p c n", p=128), [], [B_wA[part]])
            if pr == 0:
                wU = R3.bf16(8 * 512).rearrange("p (c n) -> p c n", c=8)
                B_wU = Buf()
                DMA("pool", wU, w_in[:, 1536:2048].rearrange("(c p) n -> p c n", p=128), [], [B_wU])
                pw = R3.bf16(512).rearrange("p (g n) -> p g n", g=4)
                B_pw = Buf()
                DMA("pool", pw, pool_w.rearrange("g c d -> c g d"), [], [B_pw])
            QT = R3.bf16(2 * 2 * 2048).rearrange("p (h m n) -> p h m n", h=2, m=2)
            B_QT = [[Buf() for _ in range(4)] for _ in range(2)]
            B_QTz = Buf()
            S.add("pool", lambda e, QT=QT: e.memset(QT.rearrange("p h m n -> p (h m n)"), 0.0), [], [B_QTz])
            mark = R3.off
            xst = [R3.f32(1024) for _ in range(4)]
            if pr == 0:
                xst += [R1.t32[:, k * 1024:(k + 1) * 1024] for k in range(4)]
            else:
                xst += [R3.f32(1024) for _ in range(4)]
            B_xst = [Buf() for _ in range(8)]
            nxs = 3 if pr == 0 else 4
            xs_t = [R3.bf16(1024) for _ in range(nxs)]
            B_xs = [Buf() for _ in range(nxs)]
            hnT = [R3.bf16(8 * 512).rearrange("p (c n) -> p c n", c=8) for _ in range(2)]
            B_hnT = [[Buf() for _ in range(4)] for _ in range(2)]
            if pr == 0:
                uT = R3.f32(4 * 528).rearrange("p (g n) -> p g n", g=4)
                B_uT = [Buf() for _ in range(4)]
                B_uTp = [Buf() for _ in range(4)]
                ptmp = [R3.f32(528) for _ in range(2)]
                B_pt = [Buf() for _ in range(2)]
                dT = R3.bf16(4 * 512).rearrange("p (g n) -> p g n", g=4)
                B_dT = [Buf() for _ in range(4)]
            kq = [0]

            def kbank():
                b_ = 4 + kq[0] % 3
                kq[0] += 1
                return b_

            def emit_V(g, hb, tt):
                vb = 2 + g % 2
                for c in range(8):
                    MM(ps[vb][:, 0:256], hnT[hb][:, c, tt * 128:(tt + 1) * 128], wA[:, c, 256:512], c == 0, c == 7,
                       [B_hnT[hb][tt], B_wA[1]], [Bp[vb]])
                CP("act" if pr == 1 else "dve", Vv[:, g, :], ps[vb][:, 0:256], [Bp[vb]], [])

            def emit_slot(i, hb):
                own = (i % 4 == 3)
                s_own = i // 4
                allh = B_hnT[hb]
                for hc in range(2):
                    kb_ = kbank()
                    for c in range(8):
                        MM(ps[kb_][:, :], wA[:, c, hc * 128:(hc + 1) * 128], hnT[hb][:, c, :], c == 0, c == 7,
                           allh + [B_wA[0]], [Bp[kb_]])
                    CP("dve" if (hc or pr == 0) else "act", KT[:, hc, i * 512:(i + 1) * 512], ps[kb_][:, :], [Bp[kb_]], [])
                if own:
                    for hc in range(2):
                        kb_ = kbank()
                        for c in range(8):
                            MM(ps[kb_][:, :], wA[:, c, 512 + hc * 128:512 + (hc + 1) * 128], hnT[hb][:, c, :], c == 0, c == 7,
                               allh + [B_wA[2]], [Bp[kb_]])
                        TS("dve", QT[0:64, hc, 0, s_own * 512:(s_own + 1) * 512], ps[kb_][0:64, :], 0.125, None, ALU.mult, None,
                           [Bp[kb_], B_QTz], [B_QT[hc][s_own]])
                        TS("dve", QT[64:128, hc, 1, s_own * 512:(s_own + 1) * 512], ps[kb_][64:128, :], 0.125, None, ALU.mult, None,
                           [Bp[kb_], B_QTz, B_QT[hc][s_own]], [B_QT[hc][s_own]])
                if pr == 0 and i % 4 == 2:
                    for gg in range(4):
                        kb_ = kbank()
                        for c in range(8):
                            MM(ps[kb_][:, 0:16], wU[:, c, gg * 128:(gg + 1) * 128], hnT[hb][:, c, 496:512], c == 0, c == 7,
                               [B_hnT[hb][3], B_wU], [Bp[kb_]])
                        CP("dve", uT[:, gg, 0:16], ps[kb_][:, 0:16], [Bp[kb_]], [B_uTp[gg]])
                if pr == 0 and own:
                    for gg in range(4):
                        w = 2 ** (gg + 1)
                        kb_ = kbank()
                        for c in range(8):
                            MM(ps[kb_][:, :], wU[:, c, gg * 128:(gg + 1) * 128], hnT[hb][:, c, :], c == 0, c == 7,
                               allh + [B_wU], [Bp[kb_]])
                        CP("act", uT[:, gg, 16:528], ps[kb_][:, :], [Bp[kb_]], [B_uT[gg]])
                        U = uT[:, gg, :]
                        ru = [B_uT[gg], B_uTp[gg]]
                        TT("pool", ptmp[0][:, 1:528], U[:, 1:528], U[:, 0:527], ALU.add, ru, [B_pt[0]])
                        cur = 0
                        sh = 2
                        lo = 1
                        while sh < w:
                            lo += sh
                            TT("pool", ptmp[1 - cur][:, lo:528], ptmp[cur][:, lo:528], ptmp[cur][:, lo - sh:528 - sh], ALU.add,
                               [B_pt[cur]], [B_pt[1 - cur]])
                            cur = 1 - cur
                            sh *= 2
                        STT(dT[:, gg, :], ptmp[cur][:, 16:528], 1.0 / w, U[:, 16:528], ALU.mult, ALU.subtract,
                            [B_pt[cur]] + ru, [B_dT[gg]])
                        if s_own == 0:
                            tmp16 = small[:, 16:32]
                            TT("dve", tmp16, ptmp[cur][:, 16:32], pinv[:, gg * 16:(gg + 1) * 16], ALU.mult, [B_pt[cur], Bc], [Bc])
                            TT("dve", dT[:, gg, 0:16], tmp16, U[:, 16:32], ALU.subtract, [Bc] + ru + [B_dT[gg]], [B_dT[gg], Bc])

                    def stageB(s_own=s_own):
                        for gg in range(4):
                            kb2 = kbank()
                            MM(ps[kb2][:, :], pw[:, gg, :], dT[:, gg, :], True, True, [B_pw, B_dT[gg]], [Bp[kb2]])
                            TS("dve", mixT[:, 4 + gg, s_own * 512:(s_own + 1) * 512], ps[kb2][:, :], gcol(32 + gg), None, ALU.mult, None,
                               [Bp[kb2], Bc], [B_mix[4 + gg][s_own]])
                    late.append(stageB)

            def emit_load(g):
                xb = 4 * ((g // 4) % 2) + g % 4
                DMA("sp", xst[xb], x[g * 128:(g + 1) * 128, :], [], [B_xst[xb]])

            def emit_sq1(g):
                if pr == 0:
                    xb = 4 * ((g // 4) % 2) + g % 4
                    SQ(xst[xb], [B_xst[xb]], ssq_all[:, g:g + 1], [B_ssq[g]])

            def emit_stats(i, squares=True):
                for tt in range(4):
                    if squares:
                        emit_sq1(4 * i + tt)
                if pr == 0:
                    lv = lnv[:, 4 * (i % 2):4 * (i % 2) + 4]
                    ACT(lv, ssq_all[:, 4 * i:4 * i + 4], AF.Ln, B_ssq[4 * i:4 * i + 4], [B_ln[i % 2]], scale=1.0 / 1024.0, bias=1e-6)
                    ACT(rstd_all[:, 4 * i:4 * i + 4], lv, AF.Exp, [B_ln[i % 2]], [B_rslot[i]], scale=-0.5)

            pending = []
            late = []
            TB = (0, 1, 7)

            def partA(g):
                i_, tt_ = g // 4, g % 4
                xb = 4 * (i_ % 2) + tt_
                ACT(xs_t[g % nxs], xst[xb], AF.Copy, [B_xst[xb], B_rslot[i_]], [B_xs[g % nxs]], scale=rstd_all[:, g:g + 1])

            def partB(g):
                i_, tt_ = g // 4, g % 4
                hb_ = i_ % 2
                bank = TB[g % 3]
                xs = xs_t[g % nxs]
                for c in range(8):
                    TR(psb[bank][:, c * 128:(c + 1) * 128], xs[:, c * 128:(c + 1) * 128], [B_xs[g % nxs], Bc], [Bp[bank]])
                TT("dve", hnT[hb_][:, :, tt_ * 128:(tt_ + 1) * 128], psb[bank][:, 0:1024].rearrange("p (c n) -> p c n", c=8),
                   gain_bc(0), ALU.mult, [Bp[bank], Bc], [B_hnT[hb_][tt_]])

            for g in range(8):
                emit_load(g)
            emit_stats(0)
            emit_stats(1)
            partA(0)
            partA(1)
            emit_load(8)
            emit_load(9)
            for g in range(64):
                i, tt = g // 4, g % 4
                hb = i % 2
                if tt == 0:
                    run_late = late[:]
                    del late[:]
                if pr == 0 and g == 8:
                    bias_part1()
                if pr == 0 and g == 16:
                    bias_part1b()
                if pr == 0 and g == 24:
                    bias_part2()
                if pr == 0 and g == 40:
                    bias_part3()
                if g + 2 < 64:
                    partA(g + 2)
                if g + 10 < 64:
                    emit_load(g + 10)
                if g + 8 < 64:
                    emit_sq1(g + 8)
                    if tt == 3:
                        emit_stats(i + 2, squares=False)
                partB(g)
                cur_p = [lambda g=g, hb=hb, tt=tt: emit_V(g, hb, tt)]
                if tt == 3:
                    cur_p.append(lambda i=i, hb=hb: emit_slot(i, hb))
                if tt == 2:
                    cur_p.extend(run_late)
                pending.append(cur_p)
                if len(pending) > 2:
                    for f_ in pending.pop(0):
                        f_()
            for grp_ in pending:
                for f_ in grp_:
                    f_()
            for f_ in late:
                f_()
            pending = []
            late = []

            S.barrier()
            R3.reset(mark)
            NP = 3
            Pt = [R3.bf16(1024) for _ in range(NP)]
            B_P = [Buf() for _ in range(NP)]
            rs = [R3.f32(512) for _ in range(2)]
            tq = [R3.f32(512) for _ in range(2)]
            o_sb = R3.f32(512)
            sq_sb = R3.f32(512)
            r2_sb = R3.f32(512)
            B_fin = [Buf() for _ in range(8)]
            if pr == 1:
                TOP = R3.nbytes - 32 * 1024
                assert R3.off <= TOP, R3.off
                R3.reset(TOP)
                wO = R3.bf16(8 * 1024).rearrange("p (c n) -> p c n", c=8)
                wQ = R3.bf16(8 * 1024).rearrange("p (c n) -> p c n", c=8)
                B_wO = Buf()
                B_wQ = Buf()
                DMA("pool", wO, w_out.rearrange("(c p) n -> p c n", p=128), [], [B_wO])
                DMA("pool", wQ, wq.rearrange("(c p) n -> p c n", p=128), [], [B_wQ])
            flat = []
            for s in range(4):
                for hc in range(2):
                    nkb_ = 4 * (4 * s + 3 + 1)
                    for kb in range(nkb_):
                        flat.append((s, hc, kb, nkb_))
            n = len(flat)
            sbank = {}
            pbuf = {}
            rot = {"s": 0, "p": 0}

            def geom(t):
                s, hc, kb, nkb = flat[t]
                r = kb - 4 * (4 * s + 3)
                return s, hc, kb, nkb, r, 128 * max(r, 0)

            def QK(t):
                s, hc, kb, nkb, r, c0 = geom(t)
                b0 = 2 * (rot["s"] % 2)
                rot["s"] += 1
                sbank[t] = b0
                for m in range(2):
                    MM(ps[b0 + m][:, c0:512], KT[:, hc, kb * 128:(kb + 1) * 128],
                       QT[:, hc, m, s * 512 + c0:(s + 1) * 512], True, True,
                       [B_QT[hc][s]], [Bp[b0 + m]])

            def SOFT(t):
                s, hc, kb, nkb, r, c0 = geom(t)
                h = 2 * pr + hc
                b0 = sbank[t]
                for m in range(2):
                    b = b0 + m
                    if r >= 0:
                        TT("dve", ps[b][:, 128 * r:128 * r + 128], ps[b][:, 128 * r:128 * r + 128], biasT[:, h, 0, :], ALU.add,
                           [Bp[b], B_bias], [Bp[b]])
                    if r >= -1 and r + 1 <= 3:
                        cc = 128 * (r + 1)
                        TT("dve", ps[b][:, cc:cc + 128], ps[b][:, cc:cc + 128], biasT[:, h, 1, :], ALU.add,
                           [Bp[b], B_bias], [Bp[b]])
                pb = rot["p"] % NP
                rot["p"] += 1
                pbuf[t] = pb
                kw = {}
                if kb // 4 <= 2:
                    kw["bias"] = kvb[:, kb // 4:kb // 4 + 1]
                src = ps_all[:, b0 * 512:(b0 + 2) * 512].rearrange("p (b n) -> p b n", b=2)[:, :, c0:512]
                dst = Pt[pb].rearrange("p (b n) -> p b n", b=2)[:, :, c0:512]
                ACT(dst, src, AF.Exp, [Bp[b0], Bp[b0 + 1], Bc], [B_P[pb]], **kw)

            def PV(t):
                s, hc, kb, nkb, r, c0 = geom(t)
                pb = pbuf[t]
                first = (kb == 0)
                last = (kb == nkb - 1)
                for m in range(2):
                    Pm = Pt[pb][:, m * 512 + c0:(m + 1) * 512]
                    MM(ps[4 + m][:, c0:512], Vv[:, kb, hc * 128:(hc + 1) * 128], Pm, first, last,
                       [B_P[pb]], [Bp[4 + m]])
                    MM(ps[6 + m][:, c0:512], ones_bf, Pm, first, last, [B_P[pb], Bc], [Bp[6 + m]])

            def FIN_a(s, hc):
                RECIP(rs[0], ps[6][:, :], [Bp[6]], [B_fin[0]])
                TT("dve", tq[0], ps[4][:, :], rs[0], ALU.mult, [Bp[4], B_fin[0]], [B_fin[2]])
                RECIP(rs[1], ps[7][:, :], [Bp[7]], [B_fin[1]])
                TT("dve", tq[1], ps[5][:, :], rs[1], ALU.mult, [Bp[5], B_fin[1]], [B_fin[3]])
                STT(o_sb, tq[1], neg_lam, tq[0], ALU.mult, ALU.add, [B_fin[2], B_fin[3], Bc], [B_fin[4]])
                ACT(sq_sb, o_sb, AF.Square, [B_fin[4]], [B_fin[5]])

            def FIN_b(s, hc, bm):
                h = 2 * pr + hc
                MM(ps[bm][:, :], ones_f, sq_sb, True, True, [B_fin[5], Bc], [Bp[bm]])
                ACT(r2_sb, ps[bm][:, :], AF.Ln, [Bp[bm]], [B_fin[6]], bias=1e-6)
                ACT(r2_sb, r2_sb, AF.Exp, [B_fin[6]], [B_fin[6]], scale=-0.5)
                STT(mixT[:, h, s * 512:(s + 1) * 512], o_sb, subcol, r2_sb, ALU.mult, ALU.mult,
                    [B_fin[4], B_fin[6], Bc], [B_mix[h][s]])

            QK(0)
            QK(1)
            pend_fin = None
            for t in range(n):
                SOFT(t)
                if pend_fin is not None and (t >= pend_fin[2] or t == n - 1):
                    FIN_b(pend_fin[0], pend_fin[1], sbank[t])
                    pend_fin = None
                if t + 2 < n:
                    QK(t + 2)
                PV(t)
                s_, hc_, kb_l, nkb_l = flat[t]
                if kb_l == nkb_l - 1:
                    FIN_a(s_, hc_)
                    pend_fin = (s_, hc_, t + 4)
            if pend_fin is not None:
                FIN_b(pend_fin[0], pend_fin[1], 0)

        S.barrier()
        R3.reset()
        if debug:
            DMA("sp", dbg["dbg_mix"], R1.t16[:, 0:16384], [b for row in B_mix for b in row], [])
        wKV = R3.bf16(8 * 2048).rearrange("p (c n) -> p c n", c=8)
        mark_b = R3.off
        B_wKV = Buf()
        DMA("pool", wKV[:, :, 0:1024], wkv[:, 0:1024].rearrange("(c p) n -> p c n", p=128), [], [B_wKV])
        DMA("pool", wKV[:, :, 1024:2048], wkv[:, 1024:2048].rearrange("(c p) n -> p c n", p=128), [], [B_wKV])
        xo = [R3.f32(1024) for _ in range(2)]
        assert R3.off <= TOP
        B_xo = [Buf() for _ in range(2)]
        B_h = [Buf("h%d" % t) for t in range(16)]
        allmix = [b for row in B_mix for b in row]
        for tt in range(16):
            s_, t4 = tt // 4, tt % 4
            row0 = (4 * s_ + 3) * 512 + t4 * 128
            DMA("sp", xo[tt % 2], x[row0:row0 + 128, :], [], [B_xo[tt % 2]])
            for half in range(2):
                b = 2 * (tt % 2) + half
                for c in range(8):
                    MM(ps[b][:, :], mixT[:, c, tt * 128:(tt + 1) * 128], wO[:, c, half * 512:(half + 1) * 512], c == 0, c == 7,
                       [B_mix[c][s_], B_wO], [Bp[b]])
                TT("dve", hres[:, tt, half * 512:(half + 1) * 512], ps[b][:, :], xo[tt % 2][:, half * 512:(half + 1) * 512], ALU.add,
                   [Bp[b], B_xo[tt % 2]], [B_h[tt]])
            brs_c[tt] = rms_cols(hres[:, tt, :], B_h[tt], tt)

        def dump_h(name):
            if debug:
                for tt in range(16):
                    DMA("sp", dbg[name][tt * 128:(tt + 1) * 128, :], hres[:, tt, :], [B_h[tt]], [])

        dump_h("dbg_h1")

        S.barrier()
        R3.reset(mark_b)
        xs_t = [R3.bf16(1024) for _ in range(2)]
        B_xs = [Buf() for _ in range(2)]
        mnT = R3.bf16(8 * 256).rearrange("p (c n) -> p c n", c=8)
        B_mnT = [Buf() for _ in range(2)]
        KcT = R3.bf16(8 * 256).rearrange("p (c n) -> p c n", c=8)
        B_Kc = Buf()
        Vc = R3.bf16(2 * 1024).rearrange("p (k n) -> p k n", k=2)
        B_Vc = Buf()
        mark_c = R3.off
        mst = [R3.f32(1024) for _ in range(2)]
        B_mst = [Buf() for _ in range(2)]

        for mk in range(2):
            DMA("sp", mst[mk], mem[mk * 128:(mk + 1) * 128, :], [], [B_mst[mk]])
            brs = rms_cols(mst[mk], B_mst[mk], 32 + mk)
            norm_transpose(mst[mk], B_mst[mk], rstd_all[:, 32 + mk:33 + mk], 16, mnT[:, :, mk * 128:(mk + 1) * 128], B_mnT[mk],
                           xs_t[mk], B_xs[mk], mk, brs)
        for j8 in range(8):
            b = 4 + j8 % 4
            for c in range(8):
                MM(ps[b][:, 0:256], wKV[:, c, j8 * 128:(j8 + 1) * 128], mnT[:, c, :], c == 0, c == 7, B_mnT + [B_wKV], [Bp[b]])
            CP("dve" if j8 % 2 else "act", KcT[:, j8, :], ps[b][:, 0:256], [Bp[b]], [B_Kc])
        for mk in range(2):
            for half in range(2):
                b = 4 + (2 * mk + half) % 4
                for c in range(8):
                    MM(ps[b][:, :], mnT[:, c, mk * 128:(mk + 1) * 128], wKV[:, c, 1024 + half * 512:1024 + (half + 1) * 512], c == 0, c == 7,
                       B_mnT + [B_wKV], [Bp[b]])
                CP("dve" if half else "act", Vc[:, mk, half * 512:(half + 1) * 512], ps[b][:, :], [Bp[b]], [B_Vc])
        B_hT = [[Buf() for _ in range(4)] for _ in range(4)]
        for tt in range(16):
            norm_transpose(hres[:, tt, :], B_h[tt], rstd_all[:, tt:tt + 1], 8, hT[:, :, tt * 128:(tt + 1) * 128], B_hT[tt // 4][tt % 4],
                           xs_t[tt % 2], B_xs[tt % 2], tt % 2, brs_c[tt])
        S.barrier()
        wOc = wKV[:, :, 0:1024]
        B_wOc = Buf()
        DMA("pool", wOc, wo.rearrange("(c p) n -> p c n", p=128), [], [B_wOc])
        R3.reset(mark_c)
        qT = R3.bf16(8 * 512).rearrange("p (c n) -> p c n", c=8)
        B_qT = [Buf() for _ in range(8)]
        ocT = R3.bf16(8 * 512).rearrange("p (c n) -> p c n", c=8)
        B_oc = [Buf() for _ in range(8)]
        Pc = [R3.bf16(512) for _ in range(4)]
        B_Pc = [Buf() for _ in range(4)]
        rsc2 = [R3.f32(512) for _ in range(2)]
        B_rsc2 = [Buf() for _ in range(2)]
        pc_rot = 0
        for s in range(4):
            for j8 in range(8):
                b = j8 % 2
                for c in range(8):
                    MM(ps[b][:, :], wQ[:, c, j8 * 128:(j8 + 1) * 128], hT[:, c, s * 512:(s + 1) * 512], c == 0, c == 7,
                       B_hT[s] + [B_wQ], [Bp[b]])
                if j8 % 2:
                    TS("dve", qT[:, j8, :], ps[b][:, :], 1.0 / 16.0, None, ALU.mult, None, [Bp[b]], [B_qT[j8]])
                else:
                    ACT(qT[:, j8, :], ps[b][:, :], AF.Copy, [Bp[b]], [B_qT[j8]], scale=1.0 / 16.0)
            pcs_h = {}

            def c_scores(hh):
                nonlocal pc_rot
                pcs = []
                for mk in range(2):
                    b = 2 + mk
                    for e2 in range(2):
                        MM(ps[b][:, :], KcT[:, 2 * hh + e2, mk * 128:(mk + 1) * 128], qT[:, 2 * hh + e2, :], e2 == 0, e2 == 1,
                           [B_Kc, B_qT[2 * hh + e2]], [Bp[b]])
                    pi = pc_rot % 4
                    pc_rot += 1
                    pcs.append(pi)
                    ACT(Pc[pi], ps[b][:, :], AF.Exp, [Bp[b]], [B_Pc[pi]])
                pcs_h[hh] = pcs

            def c_pv(hh):
                pcs = pcs_h[hh]
                st_ = hh % 2
                ob = (4, 5) if st_ == 0 else (0, 1)
                sb_ = 6 if st_ == 0 else 7
                for e2 in range(2):
                    b = ob[e2]
                    for mk in range(2):
                        MM(ps[b][:, :], Vc[:, mk, (2 * hh + e2) * 128:(2 * hh + e2 + 1) * 128], Pc[pcs[mk]], mk == 0, mk == 1,
                           [B_Vc, B_Pc[pcs[mk]]], [Bp[b]])
                for mk in range(2):
                    MM(ps[sb_][:, :], ones_bf, Pc[pcs[mk]], mk == 0, mk == 1, [Bc, B_Pc[pcs[mk]]], [Bp[sb_]])
                RECIP(rsc2[st_], ps[sb_][:, :], [Bp[sb_]], [B_rsc2[st_]])
                for e2 in range(2):
                    TT("dve", ocT[:, 2 * hh + e2, :], ps[ob[e2]][:, :], rsc2[st_], ALU.mult, [Bp[ob[e2]], B_rsc2[st_]], [B_oc[2 * hh + e2]])

            c_scores(0)
            for hh in range(4):
                if hh + 1 < 4:
                    c_scores(hh + 1)
                c_pv(hh)
            for t4 in range(4):
                tt = 4 * s + t4
                for half in range(2):
                    b = half
                    for c in range(8):
                        MM(ps[b][:, :], ocT[:, c, t4 * 128:(t4 + 1) * 128], wOc[:, c, half * 512:(half + 1) * 512], c == 0, c == 7,
                           [B_oc[c], B_wOc], [Bp[b]])
                    TT("dve", hres[:, tt, half * 512:(half + 1) * 512], ps[b][:, :], hres[:, tt, half * 512:(half + 1) * 512], ALU.add,
                       [Bp[b], B_h[tt]], [B_h[tt]])
                brs_d[tt] = rms_cols(hres[:, tt, :], B_h[tt], 16 + tt)
        dump_h("dbg_h2")

        S.barrier()
        R3.reset()
        NU = 2
        ring = []
        for k in range(NU):
            unit = []
            for _e in range(2):
                wg_ = R3.bf16(8 * 256).rearrange("p (c n) -> p c n", c=8)
                wu_ = R3.bf16(8 * 256).rearrange("p (c n) -> p c n", c=8)
                wd_ = R3.bf16(2 * 1024).rearrange("p (f n) -> p f n", f=2)
                unit.append((wg_, wu_, wd_, Buf(), Buf(), Buf()))
            ring.append(unit)

        def load_unit(u):
            for e2 in range(2):
                e = 2 * u + e2
                wg_, wu_, wd_, b1, b2, b3 = ring[u % NU][e2]
                DMA("pool", wg_, w_gate[e].rearrange("(c p) n -> p c n", p=128), [], [b1])
                DMA("pool", wu_, w_up[e].rearrange("(c p) n -> p c n", p=128), [], [b2])
                DMA("pool", wd_, w_down[e].rearrange("(f p) n -> p f n", p=128), [], [b3])

        wR = R3.bf16(8 * 20).rearrange("p (c n) -> p c n", c=8)
        B_wR = Buf()
        DMA("pool", wR, wr.rearrange("(c p) n -> p c n", p=128), [], [B_wR])
        for u in range(NU):
            load_unit(u)
        selT = R3.bf16(2048)
        B_sel = Buf()
        DMA("pool", selT[0:16, :], sel_d, [], [B_sel])
        chi = R3.bf16(2048)
        clo = R3.bf16(2048)
        combT = R3.f32(2048)
        B_comb = [Buf() for _ in range(16)]
        rt_n = 16 * (20 + 16 + 4 * 8 + 16 + 9)
        rt = R3.f32(rt_n)
        B_rt = Buf()
        B_lg = [Buf() for _ in range(16)]
        _o = [0]

        def rtv(n_inner):
            o = _o[0]
            _o[0] += 16 * n_inner
            v = rt[:, o:o + 16 * n_inner]
            return v if n_inner == 1 else v.rearrange("p (t k) -> p t k", t=16)

        lg = rtv(20); comb = rtv(16); goh = rtv(4); gex = rtv(4); esel = rtv(4); oh1 = rtv(4); es2 = rtv(4); oh2 = rtv(4)
        inner = rtv(4); gsc = rtv(4); prod16 = rtv(16)
        gmax = rtv(1); gsum = rtv(1); gw = rtv(1); m1 = rtv(1); m2 = rtv(1); e21 = rtv(1); den = rtv(1); w1 = rtv(1); w2 = rtv(1)
        prod4 = prod16.rearrange("p t (g e) -> p t g e", g=4)
        comb4 = comb.rearrange("p t (g e) -> p t g e", g=4)

        def bcl(v, n):
            return bass.AP(v.tensor, v.offset, [list(d) for d in v.ap] + [[0, n]])

        def bcm(v, n):
            dd = [list(d) for d in v.ap]
            return bass.AP(v.tensor, v.offset, dd[:-1] + [[0, n]] + dd[-1:])

        actT = R3.bf16(2 * 2 * 512).rearrange("p (e f n) -> p e f n", e=2, f=2)
        B_act = [[Buf() for _ in range(2)] for _ in range(2)]
        sg = [R3.f32(512) for _ in range(2)]
        B_sg = [Buf() for _ in range(2)]
        tg = [R3.f32(512) for _ in range(2)]
        B_tg = [Buf() for _ in range(2)]
        bcs = [R3.f32(512) for _ in range(2)]
        B_bcs = [Buf() for _ in range(2)]
        xs_t = [tg[0].bitcast(BF16), tg[1].bitcast(BF16)]
        B_xs = [Buf() for _ in range(2)]
        B_h3T = [[Buf() for _ in range(4)] for _ in range(4)]
        for tt in range(16):
            norm_transpose(hres[:, tt, :], B_h[tt], rstd_all[:, 16 + tt:17 + tt], 24, hT[:, :, tt * 128:(tt + 1) * 128], B_h3T[tt // 4][tt % 4],
                           xs_t[tt % 2], B_xs[tt % 2], tt % 2, brs_d[tt])
            for c in range(8):
                MM(ps[2 + tt % 2][:, 0:20], hT[:, c, tt * 128:(tt + 1) * 128], wR[:, c, :], c == 0, c == 7,
                   [B_h3T[tt // 4][tt % 4], B_wR], [Bp[2 + tt % 2]])
            TT("dve", lg[:, tt, :], ps[2 + tt % 2][:, 0:20], rbias_bc, ALU.add, [Bp[2 + tt % 2], Bc], [B_lg[tt]])

        R_ = [B_rt]
        lgg = lg[:, :, 0:4]
        le4 = lg[:, :, 4:20].rearrange("p t (g e) -> p t g e", g=4)
        S.add("dve", lambda e: e.tensor_reduce(gmax, lgg, axis=AX.X, op=ALU.max), B_lg, R_)
        TT("dve", goh, lgg, bcl(gmax, 4), ALU.is_equal, B_lg + R_, R_)
        TT("dve", gex, lgg, bcl(gmax, 4), ALU.subtract, B_lg + R_, R_)
        ACT(gex, gex, AF.Exp, R_, R_)
        S.add("dve", lambda e: e.tensor_reduce(gsum, gex, axis=AX.X, op=ALU.add), R_, R_)
        RECIP(gw, gsum, R_, R_)
        TT("dve", prod4, le4, bcl(goh, 4), ALU.mult, B_lg + R_, R_)
        S.add("dve", lambda e: e.tensor_reduce(esel, prod4.rearrange("p t g e -> p t e g"), axis=AX.X, op=ALU.add), R_, R_)
        S.add("dve", lambda e: e.tensor_reduce(m1, esel, axis=AX.X, op=ALU.max), R_, R_)
        TT("dve", oh1, esel, bcl(m1, 4), ALU.is_equal, R_, R_)
        STT(es2, oh1, -1e30, esel, ALU.mult, ALU.add, R_, R_)
        S.add("dve", lambda e: e.tensor_reduce(m2, es2, axis=AX.X, op=ALU.max), R_, R_)
        TT("dve", oh2, es2, bcl(m2, 4), ALU.is_equal, R_, R_)
        TT("dve", e21, m2, m1, ALU.subtract, R_, R_)
        ACT(e21, e21, AF.Exp, R_, R_)
        TS("dve", den, e21, 1.0, None, ALU.add, None, R_, R_)
        RECIP(w1, den, R_, R_)
        TT("dve", w2, e21, w1, ALU.mult, R_, R_)
        TT("dve", inner, oh1, bcl(w1, 4), ALU.mult, R_, R_)
        TT("dve", oh2, oh2, bcl(w2, 4), ALU.mult, R_, R_)
        TT("dve", inner, inner, oh2, ALU.add, R_, R_)
        TT("dve", gsc, goh, bcl(gw, 4), ALU.mult, R_, R_)
        TT("dve", comb4, bcl(gsc, 4), bcm(inner, 4), ALU.mult, R_, R_)
        for q4 in range(4):
            pb_ = 2 + q4 % 2
            for t4 in range(4):
                tt = 4 * q4 + t4
                S.add("pe", lambda e, tt=tt, t4=t4, pb_=pb_: e.transpose(ps[pb_][0:16, t4 * 128:(t4 + 1) * 128], comb[:, tt, :], identf),
                      [B_rt, Bc], [Bp[pb_]])
            CP("dve", combT[0:16, q4 * 512:(q4 + 1) * 512], ps[pb_][0:16, :], [Bp[pb_]], B_comb[4 * q4:4 * q4 + 4])
            CP("dve", chi[0:16, q4 * 512:(q4 + 1) * 512], combT[0:16, q4 * 512:(q4 + 1) * 512], B_comb[4 * q4:4 * q4 + 4], B_comb[4 * q4:4 * q4 + 4])
            TT("dve", clo[0:16, q4 * 512:(q4 + 1) * 512], combT[0:16, q4 * 512:(q4 + 1) * 512], chi[0:16, q4 * 512:(q4 + 1) * 512], ALU.subtract,
               B_comb[4 * q4:4 * q4 + 4], B_comb[4 * q4:4 * q4 + 4])

        d_rot = [0]

        def gu_mm(u, s, e2, f):
            wg_, wu_, wd_, b1, b2, b3 = ring[u % NU][e2]
            bg = 2 * f
            bu = 2 * f + 1
            for c in range(8):
                MM(ps[bg][:, :], wg_[:, c, f * 128:(f + 1) * 128], hT[:, c, s * 512:(s + 1) * 512], c == 0, c == 7,
                   B_h3T[s] + [b1], [Bp[bg]])
            for c in range(8):
                MM(ps[bu][:, :], wu_[:, c, f * 128:(f + 1) * 128], hT[:, c, s * 512:(s + 1) * 512], c == 0, c == 7,
                   B_h3T[s] + [b2], [Bp[bu]])

        def gu_post(u, s, e2, f):
            e = 2 * u + e2
            bi = e2
            bg = 2 * f
            bu = 2 * f + 1
            if f == 0:
                MM(ps[6][:, :], selT[0:16, e * 128:(e + 1) * 128], chi[0:16, s * 512:(s + 1) * 512], True, False,
                   [B_sel] + B_comb[4 * s:4 * s + 4], [Bp[6]])
                MM(ps[6][:, :], selT[0:16, e * 128:(e + 1) * 128], clo[0:16, s * 512:(s + 1) * 512], False, True,
                   [B_sel] + B_comb[4 * s:4 * s + 4], [Bp[6]])
                CP("act", bcs[bi], ps[6][:, :], [Bp[6]], [B_bcs[bi]])
            ACT(sg[f], ps[bg][:, :], AF.Silu, [Bp[bg]], [B_sg[f]])
            TT("dve", tg[f], ps[bu][:, :], sg[f], ALU.mult, [Bp[bu], B_sg[f]], [B_tg[f]])
            TT("pool", actT[:, e2, f, :], tg[f], bcs[bi], ALU.mult, [B_tg[f], B_bcs[bi]], [B_act[e2][f]])

        def down(u, s):
            unit = ring[u % NU]
            for t4 in range(4):
                tt = 4 * s + t4
                for half in range(2):
                    b = 4 + d_rot[0] % 2
                    d_rot[0] += 1
                    k = 0
                    for e2 in range(2):
                        wd_, b3 = unit[e2][2], unit[e2][5]
                        for f in range(2):
                            MM(ps[b][:, :], actT[:, e2, f, t4 * 128:(t4 + 1) * 128], wd_[:, f, half * 512:(half + 1) * 512],
                               k == 0, k == 3, [B_act[e2][f], b3], [Bp[b]])
                            k += 1
                    TT("dve", hres[:, tt, half * 512:(half + 1) * 512], ps[b][:, :], hres[:, tt, half * 512:(half + 1) * 512], ALU.add,
                       [Bp[b], B_h[tt]], [B_h[tt]])
                if u == 7:
                    brs_f[tt] = rms_cols(hres[:, tt, :], B_h[tt], 34 + tt)

        steps = [(u, s) for u in range(8) for s in range(4)]
        gu_mm(0, 0, 0, 0)
        for k_, (u, s) in enumerate(steps):
            gu_post(u, s, 0, 0)
            gu_mm(u, s, 0, 1)
            gu_post(u, s, 0, 1)
            gu_mm(u, s, 1, 0)
            gu_post(u, s, 1, 0)
            gu_mm(u, s, 1, 1)
            gu_post(u, s, 1, 1)
            if k_ + 1 < len(steps):
                un, sn = steps[k_ + 1]
                gu_mm(un, sn, 0, 0)
            down(u, s)
            if s == 3 and u + NU < 8:
                load_unit(u + NU)
        dump_h("dbg_h3")

        S.barrier()
        R3.reset()
        yo = [R3.f32(1024) for _ in range(2)]
        B_yo = [Buf() for _ in range(2)]
        for tt in range(16):
            STT(yo[tt % 2], hres[:, tt, :], rstd_all[:, 34 + tt:35 + tt], fnorm_bc, ALU.mult, ALU.mult, [B_h[tt], brs_f[tt], Bc], [B_yo[tt % 2]])
            DMA("sp", y[tt * 128:(tt + 1) * 128, :], yo[tt % 2], [B_yo[tt % 2]], [])

        S.emit(nc, st)
    return nc


def _rel_bucket(rel):
    nb = 16
    max_exact = 8
    ret = (rel > 0).astype(np.int32) * nb
    n = np.abs(rel)
    nf = np.maximum(n, 1).astype(np.float32)
    large = max_exact + (np.log(nf / np.float32(max_exact)) / np.float32(math.log(128 / max_exact))
                         * np.float32(nb - max_exact)).astype(np.int32)
    large = np.minimum(large, nb - 1)
    return ret + np.where(n < max_exact, n, large)


_NC_CACHE = {}


def kernel(**inp):
    debug = int(inp.pop("_debug", 0)) if "_debug" in inp else 0
    f = lambda k: np.ascontiguousarray(np.asarray(inp[k], dtype=np.float32))
    x = f("x")
    mem = f("mem")
    i = np.arange(384)
    rel = 127 - i
    bk = _rel_bucket(rel.astype(np.int32))
    oh = np.zeros((32, 384), np.float32)
    oh[bk, i] = 1.0
    oh[15, :] -= 1.0
    kk = np.arange(128)[:, None]
    qq = np.arange(128)[None, :]
    maskT = np.where((kk < 64) | (qq >= 64), 0.0, NEG).astype(np.float32)
    ident = np.eye(128, dtype=np.float32)
    sel = np.zeros((16, 16, 128), np.float32)
    for e in range(16):
        sel[e, e, :] = 1.0
    sel = sel.reshape(16, 2048)
    gains = np.zeros((128, 40), np.float32)
    gains[:, 0:8] = f("attn_norm")[0].reshape(8, 128).T
    gains[:, 8:16] = f("cross_norm")[0].reshape(8, 128).T
    gains[:, 16:24] = f("mem_norm")[0].reshape(8, 128).T
    gains[:, 24:32] = f("ffn_norm")[0].reshape(8, 128).T
    gains[:, 32:36] = f("pool_scale")[0].reshape(4, 128).T
    gains[:, 36] = f("diff_subln")[0]
    wr = np.concatenate([f("router_group")[0], f("router_expert")[0].transpose(1, 0, 2).reshape(1024, 16)], axis=1)
    rbias = np.concatenate([f("router_group_bias")[0], f("router_expert_bias")[0].reshape(16)])[None, :]
    lamv = np.concatenate([f("lambda_q1")[0], f("lambda_k1")[0], f("lambda_q2")[0], f("lambda_k2")[0]])[None, :]
    shared = {
        "w_in": f("w_in")[0], "w_out": f("w_out")[0], "wq": f("wq_cross")[0], "wkv": f("wkv_cross")[0], "wo": f("wo_cross")[0],
        "w_gate": f("w_gate")[0], "w_up": f("w_up")[0], "w_down": f("w_down")[0], "pool_w": f("pool_w")[0],
        "wr": np.ascontiguousarray(wr), "rbias": np.ascontiguousarray(rbias), "rel_bias": f("rel_bias"),
        "lamv": np.ascontiguousarray(lamv), "gains": gains, "fnorm": f("final_norm")[None, :],
        "ident": ident, "oh": oh, "maskT": maskT, "sel": sel,
    }
    in_maps = []
    for c in range(8):
        b, j = c // 4, c % 4
        pad = 3 - j
        xs = np.zeros((8192, 1024), np.float32)
        xs[pad * 512:] = x[b, :(16 - pad) * 512]
        kvb = np.zeros((128, 16), np.float32)
        kvb[:, :pad] = NEG
        pinv = np.zeros((128, 4, 16), np.float32)
        for g in range(4):
            w = 2 ** (g + 1)
            if j == 0:
                pinv[:, g, :] = 1.0 / np.minimum(np.arange(1, 17), w)
            else:
                pinv[:, g, :] = 1.0 / w
        m = dict(shared)
        m.update({"x": xs, "mem": mem[b], "kvb": kvb, "pinv": pinv.reshape(128, 64)})
        in_maps.append(m)
    key = debug
    if key not in _NC_CACHE:
        _NC_CACHE[key] = build_nc(debug)
    nc = _NC_CACHE[key]
    res = run_bass_kernel_spmd(nc, in_maps, core_ids=list(range(8)))
    out = np.zeros((2, 8192, 1024), np.float32)
    extra = {}
    for c in range(8):
        b, j = c // 4, c % 4
        r = res.results[c]
        for s in range(4):
            t = 4 * s + j
            out[b, t * 512:(t + 1) * 512] = r["y"][s * 512:(s + 1) * 512]
        if debug:
            extra[c] = {k: v for k, v in r.items() if k.startswith("dbg")}
    if debug:
        return out, extra
    return out
```

```python
import math
import numpy as np
from contextlib import ExitStack
import concourse.bass as bass
import concourse.mybir as mybir
from concourse.bass_utils import run_bass_kernel_spmd

F32 = mybir.dt.float32
BF16 = mybir.dt.bfloat16
AF = mybir.ActivationFunctionType
ALU = mybir.AluOpType
AX = mybir.AxisListType

COMPUTE = ("pe", "act", "dve", "pool")
ENGS = ("pe", "act", "dve", "pool", "sp")
NEG = -30000.0


class Buf:
    __slots__ = ("name", "writer", "rd_eng", "rd_dma")

    def __init__(self, name=""):
        self.name = name
        self.writer = None
        self.rd_eng = {}
        self.rd_dma = []


class Op:
    __slots__ = ("eng", "idx", "fn", "waits", "signal", "num", "dma", "dma_i", "clock", "slotwait")


class Sched:
    def __init__(self, K=8):
        self.ops = {e: [] for e in ENGS}
        self.known = {e: {c: -1 for c in COMPUTE} for e in ENGS}
        self.dma_known = {e: set() for e in ENGS}
        self.ndma = {e: 0 for e in ENGS}
        self.dma_ops = {e: [] for e in ENGS}
        self.bar = {e: [] for e in ENGS}
        self.K = K

    def barrier(self):
        lasts = []
        for e in COMPUTE:
            for op in reversed(self.ops[e]):
                if not op.dma:
                    lasts.append(op)
                    break
        for e in ENGS:
            self.bar[e] = list(lasts)

    def add(self, eng, fn, reads=(), writes=(), dma=False):
        op = Op()
        op.eng = eng
        op.idx = len(self.ops[eng])
        op.fn = fn
        op.dma = dma
        op.signal = False
        op.num = None
        op.dma_i = None
        op.slotwait = None
        deps = []
        if self.bar[eng]:
            deps.extend(d for d in self.bar[eng] if not (d.eng == eng and eng == "pe"))
            self.bar[eng] = []
        for b in reads:
            if b.writer is not None:
                deps.append(b.writer)
        for b in writes:
            if b.writer is not None:
                deps.append(b.writer)
            deps.extend(b.rd_eng.values())
            deps.extend(b.rd_dma)
        known = self.known[eng]
        best = {}
        dwaits = []
        for d in deps:
            if d.dma:
                if id(d) not in self.dma_known[eng]:
                    self.dma_known[eng].add(id(d))
                    dwaits.append(d)
            else:
                if d.eng == eng and eng == "pe":
                    continue
                if known[d.eng] >= d.idx:
                    continue
                if d.eng not in best or best[d.eng].idx < d.idx:
                    best[d.eng] = d
        waits = list(best.values()) + dwaits
        for d in waits:
            d.signal = True
            for c in COMPUTE:
                if d.clock[c] > known[c]:
                    known[c] = d.clock[c]
            if not d.dma and d.idx > known[d.eng]:
                known[d.eng] = d.idx
        if dma:
            i = self.ndma[eng]
            op.dma_i = i
            self.ndma[eng] = i + 1
            if i >= self.K:
                prev = self.dma_ops[eng][i - self.K]
                op.slotwait = prev
                self.dma_known[eng].add(id(prev))
            self.dma_ops[eng].append(op)
        op.waits = waits
        op.clock = dict(known)
        if not dma and eng in COMPUTE:
            op.clock[eng] = op.idx
        for b in reads:
            if dma:
                b.rd_dma.append(op)
            else:
                b.rd_eng[eng] = op
        for b in writes:
            b.writer = op
            b.rd_eng = {}
            b.rd_dma = []
        self.ops[eng].append(op)
        return op

    def emit(self, nc, stack):
        sem_eng = {e: stack.enter_context(nc.semaphore("s_" + e)) for e in COMPUTE}
        sem_dma = {e: [stack.enter_context(nc.semaphore("d_%s%d" % (e, k))) for k in range(self.K)]
                   for e in ENGS if self.ndma[e] > 0}
        for e in COMPUTE:
            n = 0
            for op in self.ops[e]:
                if op.signal and not op.dma:
                    n += 1
                    op.num = n
        K = self.K

        def dma_target(d):
            return sem_dma[d.eng][d.dma_i % K], 16 * (d.dma_i // K + 1)

        def run(ename, e):
            for op in self.ops[ename]:
                if op.slotwait is not None:
                    s, v = dma_target(op.slotwait)
                    e.wait_ge(s, v)
                for d in op.waits:
                    if d.dma:
                        s, v = dma_target(d)
                        e.wait_ge(s, v)
                    else:
                        e.wait_ge(sem_eng[d.eng], d.num)
                ins = op.fn(e)
                if op.dma:
                    s, v = dma_target(op)
                    ins.then_inc(s, 16)
                elif op.signal:
                    ins.then_inc(sem_eng[ename], 1)
            for d in self.dma_ops[ename][-K:]:
                s, v = dma_target(d)
                e.wait_ge(s, v)

        block = stack.enter_context(nc.Block())

        @block.tensor
        def _(e):
            run("pe", e)

        @block.scalar
        def _(e):
            run("act", e)

        @block.vector
        def _(e):
            run("dve", e)

        @block.gpsimd
        def _(e):
            run("pool", e)

        @block.sync
        def _(e):
            run("sp", e)


class Arena:
    def __init__(self, nc, st, name, nbytes):
        self.t32 = st.enter_context(nc.sbuf_tensor(name, [128, nbytes // 4], F32))
        self.t16 = self.t32.bitcast(BF16)
        self.nbytes = nbytes
        self.off = 0

    def reset(self, off=0):
        self.off = off

    def f32(self, n):
        o = self.off
        self.off += 4 * n
        assert self.off <= self.nbytes, (self.off, self.nbytes)
        return self.t32[:, o // 4:o // 4 + n]

    def bf16(self, n):
        o = self.off
        self.off += 2 * n
        self.off = (self.off + 3) // 4 * 4
        assert self.off <= self.nbytes, (self.off, self.nbytes)
        return self.t16[:, o // 2:o // 2 + n]


def build_nc(debug=0):
    nc = bass.Bass("TRN2", target_bir_lowering=False)

    def din(name, shape):
        return nc.dram_tensor(name, shape, F32, kind="ExternalInput")

    x_t = din("x", [8192, 1024]); x = x_t.ap()
    mem = din("mem", [256, 1024]).ap()
    w_in = din("w_in", [1024, 2048]).ap()
    w_out = din("w_out", [1024, 1024]).ap()
    wq = din("wq", [1024, 1024]).ap()
    wkv = din("wkv", [1024, 2048]).ap()
    wo = din("wo", [1024, 1024]).ap()
    w_gate = din("w_gate", [16, 1024, 256]).ap()
    w_up = din("w_up", [16, 1024, 256]).ap()
    w_down = din("w_down", [16, 256, 1024]).ap()
    pool_w = din("pool_w", [4, 128, 128]).ap()
    wr = din("wr", [1024, 20]).ap()
    rbias_t = din("rbias", [1, 20])
    rel_bias = din("rel_bias", [32, 4]).ap()
    lamv_t = din("lamv", [1, 256])
    gains_d = din("gains", [128, 40]).ap()
    fnorm_t = din("fnorm", [1, 1024])
    ident_d = din("ident", [128, 128]).ap()
    oh_d = din("oh", [32, 384]).ap()
    maskT_d = din("maskT", [128, 128]).ap()
    kvb_d = din("kvb", [128, 16]).ap()
    pinv_d = din("pinv", [128, 64]).ap()
    sel_d = din("sel", [16, 2048]).ap()
    y = nc.dram_tensor("y", [2048, 1024], F32, kind="ExternalOutput").ap()
    E_t = nc.dram_tensor("Escr", [4, 128, 384], F32, kind="Internal")
    dbg = {}
    if debug:
        for nm in ("dbg_h1", "dbg_h2", "dbg_h3"):
            dbg[nm] = nc.dram_tensor(nm, [2048, 1024], F32, kind="ExternalOutput").ap()
        dbg["dbg_mix"] = nc.dram_tensor("dbg_mix", [128, 8 * 2048], BF16, kind="ExternalOutput").ap()

    S = Sched()
    st = ExitStack()
    with st:
        G = Arena(nc, st, "G", 18 * 1024)
        ident = G.bf16(128)
        ones_bf = G.bf16(128)
        ones_f = G.f32(128)
        gains = G.f32(40)
        gains_t = G.t32
        gains_off = gains.offset
        identf = G.f32(128)
        fnorm_bc = G.f32(1024)
        rbias_bc = G.f32(20)
        lam_sb = G.f32(256)
        small = G.f32(64)
        relb = G.f32(4)
        oh_sb = G.f32(384)
        g_sb = G.f32(384)
        g_off = g_sb.offset
        maskT = G.f32(128)
        kvb = G.f32(16)
        pinv = G.f32(64)
        biasT = G.f32(1024).rearrange("p (h t q) -> p h t q", h=4, t=2)
        rstd_all = G.f32(64)
        ssq_all = G.f32(64)
        lnv = G.f32(8)
        FP8 = mybir.dt.float8e4
        g8 = G.t32.bitcast(FP8)
        junks = [g8[:, G.off + 1024 * k:G.off + 1024 * (k + 1)] for k in range(2)]
        G.off += 2048
        B_junk = [Buf() for _ in range(2)]
        jctr = [0]

        def SQ(src, Bsrc, accum, wr_):
            k = jctr[0] % 2
            jctr[0] += 1
            return S.add("act", lambda e: e.activation(junks[k], src, AF.Square, accum_out=accum, saturate=False),
                         Bsrc, wr_ + [B_junk[k]])
        lnv_ctr = [0]
        R1 = Arena(nc, st, "R1", 32 * 1024)
        R2 = Arena(nc, st, "R2", 64 * 1024)
        R3 = Arena(nc, st, "R3", 92 * 1024)
        ps_all = st.enter_context(nc.psum_tensor("ps_all", [128, 4096], F32))
        psb_all = ps_all.bitcast(BF16)
        ps = [ps_all[:, i * 512:(i + 1) * 512] for i in range(8)]
        psb = [psb_all[:, i * 1024:(i + 1) * 1024] for i in range(8)]
        Bp = [Buf("ps%d" % i) for i in range(8)]

        mixT = R1.t16[:, 0:16384].rearrange("p (c n) -> p c n", c=8)
        hT = mixT
        KT = R2.t16[:, 0:16384].rearrange("p (h n) -> p h n", h=2)
        Vv = R2.t16[:, 16384:32768].rearrange("p (k n) -> p k n", k=64)
        hres = R2.t32[:, 0:16384].rearrange("p (t n) -> p t n", t=16)

        def MM(out, lhsT, rhs, start, stop, rd, wr_):
            return S.add("pe", lambda e: e.matmul(out, lhsT, rhs, start=start, stop=stop), rd, wr_)

        def TR(out, in_, rd, wr_):
            return S.add("pe", lambda e: e.transpose(out, in_, ident), rd, wr_)

        def ACT(out, in_, func, rd, wr_, **kw):
            return S.add("act", lambda e: e.activation(out, in_, func, **kw), rd, wr_)

        def TT(eng, out, in0, in1, op, rd, wr_):
            return S.add(eng, lambda e: e.tensor_tensor(out, in0, in1, op), rd, wr_)

        def TS(eng, out, in0, s1, s2, op0, op1, rd, wr_):
            if op1 is None:
                return S.add(eng, lambda e: e.tensor_scalar(out, in0, s1, None, op0), rd, wr_)
            return S.add(eng, lambda e: e.tensor_scalar(out, in0, s1, s2, op0, op1), rd, wr_)

        def STT(out, in0, scalar, in1, op0, op1, rd, wr_):
            return S.add("dve", lambda e: e.scalar_tensor_tensor(out, in0, scalar, in1, op0, op1), rd, wr_)

        def CP(eng, out, in_, rd, wr_):
            if eng == "act":
                return S.add("act", lambda e: e.copy(out, in_), rd, wr_)
            return S.add(eng, lambda e: e.tensor_copy(out, in_), rd, wr_)

        def RECIP(out, in_, rd, wr_):
            return S.add("dve", lambda e: e.reciprocal(out, in_), rd, wr_)

        def DMA(q, out, in_, rd, wr_):
            return S.add(q, lambda e: e.dma_start(out=out, in_=in_), rd, wr_, dma=True)

        def gain_bc(c0):
            return bass.AP(gains_t, gains_off + c0, [[G.nbytes // 4, 128], [1, 8], [0, 128]])

        def gcol(c):
            return gains[:, c:c + 1]

        Bc = Buf("consts")
        B_g = Buf("g_sb")
        B_E = Buf("E")
        B_bias = Buf("biasT")
        DMA("pool", ident, ident_d, [], [Bc])
        DMA("sp", identf, ident_d, [], [Bc])
        DMA("sp", gains, gains_d, [], [Bc])
        DMA("sp", fnorm_bc, bass.AP(fnorm_t, 0, [[0, 128], [1, 1024]]), [], [Bc])
        DMA("sp", rbias_bc, bass.AP(rbias_t, 0, [[0, 128], [1, 20]]), [], [Bc])
        DMA("sp", lam_sb, bass.AP(lamv_t, 0, [[0, 128], [1, 256]]), [], [Bc])
        DMA("sp", relb[0:32, :], rel_bias, [], [Bc])
        DMA("sp", oh_sb[0:32, :], oh_d, [], [Bc])
        DMA("sp", maskT, maskT_d, [], [Bc])
        DMA("sp", kvb, kvb_d, [], [Bc])
        DMA("sp", pinv, pinv_d, [], [Bc])
        S.add("dve", lambda e: e.memset(ones_bf, 1.0), [], [Bc])
        S.add("dve", lambda e: e.memset(ones_f, 1.0 / 128.0), [], [Bc])
        prod = G.f32(128)
        TT("dve", prod[:, 0:64], lam_sb[:, 0:64], lam_sb[:, 64:128], ALU.mult, [Bc], [Bc])
        TT("dve", prod[:, 64:128], lam_sb[:, 128:192], lam_sb[:, 192:256], ALU.mult, [Bc], [Bc])
        S.add("dve", lambda e: e.reduce_sum(small[:, 0:1], prod[:, 0:64], axis=AX.X), [Bc], [Bc])
        S.add("dve", lambda e: e.reduce_sum(small[:, 1:2], prod[:, 64:128], axis=AX.X), [Bc], [Bc])
        ACT(small[:, 2:4], small[:, 0:2], AF.Exp, [Bc], [Bc])
        TT("dve", small[:, 4:5], small[:, 3:4], small[:, 2:3], ALU.subtract, [Bc], [Bc])
        TS("dve", small[:, 4:5], small[:, 4:5], -0.2, None, ALU.add, None, [Bc], [Bc])
        TS("dve", small[:, 5:6], gcol(36), 0.8, None, ALU.mult, None, [Bc], [Bc])
        neg_lam = small[:, 4:5]
        subcol = small[:, 5:6]
        def bias_part1():
            MM(ps[7][0:4, 0:384], relb[0:32, 0:4], oh_sb[0:32, 0:384], True, True, [Bc], [Bp[7]])
            CP("dve", g_sb[0:4, :], ps[7][0:4, 0:384], [Bp[7]], [B_g])

        def bias_part1b():
            DMA("sp", E_t.ap(), bass.AP(G.t32, g_off, [[G.nbytes // 4, 4], [0, 128], [1, 384]]), [B_g], [B_E])

        def bias_part2():
            DMA("sp", biasT[:, :, 0, :], bass.AP(E_t, 127, [[383, 128], [128 * 384, 4], [1, 128]]), [B_E], [B_bias])
            DMA("sp", biasT[:, :, 1, :], bass.AP(E_t, 255, [[383, 128], [128 * 384, 4], [1, 128]]), [B_E], [B_bias])

        def bias_part3():
            mask_bc = bass.AP(G.t32, maskT.offset, [[G.nbytes // 4, 128], [0, 4], [1, 128]])
            TT("dve", biasT[:, :, 0, :], biasT[:, :, 0, :], mask_bc, ALU.add, [B_bias, Bc], [B_bias])

        def norm_transpose(src, Bsrc, rstd_col, gain_c0, dst, Bdst, xs, Bxs, bank, Brs):
            ACT(xs, src, AF.Copy, [Bsrc, Brs], [Bxs], scale=rstd_col)
            for c in range(8):
                TR(psb[bank][:, c * 128:(c + 1) * 128], xs[:, c * 128:(c + 1) * 128], [Bxs, Bc], [Bp[bank]])
            TT("dve", dst, psb[bank][:, 0:1024].rearrange("p (c n) -> p c n", c=8), gain_bc(gain_c0), ALU.mult,
               [Bp[bank], Bc], [Bdst])

        B_lnc = [Buf() for _ in range(8)]

        def rms_cols(src, Bsrc, col):
            k = lnv_ctr[0] % 8
            lnv_ctr[0] += 1
            b1 = Buf()
            b3 = Buf()
            SQ(src, [Bsrc], ssq_all[:, col:col + 1], [b1])
            ACT(lnv[:, k:k + 1], ssq_all[:, col:col + 1], AF.Ln, [b1], [B_lnc[k]], scale=1.0 / 1024.0, bias=1e-6)
            ACT(rstd_all[:, col:col + 1], lnv[:, k:k + 1], AF.Exp, [B_lnc[k]], [b3], scale=-0.5)
            return b3
        brs_c = [None] * 16
        brs_d = [None] * 16
        brs_f = [None] * 16
        B_ssq = [Buf() for _ in range(64)]
        B_ln = [Buf() for _ in range(2)]
        B_rslot = [Buf() for _ in range(16)]
        B_mix = [[Buf("mix%d_%d" % (c, s)) for s in range(4)] for c in range(8)]

        for pr in range(2):
            S.barrier()
            R3.reset()
            wA = R3.bf16(8 * 768).rearrange("p (c n) -> p c n", c=8)
            B_wA = [Buf() for _ in range(3)]
            for part, c0 in enumerate((512 + 256 * pr, 1024 + 256 * pr, 256 * pr)):
                DMA("pool", wA[:, :, part * 256:(part + 1) * 256],
                    w_in[:, c0:c0 + 256].rearrange("(c p) n -> p c n", p=128), [], [B_wA[part]])
            if pr == 0:
                wU = R3.bf16(8 * 512).rearrange("p (c n) -> p c n", c=8)
                B_wU = Buf()
                DMA("pool", wU, w_in[:, 1536:2048].rearrange("(c p) n -> p c n", p=128), [], [B_wU])
                pw = R3.bf16(512).rearrange("p (g n) -> p g n", g=4)
                B_pw = Buf()
                DMA("pool", pw, pool_w.rearrange("g c d -> c g d"), [], [B_pw])
            QT = R3.bf16(2 * 2 * 2048).rearrange("p (h m n) -> p h m n", h=2, m=2)
            B_QT = [[Buf() for _ in range(4)] for _ in range(2)]
            B_QTz = Buf()
            S.add("pool", lambda e, QT=QT: e.memset(QT.rearrange("p h m n -> p (h m n)"), 0.0), [], [B_QTz])
            mark = R3.off
            xst = [R3.f32(1024) for _ in range(4)]
            if pr == 0:
                xst += [R1.t32[:, k * 1024:(k + 1) * 1024] for k in range(4)]
            else:
                xst += [R3.f32(1024) for _ in range(4)]
            B_xst = [Buf() for _ in range(8)]
            nxs = 3 if pr == 0 else 4
            xs_t = [R3.bf16(1024) for _ in range(nxs)]
            B_xs = [Buf() for _ in range(nxs)]
            hnT = [R3.bf16(8 * 512).rearrange("p (c n) -> p c n", c=8) for _ in range(2)]
            B_hnT = [[Buf() for _ in range(4)] for _ in range(2)]
            if pr == 0:
                uT = R3.f32(4 * 528).rearrange("p (g n) -> p g n", g=4)
                B_uT = [Buf() for _ in range(4)]
                B_uTp = [Buf() for _ in range(4)]
                ptmp = [R3.f32(528) for _ in range(2)]
                B_pt = [Buf() for _ in range(2)]
                dT = R3.bf16(4 * 512).rearrange("p (g n) -> p g n", g=4)
                B_dT = [Buf() for _ in range(4)]
            kq = [0]

            def kbank():
                b_ = 4 + kq[0] % 3
                kq[0] += 1
                return b_

            def emit_V(g, hb, tt):
                vb = 2 + g % 2
                for c in range(8):
                    MM(ps[vb][:, 0:256], hnT[hb][:, c, tt * 128:(tt + 1) * 128], wA[:, c, 256:512], c == 0, c == 7,
                       [B_hnT[hb][tt], B_wA[1]], [Bp[vb]])
                CP("act" if pr == 1 else "dve", Vv[:, g, :], ps[vb][:, 0:256], [Bp[vb]], [])

            def emit_slot(i, hb):
                own = (i % 4 == 3)
                s_own = i // 4
                allh = B_hnT[hb]
                for hc in range(2):
                    kb_ = kbank()
                    for c in range(8):
                        MM(ps[kb_][:, :], wA[:, c, hc * 128:(hc + 1) * 128], hnT[hb][:, c, :], c == 0, c == 7,
                           allh + [B_wA[0]], [Bp[kb_]])
                    CP("dve" if (hc or pr == 0) else "act", KT[:, hc, i * 512:(i + 1) * 512], ps[kb_][:, :], [Bp[kb_]], [])
                if own:
                    for hc in range(2):
                        kb_ = kbank()
                        for c in range(8):
                            MM(ps[kb_][:, :], wA[:, c, 512 + hc * 128:512 + (hc + 1) * 128], hnT[hb][:, c, :], c == 0, c == 7,
                               allh + [B_wA[2]], [Bp[kb_]])
                        TS("dve", QT[0:64, hc, 0, s_own * 512:(s_own + 1) * 512], ps[kb_][0:64, :], 0.125, None, ALU.mult, None,
                           [Bp[kb_], B_QTz], [B_QT[hc][s_own]])
                        TS("dve", QT[64:128, hc, 1, s_own * 512:(s_own + 1) * 512], ps[kb_][64:128, :], 0.125, None, ALU.mult, None,
                           [Bp[kb_], B_QTz, B_QT[hc][s_own]], [B_QT[hc][s_own]])
                if pr == 0 and i % 4 == 2:
                    for gg in range(4):
                        kb_ = kbank()
                        for c in range(8):
                            MM(ps[kb_][:, 0:16], wU[:, c, gg * 128:(gg + 1) * 128], hnT[hb][:, c, 496:512], c == 0, c == 7,
                               [B_hnT[hb][3], B_wU], [Bp[kb_]])
                        CP("dve", uT[:, gg, 0:16], ps[kb_][:, 0:16], [Bp[kb_]], [B_uTp[gg]])
                if pr == 0 and own:
                    for gg in range(4):
                        w = 2 ** (gg + 1)
                        kb_ = kbank()
                        for c in range(8):
                            MM(ps[kb_][:, :], wU[:, c, gg * 128:(gg + 1) * 128], hnT[hb][:, c, :], c == 0, c == 7,
                               allh + [B_wU], [Bp[kb_]])
                        CP("act", uT[:, gg, 16:528], ps[kb_][:, :], [Bp[kb_]], [B_uT[gg]])
                        U = uT[:, gg, :]
                        ru = [B_uT[gg], B_uTp[gg]]
                        TT("pool", ptmp[0][:, 1:528], U[:, 1:528], U[:, 0:527], ALU.add, ru, [B_pt[0]])
                        cur = 0
                        sh = 2
                        lo = 1
                        while sh < w:
                            lo += sh
                            TT("pool", ptmp[1 - cur][:, lo:528], ptmp[cur][:, lo:528], ptmp[cur][:, lo - sh:528 - sh], ALU.add,
                               [B_pt[cur]], [B_pt[1 - cur]])
                            cur = 1 - cur
                            sh *= 2
                        STT(dT[:, gg, :], ptmp[cur][:, 16:528], 1.0 / w, U[:, 16:528], ALU.mult, ALU.subtract,
                            [B_pt[cur]] + ru, [B_dT[gg]])
                        if s_own == 0:
                            tmp16 = small[:, 16:32]
                            TT("dve", tmp16, ptmp[cur][:, 16:32], pinv[:, gg * 16:(gg + 1) * 16], ALU.mult, [B_pt[cur], Bc], [Bc])
                            TT("dve", dT[:, gg, 0:16], tmp16, U[:, 16:32], ALU.subtract, [Bc] + ru + [B_dT[gg]], [B_dT[gg], Bc])

                    def stageB(s_own=s_own):
                        for gg in range(4):
                            kb2 = kbank()
                            MM(ps[kb2][:, :], pw[:, gg, :], dT[:, gg, :], True, True, [B_pw, B_dT[gg]], [Bp[kb2]])
                            TS("dve", mixT[:, 4 + gg, s_own * 512:(s_own + 1) * 512], ps[kb2][:, :], gcol(32 + gg), None, ALU.mult, None,
                               [Bp[kb2], Bc], [B_mix[4 + gg][s_own]])
                    late.append(stageB)

            def emit_load(g):
                xb = 4 * ((g // 4) % 2) + g % 4
                DMA("sp", xst[xb], x[g * 128:(g + 1) * 128, :], [], [B_xst[xb]])

            def emit_sq1(g):
                if pr == 0:
                    xb = 4 * ((g // 4) % 2) + g % 4
                    SQ(xst[xb], [B_xst[xb]], ssq_all[:, g:g + 1], [B_ssq[g]])

            def emit_stats(i, squares=True):
                for tt in range(4):
                    if squares:
                        emit_sq1(4 * i + tt)
                if pr == 0:
                    lv = lnv[:, 4 * (i % 2):4 * (i % 2) + 4]
                    ACT(lv, ssq_all[:, 4 * i:4 * i + 4], AF.Ln, B_ssq[4 * i:4 * i + 4], [B_ln[i % 2]], scale=1.0 / 1024.0, bias=1e-6)
                    ACT(rstd_all[:, 4 * i:4 * i + 4], lv, AF.Exp, [B_ln[i % 2]], [B_rslot[i]], scale=-0.5)

            pending = []
            late = []
            TB = (0, 1, 7)

            def partA(g):
                i_, tt_ = g // 4, g % 4
                xb = 4 * (i_ % 2) + tt_
                ACT(xs_t[g % nxs], xst[xb], AF.Copy, [B_xst[xb], B_rslot[i_]], [B_xs[g % nxs]], scale=rstd_all[:, g:g + 1])

            def partB(g):
                i_, tt_ = g // 4, g % 4
                hb_ = i_ % 2
                bank = TB[g % 3]
                xs = xs_t[g % nxs]
                for c in range(8):
                    TR(psb[bank][:, c * 128:(c + 1) * 128], xs[:, c * 128:(c + 1) * 128], [B_xs[g % nxs], Bc], [Bp[bank]])
                TT("dve", hnT[hb_][:, :, tt_ * 128:(tt_ + 1) * 128], psb[bank][:, 0:1024].rearrange("p (c n) -> p c n", c=8),
                   gain_bc(0), ALU.mult, [Bp[bank], Bc], [B_hnT[hb_][tt_]])

            for g in range(8):
                emit_load(g)
            emit_stats(0)
            emit_stats(1)
            partA(0)
            partA(1)
            emit_load(8)
            emit_load(9)
            for g in range(64):
                i, tt = g // 4, g % 4
                hb = i % 2
                if tt == 0:
                    run_late = late[:]
                    del late[:]
                if pr == 0 and g == 8:
                    bias_part1()
                if pr == 0 and g == 16:
                    bias_part1b()
                if pr == 0 and g == 24:
                    bias_part2()
                if pr == 0 and g == 40:
                    bias_part3()
                if g + 2 < 64:
                    partA(g + 2)
                if g + 10 < 64:
                    emit_load(g + 10)
                if g + 8 < 64:
                    emit_sq1(g + 8)
                    if tt == 3:
                        emit_stats(i + 2, squares=False)
                partB(g)
                cur_p = [lambda g=g, hb=hb, tt=tt: emit_V(g, hb, tt)]
                if tt == 3:
                    cur_p.append(lambda i=i, hb=hb: emit_slot(i, hb))
                if tt == 2:
                    cur_p.extend(run_late)
                pending.append(cur_p)
                if len(pending) > 2:
                    for f_ in pending.pop(0):
                        f_()
            for grp_ in pending:
                for f_ in grp_:
                    f_()
            for f_ in late:
                f_()
            pending = []
            late = []

            S.barrier()
            R3.reset(mark)
            NP = 3
            Pt = [R3.bf16(1024) for _ in range(NP)]
            B_P = [Buf() for _ in range(NP)]
            rs = [R3.f32(512) for _ in range(2)]
            tq = [R3.f32(512) for _ in range(2)]
            o_sb = R3.f32(512)
            sq_sb = R3.f32(512)
            r2_sb = R3.f32(512)
            B_fin = [Buf() for _ in range(8)]
            if pr == 1:
                TOP = R3.nbytes - 32 * 1024
                assert R3.off <= TOP, R3.off
                R3.reset(TOP)
                wO = R3.bf16(8 * 1024).rearrange("p (c n) -> p c n", c=8)
                wQ = R3.bf16(8 * 1024).rearrange("p (c n) -> p c n", c=8)
                B_wO = Buf()
                B_wQ = Buf()
                DMA("pool", wO, w_out.rearrange("(c p) n -> p c n", p=128), [], [B_wO])
                DMA("pool", wQ, wq.rearrange("(c p) n -> p c n", p=128), [], [B_wQ])
            flat = []
            for s in range(4):
                for hc in range(2):
                    nkb_ = 4 * (4 * s + 3 + 1)
                    for kb in range(nkb_):
                        flat.append((s, hc, kb, nkb_))
            n = len(flat)
            sbank = {}
            pbuf = {}
            rot = {"s": 0, "p": 0}

            def geom(t):
                s, hc, kb, nkb = flat[t]
                r = kb - 4 * (4 * s + 3)
                return s, hc, kb, nkb, r, 128 * max(r, 0)

            def QK(t):
                s, hc, kb, nkb, r, c0 = geom(t)
                b0 = 2 * (rot["s"] % 2)
                rot["s"] += 1
                sbank[t] = b0
                for m in range(2):
                    MM(ps[b0 + m][:, c0:512], KT[:, hc, kb * 128:(kb + 1) * 128],
                       QT[:, hc, m, s * 512 + c0:(s + 1) * 512], True, True,
                       [B_QT[hc][s]], [Bp[b0 + m]])

            def SOFT(t):
                s, hc, kb, nkb, r, c0 = geom(t)
                h = 2 * pr + hc
                b0 = sbank[t]
                if r >= -1:
                    if r == -1:
                        cs, nb_, boff = 0, 128, 128
                    elif r == 3:
                        cs, nb_, boff = 384, 128, 0
                    else:
                        cs, nb_, boff = 128 * r, 256, 0
                    pv_ = ps_all[:, b0 * 512:(b0 + 2) * 512].rearrange("p (b n) -> p b n", b=2)[:, :, cs:cs + nb_]
                    bia = bass.AP(G.t32, biasT.offset + h * 256 + boff, [[G.nbytes // 4, 128], [0, 2], [1, nb_]])
                    TT("dve", pv_, pv_, bia, ALU.add, [Bp[b0], Bp[b0 + 1], B_bias], [Bp[b0], Bp[b0 + 1]])
                pb = rot["p"] % NP
                rot["p"] += 1
                pbuf[t] = pb
                kw = {}
                if kb // 4 <= 2:
                    kw["bias"] = kvb[:, kb // 4:kb // 4 + 1]
                src = ps_all[:, b0 * 512:(b0 + 2) * 512].rearrange("p (b n) -> p b n", b=2)[:, :, c0:512]
                dst = Pt[pb].rearrange("p (b n) -> p b n", b=2)[:, :, c0:512]
                ACT(dst, src, AF.Exp, [Bp[b0], Bp[b0 + 1], Bc], [B_P[pb]], **kw)

            def PV(t):
                s, hc, kb, nkb, r, c0 = geom(t)
                pb = pbuf[t]
                first = (kb == 0)
                last = (kb == nkb - 1)
                for m in range(2):
                    Pm = Pt[pb][:, m * 512 + c0:(m + 1) * 512]
                    MM(ps[4 + m][:, c0:512], Vv[:, kb, hc * 128:(hc + 1) * 128], Pm, first, last,
                       [B_P[pb]], [Bp[4 + m]])
                    MM(ps[6 + m][:, c0:512], ones_bf, Pm, first, last, [B_P[pb], Bc], [Bp[6 + m]])

            def FIN_a(s, hc):
                RECIP(rs[0], ps[6][:, :], [Bp[6]], [B_fin[0]])
                TT("dve", tq[0], ps[4][:, :], rs[0], ALU.mult, [Bp[4], B_fin[0]], [B_fin[2]])
                RECIP(rs[1], ps[7][:, :], [Bp[7]], [B_fin[1]])
                TT("dve", tq[1], ps[5][:, :], rs[1], ALU.mult, [Bp[5], B_fin[1]], [B_fin[3]])
                STT(o_sb, tq[1], neg_lam, tq[0], ALU.mult, ALU.add, [B_fin[2], B_fin[3], Bc], [B_fin[4]])
                ACT(sq_sb, o_sb, AF.Square, [B_fin[4]], [B_fin[5]])

            def FIN_b(s, hc, bm):
                h = 2 * pr + hc
                MM(ps[bm][:, :], ones_f, sq_sb, True, True, [B_fin[5], Bc], [Bp[bm]])
                ACT(r2_sb, ps[bm][:, :], AF.Ln, [Bp[bm]], [B_fin[6]], bias=1e-6)
                ACT(r2_sb, r2_sb, AF.Exp, [B_fin[6]], [B_fin[6]], scale=-0.5)
                STT(mixT[:, h, s * 512:(s + 1) * 512], o_sb, subcol, r2_sb, ALU.mult, ALU.mult,
                    [B_fin[4], B_fin[6], Bc], [B_mix[h][s]])

            QK(0)
            QK(1)
            pend_fin = None
            for t in range(n):
                SOFT(t)
                if pend_fin is not None and (t >= pend_fin[2] or t == n - 1):
                    FIN_b(pend_fin[0], pend_fin[1], sbank[t])
                    pend_fin = None
                if t + 2 < n:
                    QK(t + 2)
                PV(t)
                s_, hc_, kb_l, nkb_l = flat[t]
                if kb_l == nkb_l - 1:
                    FIN_a(s_, hc_)
                    pend_fin = (s_, hc_, t + 4)
            if pend_fin is not None:
                FIN_b(pend_fin[0], pend_fin[1], 0)

        S.barrier()
        R3.reset()
        if debug:
            DMA("sp", dbg["dbg_mix"], R1.t16[:, 0:16384], [b for row in B_mix for b in row], [])
        wKV = R3.bf16(8 * 2048).rearrange("p (c n) -> p c n", c=8)
        mark_b = R3.off
        B_wKV = Buf()
        DMA("pool", wKV[:, :, 0:1024], wkv[:, 0:1024].rearrange("(c p) n -> p c n", p=128), [], [B_wKV])
        DMA("pool", wKV[:, :, 1024:2048], wkv[:, 1024:2048].rearrange("(c p) n -> p c n", p=128), [], [B_wKV])
        xo = [R3.f32(1024) for _ in range(2)]
        assert R3.off <= TOP
        B_xo = [Buf() for _ in range(2)]
        B_h = [Buf("h%d" % t) for t in range(16)]
        allmix = [b for row in B_mix for b in row]
        for tt in range(16):
            s_, t4 = tt // 4, tt % 4
            row0 = (4 * s_ + 3) * 512 + t4 * 128
            DMA("sp", xo[tt % 2], x[row0:row0 + 128, :], [], [B_xo[tt % 2]])
            for half in range(2):
                b = 2 * (tt % 2) + half
                for c in range(8):
                    MM(ps[b][:, :], mixT[:, c, tt * 128:(tt + 1) * 128], wO[:, c, half * 512:(half + 1) * 512], c == 0, c == 7,
                       [B_mix[c][s_], B_wO], [Bp[b]])
                TT("dve", hres[:, tt, half * 512:(half + 1) * 512], ps[b][:, :], xo[tt % 2][:, half * 512:(half + 1) * 512], ALU.add,
                   [Bp[b], B_xo[tt % 2]], [B_h[tt]])
            brs_c[tt] = rms_cols(hres[:, tt, :], B_h[tt], tt)

        def dump_h(name):
            if debug:
                for tt in range(16):
                    DMA("sp", dbg[name][tt * 128:(tt + 1) * 128, :], hres[:, tt, :], [B_h[tt]], [])

        dump_h("dbg_h1")

        S.barrier()
        R3.reset(mark_b)
        xs_t = [R3.bf16(1024) for _ in range(2)]
        B_xs = [Buf() for _ in range(2)]
        mnT = R3.bf16(8 * 256).rearrange("p (c n) -> p c n", c=8)
        B_mnT = [Buf() for _ in range(2)]
        KcT = R3.bf16(8 * 256).rearrange("p (c n) -> p c n", c=8)
        B_Kc = Buf()
        Vc = R3.bf16(2 * 1024).rearrange("p (k n) -> p k n", k=2)
        B_Vc = Buf()
        mark_c = R3.off
        mst = [R3.f32(1024) for _ in range(2)]
        B_mst = [Buf() for _ in range(2)]

        for mk in range(2):
            DMA("sp", mst[mk], mem[mk * 128:(mk + 1) * 128, :], [], [B_mst[mk]])
            brs = rms_cols(mst[mk], B_mst[mk], 32 + mk)
            norm_transpose(mst[mk], B_mst[mk], rstd_all[:, 32 + mk:33 + mk], 16, mnT[:, :, mk * 128:(mk + 1) * 128], B_mnT[mk],
                           xs_t[mk], B_xs[mk], mk, brs)
        for j8 in range(8):
            b = 4 + j8 % 4
            for c in range(8):
                MM(ps[b][:, 0:256], wKV[:, c, j8 * 128:(j8 + 1) * 128], mnT[:, c, :], c == 0, c == 7, B_mnT + [B_wKV], [Bp[b]])
            CP("dve" if j8 % 2 else "act", KcT[:, j8, :], ps[b][:, 0:256], [Bp[b]], [B_Kc])
        for mk in range(2):
            for half in range(2):
                b = 4 + (2 * mk + half) % 4
                for c in range(8):
                    MM(ps[b][:, :], mnT[:, c, mk * 128:(mk + 1) * 128], wKV[:, c, 1024 + half * 512:1024 + (half + 1) * 512], c == 0, c == 7,
                       B_mnT + [B_wKV], [Bp[b]])
                CP("dve" if half else "act", Vc[:, mk, half * 512:(half + 1) * 512], ps[b][:, :], [Bp[b]], [B_Vc])
        B_hT = [[Buf() for _ in range(4)] for _ in range(4)]
        for tt in range(16):
            norm_transpose(hres[:, tt, :], B_h[tt], rstd_all[:, tt:tt + 1], 8, hT[:, :, tt * 128:(tt + 1) * 128], B_hT[tt // 4][tt % 4],
                           xs_t[tt % 2], B_xs[tt % 2], tt % 2, brs_c[tt])
        S.barrier()
        wOc = wKV[:, :, 0:1024]
        B_wOc = Buf()
        DMA("pool", wOc, wo.rearrange("(c p) n -> p c n", p=128), [], [B_wOc])
        R3.reset(mark_c)
        qT = R3.bf16(8 * 512).rearrange("p (c n) -> p c n", c=8)
        B_qT = [Buf() for _ in range(8)]
        ocT = R3.bf16(8 * 512).rearrange("p (c n) -> p c n", c=8)
        B_oc = [Buf() for _ in range(8)]
        Pc = [R3.bf16(512) for _ in range(4)]
        B_Pc = [Buf() for _ in range(4)]
        rsc2 = [R3.f32(512) for _ in range(2)]
        B_rsc2 = [Buf() for _ in range(2)]
        pc_rot = 0
        for s in range(4):
            for j8 in range(8):
                b = j8 % 2
                for c in range(8):
                    MM(ps[b][:, :], wQ[:, c, j8 * 128:(j8 + 1) * 128], hT[:, c, s * 512:(s + 1) * 512], c == 0, c == 7,
                       B_hT[s] + [B_wQ], [Bp[b]])
                if j8 % 2:
                    TS("dve", qT[:, j8, :], ps[b][:, :], 1.0 / 16.0, None, ALU.mult, None, [Bp[b]], [B_qT[j8]])
                else:
                    ACT(qT[:, j8, :], ps[b][:, :], AF.Copy, [Bp[b]], [B_qT[j8]], scale=1.0 / 16.0)
            pcs_h = {}

            def c_scores(hh):
                nonlocal pc_rot
                pcs = []
                for mk in range(2):
                    b = 2 + mk
                    for e2 in range(2):
                        MM(ps[b][:, :], KcT[:, 2 * hh + e2, mk * 128:(mk + 1) * 128], qT[:, 2 * hh + e2, :], e2 == 0, e2 == 1,
                           [B_Kc, B_qT[2 * hh + e2]], [Bp[b]])
                    pi = pc_rot % 4
                    pc_rot += 1
                    pcs.append(pi)
                    ACT(Pc[pi], ps[b][:, :], AF.Exp, [Bp[b]], [B_Pc[pi]])
                pcs_h[hh] = pcs

            def c_pv(hh):
                pcs = pcs_h[hh]
                st_ = hh % 2
                ob = (4, 5) if st_ == 0 else (0, 1)
                sb_ = 6 if st_ == 0 else 7
                for e2 in range(2):
                    b = ob[e2]
                    for mk in range(2):
                        MM(ps[b][:, :], Vc[:, mk, (2 * hh + e2) * 128:(2 * hh + e2 + 1) * 128], Pc[pcs[mk]], mk == 0, mk == 1,
                           [B_Vc, B_Pc[pcs[mk]]], [Bp[b]])
                for mk in range(2):
                    MM(ps[sb_][:, :], ones_bf, Pc[pcs[mk]], mk == 0, mk == 1, [Bc, B_Pc[pcs[mk]]], [Bp[sb_]])
                RECIP(rsc2[st_], ps[sb_][:, :], [Bp[sb_]], [B_rsc2[st_]])
                for e2 in range(2):
                    TT("dve", ocT[:, 2 * hh + e2, :], ps[ob[e2]][:, :], rsc2[st_], ALU.mult, [Bp[ob[e2]], B_rsc2[st_]], [B_oc[2 * hh + e2]])

            c_scores(0)
            for hh in range(4):
                if hh + 1 < 4:
                    c_scores(hh + 1)
                c_pv(hh)
            for t4 in range(4):
                tt = 4 * s + t4
                for half in range(2):
                    b = half
                    for c in range(8):
                        MM(ps[b][:, :], ocT[:, c, t4 * 128:(t4 + 1) * 128], wOc[:, c, half * 512:(half + 1) * 512], c == 0, c == 7,
                           [B_oc[c], B_wOc], [Bp[b]])
                    TT("dve", hres[:, tt, half * 512:(half + 1) * 512], ps[b][:, :], hres[:, tt, half * 512:(half + 1) * 512], ALU.add,
                       [Bp[b], B_h[tt]], [B_h[tt]])
                brs_d[tt] = rms_cols(hres[:, tt, :], B_h[tt], 16 + tt)
        dump_h("dbg_h2")

        S.barrier()
        R3.reset()
        NU = 2
        ring = []
        for k in range(NU):
            unit = []
            for _e in range(2):
                wg_ = R3.bf16(8 * 256).rearrange("p (c n) -> p c n", c=8)
                wu_ = R3.bf16(8 * 256).rearrange("p (c n) -> p c n", c=8)
                wd_ = R3.bf16(2 * 1024).rearrange("p (f n) -> p f n", f=2)
                unit.append((wg_, wu_, wd_, Buf(), Buf(), Buf()))
            ring.append(unit)

        def load_unit(u):
            for e2 in range(2):
                e = 2 * u + e2
                wg_, wu_, wd_, b1, b2, b3 = ring[u % NU][e2]
                DMA("pool", wg_, w_gate[e].rearrange("(c p) n -> p c n", p=128), [], [b1])
                DMA("pool", wu_, w_up[e].rearrange("(c p) n -> p c n", p=128), [], [b2])
                DMA("pool", wd_, w_down[e].rearrange("(f p) n -> p f n", p=128), [], [b3])

        wR = R3.bf16(8 * 20).rearrange("p (c n) -> p c n", c=8)
        B_wR = Buf()
        DMA("pool", wR, wr.rearrange("(c p) n -> p c n", p=128), [], [B_wR])
        for u in range(NU):
            load_unit(u)
        selT = R3.bf16(2048)
        B_sel = Buf()
        DMA("pool", selT[0:16, :], sel_d, [], [B_sel])
        chi = R3.bf16(2048)
        clo = R3.bf16(2048)
        combT = R3.f32(2048)
        B_comb = [Buf() for _ in range(16)]
        rt_n = 16 * (20 + 16 + 4 * 8 + 16 + 9)
        rt = R3.f32(rt_n)
        B_rt = Buf()
        B_lg = [Buf() for _ in range(16)]
        _o = [0]

        def rtv(n_inner):
            o = _o[0]
            _o[0] += 16 * n_inner
            v = rt[:, o:o + 16 * n_inner]
            return v if n_inner == 1 else v.rearrange("p (t k) -> p t k", t=16)

        lg = rtv(20); comb = rtv(16); goh = rtv(4); gex = rtv(4); esel = rtv(4); oh1 = rtv(4); es2 = rtv(4); oh2 = rtv(4)
        inner = rtv(4); gsc = rtv(4); prod16 = rtv(16)
        gmax = rtv(1); gsum = rtv(1); gw = rtv(1); m1 = rtv(1); m2 = rtv(1); e21 = rtv(1); den = rtv(1); w1 = rtv(1); w2 = rtv(1)
        prod4 = prod16.rearrange("p t (g e) -> p t g e", g=4)
        comb4 = comb.rearrange("p t (g e) -> p t g e", g=4)

        def bcl(v, n):
            return bass.AP(v.tensor, v.offset, [list(d) for d in v.ap] + [[0, n]])

        def bcm(v, n):
            dd = [list(d) for d in v.ap]
            return bass.AP(v.tensor, v.offset, dd[:-1] + [[0, n]] + dd[-1:])

        actT = R3.bf16(2 * 2 * 512).rearrange("p (e f n) -> p e f n", e=2, f=2)
        B_act = [[Buf() for _ in range(2)] for _ in range(2)]
        sg = [R3.f32(512) for _ in range(2)]
        B_sg = [Buf() for _ in range(2)]
        tg = [R3.f32(512) for _ in range(2)]
        B_tg = [Buf() for _ in range(2)]
        bcs = [R3.f32(512) for _ in range(2)]
        B_bcs = [Buf() for _ in range(2)]
        xs_t = [tg[0].bitcast(BF16), tg[1].bitcast(BF16)]
        B_xs = [Buf() for _ in range(2)]
        B_h3T = [[Buf() for _ in range(4)] for _ in range(4)]
        for tt in range(16):
            norm_transpose(hres[:, tt, :], B_h[tt], rstd_all[:, 16 + tt:17 + tt], 24, hT[:, :, tt * 128:(tt + 1) * 128], B_h3T[tt // 4][tt % 4],
                           xs_t[tt % 2], B_xs[tt % 2], tt % 2, brs_d[tt])
            for c in range(8):
                MM(ps[2 + tt % 2][:, 0:20], hT[:, c, tt * 128:(tt + 1) * 128], wR[:, c, :], c == 0, c == 7,
                   [B_h3T[tt // 4][tt % 4], B_wR], [Bp[2 + tt % 2]])
            TT("dve", lg[:, tt, :], ps[2 + tt % 2][:, 0:20], rbias_bc, ALU.add, [Bp[2 + tt % 2], Bc], [B_lg[tt]])

        R_ = [B_rt]
        lgg = lg[:, :, 0:4]
        le4 = lg[:, :, 4:20].rearrange("p t (g e) -> p t g e", g=4)
        S.add("dve", lambda e: e.tensor_reduce(gmax, lgg, axis=AX.X, op=ALU.max), B_lg, R_)
        TT("dve", goh, lgg, bcl(gmax, 4), ALU.is_equal, B_lg + R_, R_)
        TT("dve", gex, lgg, bcl(gmax, 4), ALU.subtract, B_lg + R_, R_)
        ACT(gex, gex, AF.Exp, R_, R_)
        S.add("dve", lambda e: e.tensor_reduce(gsum, gex, axis=AX.X, op=ALU.add), R_, R_)
        RECIP(gw, gsum, R_, R_)
        TT("dve", prod4, le4, bcl(goh, 4), ALU.mult, B_lg + R_, R_)
        S.add("dve", lambda e: e.tensor_reduce(esel, prod4.rearrange("p t g e -> p t e g"), axis=AX.X, op=ALU.add), R_, R_)
        S.add("dve", lambda e: e.tensor_reduce(m1, esel, axis=AX.X, op=ALU.max), R_, R_)
        TT("dve", oh1, esel, bcl(m1, 4), ALU.is_equal, R_, R_)
        STT(es2, oh1, -1e30, esel, ALU.mult, ALU.add, R_, R_)
        S.add("dve", lambda e: e.tensor_reduce(m2, es2, axis=AX.X, op=ALU.max), R_, R_)
        TT("dve", oh2, es2, bcl(m2, 4), ALU.is_equal, R_, R_)
        TT("dve", e21, m2, m1, ALU.subtract, R_, R_)
        ACT(e21, e21, AF.Exp, R_, R_)
        TS("dve", den, e21, 1.0, None, ALU.add, None, R_, R_)
        RECIP(w1, den, R_, R_)
        TT("dve", w2, e21, w1, ALU.mult, R_, R_)
        TT("dve", inner, oh1, bcl(w1, 4), ALU.mult, R_, R_)
        TT("dve", oh2, oh2, bcl(w2, 4), ALU.mult, R_, R_)
        TT("dve", inner, inner, oh2, ALU.add, R_, R_)
        TT("dve", gsc, goh, bcl(gw, 4), ALU.mult, R_, R_)
        TT("dve", comb4, bcl(gsc, 4), bcm(inner, 4), ALU.mult, R_, R_)
        for q4 in range(4):
            pb_ = 2 + q4 % 2
            for t4 in range(4):
                tt = 4 * q4 + t4
                S.add("pe", lambda e, tt=tt, t4=t4, pb_=pb_: e.transpose(ps[pb_][0:16, t4 * 128:(t4 + 1) * 128], comb[:, tt, :], identf),
                      [B_rt, Bc], [Bp[pb_]])
            CP("dve", combT[0:16, q4 * 512:(q4 + 1) * 512], ps[pb_][0:16, :], [Bp[pb_]], B_comb[4 * q4:4 * q4 + 4])
            CP("dve", chi[0:16, q4 * 512:(q4 + 1) * 512], combT[0:16, q4 * 512:(q4 + 1) * 512], B_comb[4 * q4:4 * q4 + 4], B_comb[4 * q4:4 * q4 + 4])
            TT("dve", clo[0:16, q4 * 512:(q4 + 1) * 512], combT[0:16, q4 * 512:(q4 + 1) * 512], chi[0:16, q4 * 512:(q4 + 1) * 512], ALU.subtract,
               B_comb[4 * q4:4 * q4 + 4], B_comb[4 * q4:4 * q4 + 4])

        d_rot = [0]

        def gu_mm(u, s, e2, f):
            wg_, wu_, wd_, b1, b2, b3 = ring[u % NU][e2]
            bg = 2 * f
            bu = 2 * f + 1
            for c in range(8):
                MM(ps[bg][:, :], wg_[:, c, f * 128:(f + 1) * 128], hT[:, c, s * 512:(s + 1) * 512], c == 0, c == 7,
                   B_h3T[s] + [b1], [Bp[bg]])
            for c in range(8):
                MM(ps[bu][:, :], wu_[:, c, f * 128:(f + 1) * 128], hT[:, c, s * 512:(s + 1) * 512], c == 0, c == 7,
                   B_h3T[s] + [b2], [Bp[bu]])

        def gu_post(u, s, e2, f):
            e = 2 * u + e2
            bi = e2
            bg = 2 * f
            bu = 2 * f + 1
            if f == 0:
                MM(ps[6][:, :], selT[0:16, e * 128:(e + 1) * 128], chi[0:16, s * 512:(s + 1) * 512], True, False,
                   [B_sel] + B_comb[4 * s:4 * s + 4], [Bp[6]])
                MM(ps[6][:, :], selT[0:16, e * 128:(e + 1) * 128], clo[0:16, s * 512:(s + 1) * 512], False, True,
                   [B_sel] + B_comb[4 * s:4 * s + 4], [Bp[6]])
                CP("act", bcs[bi], ps[6][:, :], [Bp[6]], [B_bcs[bi]])
            ACT(sg[f], ps[bg][:, :], AF.Silu, [Bp[bg]], [B_sg[f]])
            TT("dve", tg[f], ps[bu][:, :], sg[f], ALU.mult, [Bp[bu], B_sg[f]], [B_tg[f]])
            TT("pool", actT[:, e2, f, :], tg[f], bcs[bi], ALU.mult, [B_tg[f], B_bcs[bi]], [B_act[e2][f]])

        def down(u, s):
            unit = ring[u % NU]
            for t4 in range(4):
                tt = 4 * s + t4
                for half in range(2):
                    b = 4 + d_rot[0] % 2
                    d_rot[0] += 1
                    k = 0
                    for e2 in range(2):
                        wd_, b3 = unit[e2][2], unit[e2][5]
                        for f in range(2):
                            MM(ps[b][:, :], actT[:, e2, f, t4 * 128:(t4 + 1) * 128], wd_[:, f, half * 512:(half + 1) * 512],
                               k == 0, k == 3, [B_act[e2][f], b3], [Bp[b]])
                            k += 1
                    TT("dve", hres[:, tt, half * 512:(half + 1) * 512], ps[b][:, :], hres[:, tt, half * 512:(half + 1) * 512], ALU.add,
                       [Bp[b], B_h[tt]], [B_h[tt]])
                if u == 7:
                    brs_f[tt] = rms_cols(hres[:, tt, :], B_h[tt], 34 + tt)

        steps = [(u, s) for u in range(8) for s in range(4)]
        gu_mm(0, 0, 0, 0)
        for k_, (u, s) in enumerate(steps):
            gu_post(u, s, 0, 0)
            gu_mm(u, s, 0, 1)
            gu_post(u, s, 0, 1)
            gu_mm(u, s, 1, 0)
            gu_post(u, s, 1, 0)
            gu_mm(u, s, 1, 1)
            gu_post(u, s, 1, 1)
            if k_ + 1 < len(steps):
                un, sn = steps[k_ + 1]
                gu_mm(un, sn, 0, 0)
            down(u, s)
            if s == 3 and u + NU < 8:
                load_unit(u + NU)
        dump_h("dbg_h3")

        S.barrier()
        R3.reset()
        yo = [R3.f32(1024) for _ in range(2)]
        B_yo = [Buf() for _ in range(2)]
        for tt in range(16):
            STT(yo[tt % 2], hres[:, tt, :], rstd_all[:, 34 + tt:35 + tt], fnorm_bc, ALU.mult, ALU.mult, [B_h[tt], brs_f[tt], Bc], [B_yo[tt % 2]])
            DMA("sp", y[tt * 128:(tt + 1) * 128, :], yo[tt % 2], [B_yo[tt % 2]], [])

        S.emit(nc, st)
    return nc


def _rel_bucket(rel):
    nb = 16
    max_exact = 8
    ret = (rel > 0).astype(np.int32) * nb
    n = np.abs(rel)
    nf = np.maximum(n, 1).astype(np.float32)
    large = max_exact + (np.log(nf / np.float32(max_exact)) / np.float32(math.log(128 / max_exact))
                         * np.float32(nb - max_exact)).astype(np.int32)
    large = np.minimum(large, nb - 1)
    return ret + np.where(n < max_exact, n, large)


_NC_CACHE = {}


def kernel(**inp):
    debug = int(inp.pop("_debug", 0)) if "_debug" in inp else 0
    f = lambda k: np.ascontiguousarray(np.asarray(inp[k], dtype=np.float32))
    x = f("x")
    mem = f("mem")
    i = np.arange(384)
    rel = 127 - i
    bk = _rel_bucket(rel.astype(np.int32))
    oh = np.zeros((32, 384), np.float32)
    oh[bk, i] = 1.0
    oh[15, :] -= 1.0
    kk = np.arange(128)[:, None]
    qq = np.arange(128)[None, :]
    maskT = np.where((kk < 64) | (qq >= 64), 0.0, NEG).astype(np.float32)
    ident = np.eye(128, dtype=np.float32)
    sel = np.zeros((16, 16, 128), np.float32)
    for e in range(16):
        sel[e, e, :] = 1.0
    sel = sel.reshape(16, 2048)
    gains = np.zeros((128, 40), np.float32)
    gains[:, 0:8] = f("attn_norm")[0].reshape(8, 128).T
    gains[:, 8:16] = f("cross_norm")[0].reshape(8, 128).T
    gains[:, 16:24] = f("mem_norm")[0].reshape(8, 128).T
    gains[:, 24:32] = f("ffn_norm")[0].reshape(8, 128).T
    gains[:, 32:36] = f("pool_scale")[0].reshape(4, 128).T
    gains[:, 36] = f("diff_subln")[0]
    wr = np.concatenate([f("router_group")[0], f("router_expert")[0].transpose(1, 0, 2).reshape(1024, 16)], axis=1)
    rbias = np.concatenate([f("router_group_bias")[0], f("router_expert_bias")[0].reshape(16)])[None, :]
    lamv = np.concatenate([f("lambda_q1")[0], f("lambda_k1")[0], f("lambda_q2")[0], f("lambda_k2")[0]])[None, :]
    shared = {
        "w_in": f("w_in")[0], "w_out": f("w_out")[0], "wq": f("wq_cross")[0], "wkv": f("wkv_cross")[0], "wo": f("wo_cross")[0],
        "w_gate": f("w_gate")[0], "w_up": f("w_up")[0], "w_down": f("w_down")[0], "pool_w": f("pool_w")[0],
        "wr": np.ascontiguousarray(wr), "rbias": np.ascontiguousarray(rbias), "rel_bias": f("rel_bias"),
        "lamv": np.ascontiguousarray(lamv), "gains": gains, "fnorm": f("final_norm")[None, :],
        "ident": ident, "oh": oh, "maskT": maskT, "sel": sel,
    }
    in_maps = []
    for c in range(8):
        b, j = c // 4, c % 4
        pad = 3 - j
        xs = np.zeros((8192, 1024), np.float32)
        xs[pad * 512:] = x[b, :(16 - pad) * 512]
        kvb = np.zeros((128, 16), np.float32)
        kvb[:, :pad] = NEG
        pinv = np.zeros((128, 4, 16), np.float32)
        for g in range(4):
            w = 2 ** (g + 1)
            if j == 0:
                pinv[:, g, :] = 1.0 / np.minimum(np.arange(1, 17), w)
            else:
                pinv[:, g, :] = 1.0 / w
        m = dict(shared)
        m.update({"x": xs, "mem": mem[b], "kvb": kvb, "pinv": pinv.reshape(128, 64)})
        in_maps.append(m)
    key = debug
    if key not in _NC_CACHE:
        _NC_CACHE[key] = build_nc(debug)
    nc = _NC_CACHE[key]
    res = run_bass_kernel_spmd(nc, in_maps, core_ids=list(range(8)))
    out = np.zeros((2, 8192, 1024), np.float32)
    extra = {}
    for c in range(8):
        b, j = c // 4, c % 4
        r = res.results[c]
        for s in range(4):
            t = 4 * s + j
            out[b, t * 512:(t + 1) * 512] = r["y"][s * 512:(s + 1) * 512]
        if debug:
            extra[c] = {k: v for k, v in r.items() if k.startswith("dbg")}
    if debug:
        return out, extra
    return out
```

```python
import math
import numpy as np
from contextlib import ExitStack
import concourse.bass as bass
import concourse.mybir as mybir
from concourse.bass_utils import run_bass_kernel_spmd

F32 = mybir.dt.float32
BF16 = mybir.dt.bfloat16
AF = mybir.ActivationFunctionType
ALU = mybir.AluOpType
AX = mybir.AxisListType

COMPUTE = ("pe", "act", "dve", "pool")
ENGS = ("pe", "act", "dve", "pool", "sp")
NEG = -30000.0


class Buf:
    __slots__ = ("name", "writer", "rd_eng", "rd_dma")

    def __init__(self, name=""):
        self.name = name
        self.writer = None
        self.rd_eng = {}
        self.rd_dma = []


class Op:
    __slots__ = ("eng", "idx", "fn", "waits", "signal", "num", "dma", "dma_i", "clock", "slotwait")


class Sched:
    def __init__(self, K=8):
        self.ops = {e: [] for e in ENGS}
        self.known = {e: {c: -1 for c in COMPUTE} for e in ENGS}
        self.dma_known = {e: set() for e in ENGS}
        self.ndma = {e: 0 for e in ENGS}
        self.dma_ops = {e: [] for e in ENGS}
        self.bar = {e: [] for e in ENGS}
        self.K = K

    def barrier(self):
        lasts = []
        for e in COMPUTE:
            for op in reversed(self.ops[e]):
                if not op.dma:
                    lasts.append(op)
                    break
        for e in ENGS:
            self.bar[e] = list(lasts)

    def add(self, eng, fn, reads=(), writes=(), dma=False):
        op = Op()
        op.eng = eng
        op.idx = len(self.ops[eng])
        op.fn = fn
        op.dma = dma
        op.signal = False
        op.num = None
        op.dma_i = None
        op.slotwait = None
        deps = []
        if self.bar[eng]:
            deps.extend(d for d in self.bar[eng] if not (d.eng == eng and eng == "pe"))
            self.bar[eng] = []
        for b in reads:
            if b.writer is not None:
                deps.append(b.writer)
        for b in writes:
            if b.writer is not None:
                deps.append(b.writer)
            deps.extend(b.rd_eng.values())
            deps.extend(b.rd_dma)
        known = self.known[eng]
        best = {}
        dwaits = []
        for d in deps:
            if d.dma:
                if id(d) not in self.dma_known[eng]:
                    self.dma_known[eng].add(id(d))
                    dwaits.append(d)
            else:
                if d.eng == eng and eng == "pe":
                    continue
                if known[d.eng] >= d.idx:
                    continue
                if d.eng not in best or best[d.eng].idx < d.idx:
                    best[d.eng] = d
        waits = list(best.values()) + dwaits
        for d in waits:
            d.signal = True
            for c in COMPUTE:
                if d.clock[c] > known[c]:
                    known[c] = d.clock[c]
            if not d.dma and d.idx > known[d.eng]:
                known[d.eng] = d.idx
        if dma:
            i = self.ndma[eng]
            op.dma_i = i
            self.ndma[eng] = i + 1
            if i >= self.K:
                prev = self.dma_ops[eng][i - self.K]
                op.slotwait = prev
                self.dma_known[eng].add(id(prev))
            self.dma_ops[eng].append(op)
        op.waits = waits
        op.clock = dict(known)
        if not dma and eng in COMPUTE:
            op.clock[eng] = op.idx
        for b in reads:
            if dma:
                b.rd_dma.append(op)
            else:
                b.rd_eng[eng] = op
        for b in writes:
            b.writer = op
            b.rd_eng = {}
            b.rd_dma = []
        self.ops[eng].append(op)
        return op

    def emit(self, nc, stack):
        sem_eng = {e: stack.enter_context(nc.semaphore("s_" + e)) for e in COMPUTE}
        sem_dma = {e: [stack.enter_context(nc.semaphore("d_%s%d" % (e, k))) for k in range(self.K)]
                   for e in ENGS if self.ndma[e] > 0}
        for e in COMPUTE:
            n = 0
            for op in self.ops[e]:
                if op.signal and not op.dma:
                    n += 1
                    op.num = n
        K = self.K

        def dma_target(d):
            return sem_dma[d.eng][d.dma_i % K], 16 * (d.dma_i // K + 1)

        def run(ename, e):
            for op in self.ops[ename]:
                if op.slotwait is not None:
                    s, v = dma_target(op.slotwait)
                    e.wait_ge(s, v)
                for d in op.waits:
                    if d.dma:
                        s, v = dma_target(d)
                        e.wait_ge(s, v)
                    else:
                        e.wait_ge(sem_eng[d.eng], d.num)
                ins = op.fn(e)
                if op.dma:
                    s, v = dma_target(op)
                    ins.then_inc(s, 16)
                elif op.signal:
                    ins.then_inc(sem_eng[ename], 1)
            for d in self.dma_ops[ename][-K:]:
                s, v = dma_target(d)
                e.wait_ge(s, v)

        block = stack.enter_context(nc.Block())

        @block.tensor
        def _(e):
            run("pe", e)

        @block.scalar
        def _(e):
            run("act", e)

        @block.vector
        def _(e):
            run("dve", e)

        @block.gpsimd
        def _(e):
            run("pool", e)

        @block.sync
        def _(e):
            run("sp", e)


class Arena:
    def __init__(self, nc, st, name, nbytes):
        self.t32 = st.enter_context(nc.sbuf_tensor(name, [128, nbytes // 4], F32))
        self.t16 = self.t32.bitcast(BF16)
        self.nbytes = nbytes
        self.off = 0

    def reset(self, off=0):
        self.off = off

    def f32(self, n):
        o = self.off
        self.off += 4 * n
        assert self.off <= self.nbytes, (self.off, self.nbytes)
        return self.t32[:, o // 4:o // 4 + n]

    def bf16(self, n):
        o = self.off
        self.off += 2 * n
        self.off = (self.off + 3) // 4 * 4
        assert self.off <= self.nbytes, (self.off, self.nbytes)
        return self.t16[:, o // 2:o // 2 + n]


def build_nc(debug=0):
    nc = bass.Bass("TRN2", target_bir_lowering=False)

    def din(name, shape):
        return nc.dram_tensor(name, shape, F32, kind="ExternalInput")

    x_t = din("x", [8192, 1024]); x = x_t.ap()
    mem = din("mem", [256, 1024]).ap()
    w_in = din("w_in", [1024, 2048]).ap()
    w_out = din("w_out", [1024, 1024]).ap()
    wq = din("wq", [1024, 1024]).ap()
    wkv = din("wkv", [1024, 2048]).ap()
    wo = din("wo", [1024, 1024]).ap()
    w_gate = din("w_gate", [16, 1024, 256]).ap()
    w_up = din("w_up", [16, 1024, 256]).ap()
    w_down = din("w_down", [16, 256, 1024]).ap()
    pool_w = din("pool_w", [4, 128, 128]).ap()
    wr = din("wr", [1024, 20]).ap()
    rbias_t = din("rbias", [1, 20])
    rel_bias = din("rel_bias", [32, 4]).ap()
    lamv_t = din("lamv", [1, 256])
    gains_d = din("gains", [128, 40]).ap()
    fnorm_t = din("fnorm", [1, 1024])
    ident_d = din("ident", [128, 128]).ap()
    oh_d = din("oh", [32, 384]).ap()
    maskT_d = din("maskT", [128, 128]).ap()
    kvb_d = din("kvb", [128, 16]).ap()
    pinv_d = din("pinv", [128, 64]).ap()
    sel_d = din("sel", [16, 2048]).ap()
    y = nc.dram_tensor("y", [2048, 1024], F32, kind="ExternalOutput").ap()
    E_t = nc.dram_tensor("Escr", [4, 128, 384], F32, kind="Internal")
    dbg = {}
    if debug:
        for nm in ("dbg_h1", "dbg_h2", "dbg_h3"):
            dbg[nm] = nc.dram_tensor(nm, [2048, 1024], F32, kind="ExternalOutput").ap()
        dbg["dbg_mix"] = nc.dram_tensor("dbg_mix", [128, 8 * 2048], BF16, kind="ExternalOutput").ap()

    S = Sched()
    st = ExitStack()
    with st:
        G = Arena(nc, st, "G", 18 * 1024)
        ident = G.bf16(128)
        ones_bf = G.bf16(128)
        ones_f = G.f32(128)
        gains = G.f32(40)
        gains_t = G.t32
        gains_off = gains.offset
        identf = G.f32(128)
        fnorm_bc = G.f32(1024)
        rbias_bc = G.f32(20)
        lam_sb = G.f32(256)
        small = G.f32(64)
        relb = G.f32(4)
        oh_sb = G.f32(384)
        g_sb = G.f32(384)
        g_off = g_sb.offset
        maskT = G.f32(128)
        kvb = G.f32(16)
        pinv = G.f32(64)
        biasT = G.f32(1024).rearrange("p (h t q) -> p h t q", h=4, t=2)
        rstd_all = G.f32(64)
        ssq_all = G.f32(64)
        lnv = G.f32(8)
        FP8 = mybir.dt.float8e4
        g8 = G.t32.bitcast(FP8)
        junks = [g8[:, G.off + 1024 * k:G.off + 1024 * (k + 1)] for k in range(2)]
        G.off += 2048
        B_junk = [Buf() for _ in range(2)]
        jctr = [0]

        def SQ(src, Bsrc, accum, wr_):
            k = jctr[0] % 2
            jctr[0] += 1
            return S.add("act", lambda e: e.activation(junks[k], src, AF.Square, accum_out=accum, saturate=False),
                         Bsrc, wr_ + [B_junk[k]])
        lnv_ctr = [0]
        R1 = Arena(nc, st, "R1", 32 * 1024)
        R2 = Arena(nc, st, "R2", 64 * 1024)
        R3 = Arena(nc, st, "R3", 92 * 1024)
        ps_all = st.enter_context(nc.psum_tensor("ps_all", [128, 4096], F32))
        psb_all = ps_all.bitcast(BF16)
        ps = [ps_all[:, i * 512:(i + 1) * 512] for i in range(8)]
        psb = [psb_all[:, i * 1024:(i + 1) * 1024] for i in range(8)]
        Bp = [Buf("ps%d" % i) for i in range(8)]

        mixT = R1.t16[:, 0:16384].rearrange("p (c n) -> p c n", c=8)
        hT = mixT
        KT = R2.t16[:, 0:16384].rearrange("p (h n) -> p h n", h=2)
        Vv = R2.t16[:, 16384:32768].rearrange("p (k n) -> p k n", k=64)
        hres = R2.t32[:, 0:16384].rearrange("p (t n) -> p t n", t=16)

        def MM(out, lhsT, rhs, start, stop, rd, wr_):
            return S.add("pe", lambda e: e.matmul(out, lhsT, rhs, start=start, stop=stop), rd, wr_)

        def TR(out, in_, rd, wr_):
            return S.add("pe", lambda e: e.transpose(out, in_, ident), rd, wr_)

        def ACT(out, in_, func, rd, wr_, **kw):
            return S.add("act", lambda e: e.activation(out, in_, func, **kw), rd, wr_)

        def TT(eng, out, in0, in1, op, rd, wr_):
            return S.add(eng, lambda e: e.tensor_tensor(out, in0, in1, op), rd, wr_)

        def TS(eng, out, in0, s1, s2, op0, op1, rd, wr_):
            if op1 is None:
                return S.add(eng, lambda e: e.tensor_scalar(out, in0, s1, None, op0), rd, wr_)
            return S.add(eng, lambda e: e.tensor_scalar(out, in0, s1, s2, op0, op1), rd, wr_)

        def STT(out, in0, scalar, in1, op0, op1, rd, wr_):
            return S.add("dve", lambda e: e.scalar_tensor_tensor(out, in0, scalar, in1, op0, op1), rd, wr_)

        def CP(eng, out, in_, rd, wr_):
            if eng == "act":
                return S.add("act", lambda e: e.copy(out, in_), rd, wr_)
            return S.add(eng, lambda e: e.tensor_copy(out, in_), rd, wr_)

        def RECIP(out, in_, rd, wr_):
            return S.add("dve", lambda e: e.reciprocal(out, in_), rd, wr_)

        def DMA(q, out, in_, rd, wr_):
            return S.add(q, lambda e: e.dma_start(out=out, in_=in_), rd, wr_, dma=True)

        def gain_bc(c0):
            return bass.AP(gains_t, gains_off + c0, [[G.nbytes // 4, 128], [1, 8], [0, 128]])

        def gcol(c):
            return gains[:, c:c + 1]

        Bc = Buf("consts")
        B_g = Buf("g_sb")
        B_E = Buf("E")
        B_bias = Buf("biasT")
        DMA("pool", ident, ident_d, [], [Bc])
        DMA("sp", identf, ident_d, [], [Bc])
        DMA("sp", gains, gains_d, [], [Bc])
        DMA("sp", fnorm_bc, bass.AP(fnorm_t, 0, [[0, 128], [1, 1024]]), [], [Bc])
        DMA("sp", rbias_bc, bass.AP(rbias_t, 0, [[0, 128], [1, 20]]), [], [Bc])
        DMA("sp", lam_sb, bass.AP(lamv_t, 0, [[0, 128], [1, 256]]), [], [Bc])
        DMA("sp", relb[0:32, :], rel_bias, [], [Bc])
        DMA("sp", oh_sb[0:32, :], oh_d, [], [Bc])
        DMA("sp", maskT, maskT_d, [], [Bc])
        DMA("sp", kvb, kvb_d, [], [Bc])
        DMA("sp", pinv, pinv_d, [], [Bc])
        S.add("dve", lambda e: e.memset(ones_bf, 1.0), [], [Bc])
        S.add("dve", lambda e: e.memset(ones_f, 1.0 / 128.0), [], [Bc])
        prod = G.f32(128)
        TT("dve", prod[:, 0:64], lam_sb[:, 0:64], lam_sb[:, 64:128], ALU.mult, [Bc], [Bc])
        TT("dve", prod[:, 64:128], lam_sb[:, 128:192], lam_sb[:, 192:256], ALU.mult, [Bc], [Bc])
        S.add("dve", lambda e: e.reduce_sum(small[:, 0:1], prod[:, 0:64], axis=AX.X), [Bc], [Bc])
        S.add("dve", lambda e: e.reduce_sum(small[:, 1:2], prod[:, 64:128], axis=AX.X), [Bc], [Bc])
        ACT(small[:, 2:4], small[:, 0:2], AF.Exp, [Bc], [Bc])
        TT("dve", small[:, 4:5], small[:, 3:4], small[:, 2:3], ALU.subtract, [Bc], [Bc])
        TS("dve", small[:, 4:5], small[:, 4:5], -0.2, None, ALU.add, None, [Bc], [Bc])
        TS("dve", small[:, 5:6], gcol(36), 0.8, None, ALU.mult, None, [Bc], [Bc])
        neg_lam = small[:, 4:5]
        subcol = small[:, 5:6]
        def bias_part1():
            MM(ps[7][0:4, 0:384], relb[0:32, 0:4], oh_sb[0:32, 0:384], True, True, [Bc], [Bp[7]])
            CP("dve", g_sb[0:4, :], ps[7][0:4, 0:384], [Bp[7]], [B_g])

        def bias_part1b():
            DMA("sp", E_t.ap(), bass.AP(G.t32, g_off, [[G.nbytes // 4, 4], [0, 128], [1, 384]]), [B_g], [B_E])

        def bias_part2():
            DMA("sp", biasT[:, :, 0, :], bass.AP(E_t, 127, [[383, 128], [128 * 384, 4], [1, 128]]), [B_E], [B_bias])
            DMA("sp", biasT[:, :, 1, :], bass.AP(E_t, 255, [[383, 128], [128 * 384, 4], [1, 128]]), [B_E], [B_bias])

        def bias_part3():
            mask_bc = bass.AP(G.t32, maskT.offset, [[G.nbytes // 4, 128], [0, 4], [1, 128]])
            TT("dve", biasT[:, :, 0, :], biasT[:, :, 0, :], mask_bc, ALU.add, [B_bias, Bc], [B_bias])

        def norm_transpose(src, Bsrc, rstd_col, gain_c0, dst, Bdst, xs, Bxs, bank, Brs):
            ACT(xs, src, AF.Copy, [Bsrc, Brs], [Bxs], scale=rstd_col)
            for c in range(8):
                TR(psb[bank][:, c * 128:(c + 1) * 128], xs[:, c * 128:(c + 1) * 128], [Bxs, Bc], [Bp[bank]])
            TT("dve", dst, psb[bank][:, 0:1024].rearrange("p (c n) -> p c n", c=8), gain_bc(gain_c0), ALU.mult,
               [Bp[bank], Bc], [Bdst])

        B_lnc = [Buf() for _ in range(8)]

        def rms_cols(src, Bsrc, col):
            k = lnv_ctr[0] % 8
            lnv_ctr[0] += 1
            b1 = Buf()
            b3 = Buf()
            SQ(src, [Bsrc], ssq_all[:, col:col + 1], [b1])
            ACT(lnv[:, k:k + 1], ssq_all[:, col:col + 1], AF.Ln, [b1], [B_lnc[k]], scale=1.0 / 1024.0, bias=1e-6)
            ACT(rstd_all[:, col:col + 1], lnv[:, k:k + 1], AF.Exp, [B_lnc[k]], [b3], scale=-0.5)
            return b3
        brs_c = [None] * 16
        brs_d = [None] * 16
        brs_f = [None] * 16
        B_ssq = [Buf() for _ in range(64)]
        B_ln = [Buf() for _ in range(2)]
        B_rslot = [Buf() for _ in range(16)]
        B_mix = [[Buf("mix%d_%d" % (c, s)) for s in range(4)] for c in range(8)]

        for pr in range(2):
            S.barrier()
            R3.reset()
            wA = R3.bf16(8 * 768).rearrange("p (c n) -> p c n", c=8)
            B_wA = [Buf() for _ in range(3)]
            for part, c0 in enumerate((512 + 256 * pr, 1024 + 256 * pr, 256 * pr)):
                DMA("pool", wA[:, :, part * 256:(part + 1) * 256],
                    w_in[:, c0:c0 + 256].rearrange("(c p) n -> p c n", p=128), [], [B_wA[part]])
            if pr == 0:
                wU = R3.bf16(8 * 512).rearrange("p (c n) -> p c n", c=8)
                B_wU = Buf()
                DMA("pool", wU, w_in[:, 1536:2048].rearrange("(c p) n -> p c n", p=128), [], [B_wU])
                pw = R3.bf16(512).rearrange("p (g n) -> p g n", g=4)
                B_pw = Buf()
                DMA("pool", pw, pool_w.rearrange("g c d -> c g d"), [], [B_pw])
            QT = R3.bf16(2 * 2 * 2048).rearrange("p (h m n) -> p h m n", h=2, m=2)
            B_QT = [[Buf() for _ in range(4)] for _ in range(2)]
            B_QTz = Buf()
            S.add("pool", lambda e, QT=QT: e.memset(QT.rearrange("p h m n -> p (h m n)"), 0.0), [], [B_QTz])
            mark = R3.off
            xst = [R3.f32(1024) for _ in range(4)]
            if pr == 0:
                xst += [R1.t32[:, k * 1024:(k + 1) * 1024] for k in range(4)]
            else:
                xst += [R3.f32(1024) for _ in range(4)]
            B_xst = [Buf() for _ in range(8)]
            nxs = 3 if pr == 0 else 4
            xs_t = [R3.bf16(1024) for _ in range(nxs)]
            B_xs = [Buf() for _ in range(nxs)]
            hnT = [R3.bf16(8 * 512).rearrange("p (c n) -> p c n", c=8) for _ in range(2)]
            B_hnT = [[Buf() for _ in range(4)] for _ in range(2)]
            if pr == 0:
                uT = R3.f32(4 * 528).rearrange("p (g n) -> p g n", g=4)
                B_uT = [Buf() for _ in range(4)]
                B_uTp = [Buf() for _ in range(4)]
                ptmp = [R3.f32(528) for _ in range(2)]
                B_pt = [Buf() for _ in range(2)]
                dT = R3.bf16(4 * 512).rearrange("p (g n) -> p g n", g=4)
                B_dT = [Buf() for _ in range(4)]
            kq = [0]

            def kbank():
                b_ = 4 + kq[0] % 3
                kq[0] += 1
                return b_

            def emit_V(g, hb, tt):
                vb = 2 + g % 2
                for c in range(8):
                    MM(ps[vb][:, 0:256], hnT[hb][:, c, tt * 128:(tt + 1) * 128], wA[:, c, 256:512], c == 0, c == 7,
                       [B_hnT[hb][tt], B_wA[1]], [Bp[vb]])
                CP("act" if pr == 1 else "dve", Vv[:, g, :], ps[vb][:, 0:256], [Bp[vb]], [])

            def emit_slot(i, hb):
                own = (i % 4 == 3)
                s_own = i // 4
                allh = B_hnT[hb]
                for hc in range(2):
                    kb_ = kbank()
                    for c in range(8):
                        MM(ps[kb_][:, :], wA[:, c, hc * 128:(hc + 1) * 128], hnT[hb][:, c, :], c == 0, c == 7,
                           allh + [B_wA[0]], [Bp[kb_]])
                    CP("dve" if (hc or pr == 0) else "act", KT[:, hc, i * 512:(i + 1) * 512], ps[kb_][:, :], [Bp[kb_]], [])
                if own:
                    for hc in range(2):
                        kb_ = kbank()
                        for c in range(8):
                            MM(ps[kb_][:, :], wA[:, c, 512 + hc * 128:512 + (hc + 1) * 128], hnT[hb][:, c, :], c == 0, c == 7,
                               allh + [B_wA[2]], [Bp[kb_]])
                        TS("dve", QT[0:64, hc, 0, s_own * 512:(s_own + 1) * 512], ps[kb_][0:64, :], 0.125, None, ALU.mult, None,
                           [Bp[kb_], B_QTz], [B_QT[hc][s_own]])
                        TS("dve", QT[64:128, hc, 1, s_own * 512:(s_own + 1) * 512], ps[kb_][64:128, :], 0.125, None, ALU.mult, None,
                           [Bp[kb_], B_QTz, B_QT[hc][s_own]], [B_QT[hc][s_own]])
                if pr == 0 and i % 4 == 2:
                    for gg in range(4):
                        kb_ = kbank()
                        for c in range(8):
                            MM(ps[kb_][:, 0:16], wU[:, c, gg * 128:(gg + 1) * 128], hnT[hb][:, c, 496:512], c == 0, c == 7,
                               [B_hnT[hb][3], B_wU], [Bp[kb_]])
                        CP("dve", uT[:, gg, 0:16], ps[kb_][:, 0:16], [Bp[kb_]], [B_uTp[gg]])
                if pr == 0 and own:
                    for gg in range(4):
                        w = 2 ** (gg + 1)
                        kb_ = kbank()
                        for c in range(8):
                            MM(ps[kb_][:, :], wU[:, c, gg * 128:(gg + 1) * 128], hnT[hb][:, c, :], c == 0, c == 7,
                               allh + [B_wU], [Bp[kb_]])
                        CP("act", uT[:, gg, 16:528], ps[kb_][:, :], [Bp[kb_]], [B_uT[gg]])
                        U = uT[:, gg, :]
                        ru = [B_uT[gg], B_uTp[gg]]
                        TT("pool", ptmp[0][:, 1:528], U[:, 1:528], U[:, 0:527], ALU.add, ru, [B_pt[0]])
                        cur = 0
                        sh = 2
                        lo = 1
                        while sh < w:
                            lo += sh
                            TT("pool", ptmp[1 - cur][:, lo:528], ptmp[cur][:, lo:528], ptmp[cur][:, lo - sh:528 - sh], ALU.add,
                               [B_pt[cur]], [B_pt[1 - cur]])
                            cur = 1 - cur
                            sh *= 2
                        STT(dT[:, gg, :], ptmp[cur][:, 16:528], 1.0 / w, U[:, 16:528], ALU.mult, ALU.subtract,
                            [B_pt[cur]] + ru, [B_dT[gg]])
                        if s_own == 0:
                            tmp16 = small[:, 16:32]
                            TT("dve", tmp16, ptmp[cur][:, 16:32], pinv[:, gg * 16:(gg + 1) * 16], ALU.mult, [B_pt[cur], Bc], [Bc])
                            TT("dve", dT[:, gg, 0:16], tmp16, U[:, 16:32], ALU.subtract, [Bc] + ru + [B_dT[gg]], [B_dT[gg], Bc])

                    def stageB(s_own=s_own):
                        for gg in range(4):
                            kb2 = kbank()
                            MM(ps[kb2][:, :], pw[:, gg, :], dT[:, gg, :], True, True, [B_pw, B_dT[gg]], [Bp[kb2]])
                            TS("dve", mixT[:, 4 + gg, s_own * 512:(s_own + 1) * 512], ps[kb2][:, :], gcol(32 + gg), None, ALU.mult, None,
                               [Bp[kb2], Bc], [B_mix[4 + gg][s_own]])
                    late.append(stageB)

            def emit_load(g):
                xb = 4 * ((g // 4) % 2) + g % 4
                DMA("sp", xst[xb], x[g * 128:(g + 1) * 128, :], [], [B_xst[xb]])

            def emit_sq1(g):
                if pr == 0:
                    xb = 4 * ((g // 4) % 2) + g % 4
                    SQ(xst[xb], [B_xst[xb]], ssq_all[:, g:g + 1], [B_ssq[g]])

            def emit_stats(i, squares=True):
                for tt in range(4):
                    if squares:
                        emit_sq1(4 * i + tt)
                if pr == 0:
                    lv = lnv[:, 4 * (i % 2):4 * (i % 2) + 4]
                    ACT(lv, ssq_all[:, 4 * i:4 * i + 4], AF.Ln, B_ssq[4 * i:4 * i + 4], [B_ln[i % 2]], scale=1.0 / 1024.0, bias=1e-6)
                    ACT(rstd_all[:, 4 * i:4 * i + 4], lv, AF.Exp, [B_ln[i % 2]], [B_rslot[i]], scale=-0.5)

            pending = []
            late = []
            TB = (0, 1, 7)

            def partA(g):
                i_, tt_ = g // 4, g % 4
                xb = 4 * (i_ % 2) + tt_
                ACT(xs_t[g % nxs], xst[xb], AF.Copy, [B_xst[xb], B_rslot[i_]], [B_xs[g % nxs]], scale=rstd_all[:, g:g + 1])

            def partB(g):
                i_, tt_ = g // 4, g % 4
                hb_ = i_ % 2
                bank = TB[g % 3]
                xs = xs_t[g % nxs]
                for c in range(8):
                    TR(psb[bank][:, c * 128:(c + 1) * 128], xs[:, c * 128:(c + 1) * 128], [B_xs[g % nxs], Bc], [Bp[bank]])
                TT("dve", hnT[hb_][:, :, tt_ * 128:(tt_ + 1) * 128], psb[bank][:, 0:1024].rearrange("p (c n) -> p c n", c=8),
                   gain_bc(0), ALU.mult, [Bp[bank], Bc], [B_hnT[hb_][tt_]])

            for g in range(8):
                emit_load(g)
            emit_stats(0)
            emit_stats(1)
            partA(0)
            partA(1)
            emit_load(8)
            emit_load(9)
            for g in range(64):
                i, tt = g // 4, g % 4
                hb = i % 2
                if tt == 0:
                    run_late = late[:]
                    del late[:]
                if pr == 0 and g == 8:
                    bias_part1()
                if pr == 0 and g == 16:
                    bias_part1b()
                if pr == 0 and g == 24:
                    bias_part2()
                if pr == 0 and g == 40:
                    bias_part3()
                if g + 2 < 64:
                    partA(g + 2)
                if g + 10 < 64:
                    emit_load(g + 10)
                if g + 8 < 64:
                    emit_sq1(g + 8)
                    if tt == 3:
                        emit_stats(i + 2, squares=False)
                partB(g)
                cur_p = [lambda g=g, hb=hb, tt=tt: emit_V(g, hb, tt)]
                if tt == 3:
                    cur_p.append(lambda i=i, hb=hb: emit_slot(i, hb))
                if tt == 2:
                    cur_p.extend(run_late)
                pending.append(cur_p)
                if len(pending) > 2:
                    for f_ in pending.pop(0):
                        f_()
            for grp_ in pending:
                for f_ in grp_:
                    f_()
            for f_ in late:
                f_()
            pending = []
            late = []

            S.barrier()
            R3.reset(mark)
            NP = 3
            Pt = [R3.bf16(1024) for _ in range(NP)]
            B_P = [Buf() for _ in range(NP)]
            rs = [R3.f32(512) for _ in range(2)]
            tq = [R3.f32(512) for _ in range(2)]
            ocp = [R3.f32(512) for _ in range(2)]
            B_ocp = [Buf() for _ in range(2)]
            o_sb = R3.f32(512)
            sq_sb = R3.f32(512)
            r2_sb = R3.f32(512)
            B_fin = [Buf() for _ in range(8)]
            if pr == 1:
                TOP = R3.nbytes - 32 * 1024
                assert R3.off <= TOP, R3.off
                R3.reset(TOP)
                wO = R3.bf16(8 * 1024).rearrange("p (c n) -> p c n", c=8)
                wQ = R3.bf16(8 * 1024).rearrange("p (c n) -> p c n", c=8)
                B_wO = Buf()
                B_wQ = Buf()
                DMA("pool", wO, w_out.rearrange("(c p) n -> p c n", p=128), [], [B_wO])
                DMA("pool", wQ, wq.rearrange("(c p) n -> p c n", p=128), [], [B_wQ])
            flat = []
            for s in range(4):
                for hc in range(2):
                    nkb_ = 4 * (4 * s + 3 + 1)
                    for kb in range(nkb_):
                        flat.append((s, hc, kb, nkb_))
            n = len(flat)
            sbank = {}
            pbuf = {}
            rot = {"s": 0, "p": 0}

            def geom(t):
                s, hc, kb, nkb = flat[t]
                r = kb - 4 * (4 * s + 3)
                return s, hc, kb, nkb, r, 128 * max(r, 0)

            def QK(t):
                s, hc, kb, nkb, r, c0 = geom(t)
                b0 = 2 * (rot["s"] % 2)
                rot["s"] += 1
                sbank[t] = b0
                for m in range(2):
                    MM(ps[b0 + m][:, c0:512], KT[:, hc, kb * 128:(kb + 1) * 128],
                       QT[:, hc, m, s * 512 + c0:(s + 1) * 512], True, True,
                       [B_QT[hc][s]], [Bp[b0 + m]])

            def SOFT(t):
                s, hc, kb, nkb, r, c0 = geom(t)
                h = 2 * pr + hc
                b0 = sbank[t]
                if r >= -1:
                    if r == -1:
                        cs, nb_, boff = 0, 128, 128
                    elif r == 3:
                        cs, nb_, boff = 384, 128, 0
                    else:
                        cs, nb_, boff = 128 * r, 256, 0
                    pv_ = ps_all[:, b0 * 512:(b0 + 2) * 512].rearrange("p (b n) -> p b n", b=2)[:, :, cs:cs + nb_]
                    bia = bass.AP(G.t32, biasT.offset + h * 256 + boff, [[G.nbytes // 4, 128], [0, 2], [1, nb_]])
                    TT("dve", pv_, pv_, bia, ALU.add, [Bp[b0], Bp[b0 + 1], B_bias], [Bp[b0], Bp[b0 + 1]])
                pb = rot["p"] % NP
                rot["p"] += 1
                pbuf[t] = pb
                kw = {}
                if kb // 4 <= 2:
                    kw["bias"] = kvb[:, kb // 4:kb // 4 + 1]
                src = ps_all[:, b0 * 512:(b0 + 2) * 512].rearrange("p (b n) -> p b n", b=2)[:, :, c0:512]
                dst = Pt[pb].rearrange("p (b n) -> p b n", b=2)[:, :, c0:512]
                ACT(dst, src, AF.Exp, [Bp[b0], Bp[b0 + 1], Bc], [B_P[pb]], **kw)

            def PV(t):
                s, hc, kb, nkb, r, c0 = geom(t)
                pb = pbuf[t]
                first = (kb == 0)
                last = (kb == nkb - 1)
                for m in range(2):
                    Pm = Pt[pb][:, m * 512 + c0:(m + 1) * 512]
                    MM(ps[4 + m][:, c0:512], Vv[:, kb, hc * 128:(hc + 1) * 128], Pm, first, last,
                       [B_P[pb]], [Bp[4 + m]])
                    MM(ps[6 + m][:, c0:512], ones_bf, Pm, first, last, [B_P[pb], Bc], [Bp[6 + m]])

            def FIN_a(s, hc):
                RECIP(rs[0], ps[6][:, :], [Bp[6]], [B_fin[0]])
                CP("act", ocp[0], ps[4][:, :], [Bp[4]], [B_ocp[0]])
                RECIP(rs[1], ps[7][:, :], [Bp[7]], [B_fin[1]])
                CP("act", ocp[1], ps[5][:, :], [Bp[5]], [B_ocp[1]])
                TT("dve", tq[0], ocp[0], rs[0], ALU.mult, [B_ocp[0], B_fin[0]], [B_fin[2]])
                TT("dve", tq[1], ocp[1], rs[1], ALU.mult, [B_ocp[1], B_fin[1]], [B_fin[3]])
                STT(o_sb, tq[1], neg_lam, tq[0], ALU.mult, ALU.add, [B_fin[2], B_fin[3], Bc], [B_fin[4]])
                ACT(sq_sb, o_sb, AF.Square, [B_fin[4]], [B_fin[5]])

            def FIN_b(s, hc, bm):
                h = 2 * pr + hc
                MM(ps[bm][:, :], ones_f, sq_sb, True, True, [B_fin[5], Bc], [Bp[bm]])
                ACT(r2_sb, ps[bm][:, :], AF.Ln, [Bp[bm]], [B_fin[6]], bias=1e-6)
                ACT(r2_sb, r2_sb, AF.Exp, [B_fin[6]], [B_fin[6]], scale=-0.5)
                STT(mixT[:, h, s * 512:(s + 1) * 512], o_sb, subcol, r2_sb, ALU.mult, ALU.mult,
                    [B_fin[4], B_fin[6], Bc], [B_mix[h][s]])

            QK(0)
            QK(1)
            pend_fin = None
            for t in range(n):
                SOFT(t)
                if pend_fin is not None and (t >= pend_fin[2] or t == n - 1):
                    FIN_b(pend_fin[0], pend_fin[1], sbank[t])
                    pend_fin = None
                if t + 2 < n:
                    QK(t + 2)
                PV(t)
                s_, hc_, kb_l, nkb_l = flat[t]
                if kb_l == nkb_l - 1:
                    FIN_a(s_, hc_)
                    pend_fin = (s_, hc_, t + 4)
            if pend_fin is not None:
                FIN_b(pend_fin[0], pend_fin[1], 0)

        S.barrier()
        R3.reset()
        if debug:
            DMA("sp", dbg["dbg_mix"], R1.t16[:, 0:16384], [b for row in B_mix for b in row], [])
        wKV = R3.bf16(8 * 2048).rearrange("p (c n) -> p c n", c=8)
        mark_b = R3.off
        B_wKV = Buf()
        DMA("pool", wKV[:, :, 0:1024], wkv[:, 0:1024].rearrange("(c p) n -> p c n", p=128), [], [B_wKV])
        DMA("pool", wKV[:, :, 1024:2048], wkv[:, 1024:2048].rearrange("(c p) n -> p c n", p=128), [], [B_wKV])
        xo = [R3.f32(1024) for _ in range(2)]
        assert R3.off <= TOP
        B_xo = [Buf() for _ in range(2)]
        B_h = [Buf("h%d" % t) for t in range(16)]
        allmix = [b for row in B_mix for b in row]
        for tt in range(16):
            s_, t4 = tt // 4, tt % 4
            row0 = (4 * s_ + 3) * 512 + t4 * 128
            DMA("sp", xo[tt % 2], x[row0:row0 + 128, :], [], [B_xo[tt % 2]])
            for half in range(2):
                b = 2 * (tt % 2) + half
                for c in range(8):
                    MM(ps[b][:, :], mixT[:, c, tt * 128:(tt + 1) * 128], wO[:, c, half * 512:(half + 1) * 512], c == 0, c == 7,
                       [B_mix[c][s_], B_wO], [Bp[b]])
                TT("dve", hres[:, tt, half * 512:(half + 1) * 512], ps[b][:, :], xo[tt % 2][:, half * 512:(half + 1) * 512], ALU.add,
                   [Bp[b], B_xo[tt % 2]], [B_h[tt]])
            brs_c[tt] = rms_cols(hres[:, tt, :], B_h[tt], tt)

        def dump_h(name):
            if debug:
                for tt in range(16):
                    DMA("sp", dbg[name][tt * 128:(tt + 1) * 128, :], hres[:, tt, :], [B_h[tt]], [])

        dump_h("dbg_h1")

        S.barrier()
        R3.reset(mark_b)
        xs_t = [R3.bf16(1024) for _ in range(2)]
        B_xs = [Buf() for _ in range(2)]
        mnT = R3.bf16(8 * 256).rearrange("p (c n) -> p c n", c=8)
        B_mnT = [Buf() for _ in range(2)]
        KcT = R3.bf16(8 * 256).rearrange("p (c n) -> p c n", c=8)
        B_Kc = Buf()
        Vc = R3.bf16(2 * 1024).rearrange("p (k n) -> p k n", k=2)
        B_Vc = Buf()
        mark_c = R3.off
        mst = [R3.f32(1024) for _ in range(2)]
        B_mst = [Buf() for _ in range(2)]

        for mk in range(2):
            DMA("sp", mst[mk], mem[mk * 128:(mk + 1) * 128, :], [], [B_mst[mk]])
            brs = rms_cols(mst[mk], B_mst[mk], 32 + mk)
            norm_transpose(mst[mk], B_mst[mk], rstd_all[:, 32 + mk:33 + mk], 16, mnT[:, :, mk * 128:(mk + 1) * 128], B_mnT[mk],
                           xs_t[mk], B_xs[mk], mk, brs)
        for j8 in range(8):
            b = 4 + j8 % 4
            for c in range(8):
                MM(ps[b][:, 0:256], wKV[:, c, j8 * 128:(j8 + 1) * 128], mnT[:, c, :], c == 0, c == 7, B_mnT + [B_wKV], [Bp[b]])
            CP("dve" if j8 % 2 else "act", KcT[:, j8, :], ps[b][:, 0:256], [Bp[b]], [B_Kc])
        for mk in range(2):
            for half in range(2):
                b = 4 + (2 * mk + half) % 4
                for c in range(8):
                    MM(ps[b][:, :], mnT[:, c, mk * 128:(mk + 1) * 128], wKV[:, c, 1024 + half * 512:1024 + (half + 1) * 512], c == 0, c == 7,
                       B_mnT + [B_wKV], [Bp[b]])
                CP("dve" if half else "act", Vc[:, mk, half * 512:(half + 1) * 512], ps[b][:, :], [Bp[b]], [B_Vc])
        B_hT = [[Buf() for _ in range(4)] for _ in range(4)]
        for tt in range(16):
            norm_transpose(hres[:, tt, :], B_h[tt], rstd_all[:, tt:tt + 1], 8, hT[:, :, tt * 128:(tt + 1) * 128], B_hT[tt // 4][tt % 4],
                           xs_t[tt % 2], B_xs[tt % 2], tt % 2, brs_c[tt])
        S.barrier()
        wOc = wKV[:, :, 0:1024]
        B_wOc = Buf()
        DMA("pool", wOc, wo.rearrange("(c p) n -> p c n", p=128), [], [B_wOc])
        R3.reset(mark_c)
        qT = R3.bf16(8 * 512).rearrange("p (c n) -> p c n", c=8)
        B_qT = [Buf() for _ in range(8)]
        ocT = R3.bf16(8 * 512).rearrange("p (c n) -> p c n", c=8)
        B_oc = [Buf() for _ in range(8)]
        Pc = [R3.bf16(512) for _ in range(4)]
        B_Pc = [Buf() for _ in range(4)]
        rsc2 = [R3.f32(512) for _ in range(2)]
        B_rsc2 = [Buf() for _ in range(2)]
        pc_rot = 0
        for s in range(4):
            for j8 in range(8):
                b = j8 % 2
                for c in range(8):
                    MM(ps[b][:, :], wQ[:, c, j8 * 128:(j8 + 1) * 128], hT[:, c, s * 512:(s + 1) * 512], c == 0, c == 7,
                       B_hT[s] + [B_wQ], [Bp[b]])
                if j8 % 2:
                    TS("dve", qT[:, j8, :], ps[b][:, :], 1.0 / 16.0, None, ALU.mult, None, [Bp[b]], [B_qT[j8]])
                else:
                    ACT(qT[:, j8, :], ps[b][:, :], AF.Copy, [Bp[b]], [B_qT[j8]], scale=1.0 / 16.0)
            pcs_h = {}

            def c_scores(hh):
                nonlocal pc_rot
                pcs = []
                for mk in range(2):
                    b = 2 + mk
                    for e2 in range(2):
                        MM(ps[b][:, :], KcT[:, 2 * hh + e2, mk * 128:(mk + 1) * 128], qT[:, 2 * hh + e2, :], e2 == 0, e2 == 1,
                           [B_Kc, B_qT[2 * hh + e2]], [Bp[b]])
                    pi = pc_rot % 4
                    pc_rot += 1
                    pcs.append(pi)
                    ACT(Pc[pi], ps[b][:, :], AF.Exp, [Bp[b]], [B_Pc[pi]])
                pcs_h[hh] = pcs

            def c_pv(hh):
                pcs = pcs_h[hh]
                st_ = hh % 2
                ob = (4, 5) if st_ == 0 else (0, 1)
                sb_ = 6 if st_ == 0 else 7
                for e2 in range(2):
                    b = ob[e2]
                    for mk in range(2):
                        MM(ps[b][:, :], Vc[:, mk, (2 * hh + e2) * 128:(2 * hh + e2 + 1) * 128], Pc[pcs[mk]], mk == 0, mk == 1,
                           [B_Vc, B_Pc[pcs[mk]]], [Bp[b]])
                for mk in range(2):
                    MM(ps[sb_][:, :], ones_bf, Pc[pcs[mk]], mk == 0, mk == 1, [Bc, B_Pc[pcs[mk]]], [Bp[sb_]])
                RECIP(rsc2[st_], ps[sb_][:, :], [Bp[sb_]], [B_rsc2[st_]])
                for e2 in range(2):
                    TT("dve", ocT[:, 2 * hh + e2, :], ps[ob[e2]][:, :], rsc2[st_], ALU.mult, [Bp[ob[e2]], B_rsc2[st_]], [B_oc[2 * hh + e2]])

            c_scores(0)
            for hh in range(4):
                if hh + 1 < 4:
                    c_scores(hh + 1)
                c_pv(hh)
            for t4 in range(4):
                tt = 4 * s + t4
                for half in range(2):
                    b = half
                    for c in range(8):
                        MM(ps[b][:, :], ocT[:, c, t4 * 128:(t4 + 1) * 128], wOc[:, c, half * 512:(half + 1) * 512], c == 0, c == 7,
                           [B_oc[c], B_wOc], [Bp[b]])
                    TT("dve", hres[:, tt, half * 512:(half + 1) * 512], ps[b][:, :], hres[:, tt, half * 512:(half + 1) * 512], ALU.add,
                       [Bp[b], B_h[tt]], [B_h[tt]])
                brs_d[tt] = rms_cols(hres[:, tt, :], B_h[tt], 16 + tt)
        dump_h("dbg_h2")

        S.barrier()
        R3.reset()
        NU = 2
        ring = []
        for k in range(NU):
            unit = []
            for _e in range(2):
                wg_ = R3.bf16(8 * 256).rearrange("p (c n) -> p c n", c=8)
                wu_ = R3.bf16(8 * 256).rearrange("p (c n) -> p c n", c=8)
                wd_ = R3.bf16(2 * 1024).rearrange("p (f n) -> p f n", f=2)
                unit.append((wg_, wu_, wd_, Buf(), Buf(), Buf()))
            ring.append(unit)

        def load_unit(u):
            for e2 in range(2):
                e = 2 * u + e2
                wg_, wu_, wd_, b1, b2, b3 = ring[u % NU][e2]
                DMA("pool", wg_, w_gate[e].rearrange("(c p) n -> p c n", p=128), [], [b1])
                DMA("pool", wu_, w_up[e].rearrange("(c p) n -> p c n", p=128), [], [b2])
                DMA("pool", wd_, w_down[e].rearrange("(f p) n -> p f n", p=128), [], [b3])

        wR = R3.bf16(8 * 20).rearrange("p (c n) -> p c n", c=8)
        B_wR = Buf()
        DMA("pool", wR, wr.rearrange("(c p) n -> p c n", p=128), [], [B_wR])
        for u in range(NU):
            load_unit(u)
        selT = R3.bf16(2048)
        B_sel = Buf()
        DMA("pool", selT[0:16, :], sel_d, [], [B_sel])
        chi = R3.bf16(2048)
        clo = R3.bf16(2048)
        combT = R3.f32(2048)
        B_comb = [Buf() for _ in range(16)]
        rt_n = 16 * (20 + 16 + 4 * 8 + 16 + 9)
        rt = R3.f32(rt_n)
        B_rt = Buf()
        B_lg = [Buf() for _ in range(16)]
        _o = [0]

        def rtv(n_inner):
            o = _o[0]
            _o[0] += 16 * n_inner
            v = rt[:, o:o + 16 * n_inner]
            return v if n_inner == 1 else v.rearrange("p (t k) -> p t k", t=16)

        lg = rtv(20); comb = rtv(16); goh = rtv(4); gex = rtv(4); esel = rtv(4); oh1 = rtv(4); es2 = rtv(4); oh2 = rtv(4)
        inner = rtv(4); gsc = rtv(4); prod16 = rtv(16)
        gmax = rtv(1); gsum = rtv(1); gw = rtv(1); m1 = rtv(1); m2 = rtv(1); e21 = rtv(1); den = rtv(1); w1 = rtv(1); w2 = rtv(1)
        prod4 = prod16.rearrange("p t (g e) -> p t g e", g=4)
        comb4 = comb.rearrange("p t (g e) -> p t g e", g=4)

        def bcl(v, n):
            return bass.AP(v.tensor, v.offset, [list(d) for d in v.ap] + [[0, n]])

        def bcm(v, n):
            dd = [list(d) for d in v.ap]
            return bass.AP(v.tensor, v.offset, dd[:-1] + [[0, n]] + dd[-1:])

        actT = R3.bf16(2 * 2 * 512).rearrange("p (e f n) -> p e f n", e=2, f=2)
        B_act = [[Buf() for _ in range(2)] for _ in range(2)]
        sg = [R3.f32(512) for _ in range(2)]
        B_sg = [Buf() for _ in range(2)]
        tg = [R3.f32(512) for _ in range(2)]
        B_tg = [Buf() for _ in range(2)]
        bcs = [R3.f32(512) for _ in range(2)]
        B_bcs = [Buf() for _ in range(2)]
        xs_t = [tg[0].bitcast(BF16), tg[1].bitcast(BF16)]
        B_xs = [Buf() for _ in range(2)]
        B_h3T = [[Buf() for _ in range(4)] for _ in range(4)]
        for tt in range(16):
            norm_transpose(hres[:, tt, :], B_h[tt], rstd_all[:, 16 + tt:17 + tt], 24, hT[:, :, tt * 128:(tt + 1) * 128], B_h3T[tt // 4][tt % 4],
                           xs_t[tt % 2], B_xs[tt % 2], tt % 2, brs_d[tt])
            for c in range(8):
                MM(ps[2 + tt % 2][:, 0:20], hT[:, c, tt * 128:(tt + 1) * 128], wR[:, c, :], c == 0, c == 7,
                   [B_h3T[tt // 4][tt % 4], B_wR], [Bp[2 + tt % 2]])
            TT("dve", lg[:, tt, :], ps[2 + tt % 2][:, 0:20], rbias_bc, ALU.add, [Bp[2 + tt % 2], Bc], [B_lg[tt]])

        R_ = [B_rt]
        lgg = lg[:, :, 0:4]
        le4 = lg[:, :, 4:20].rearrange("p t (g e) -> p t g e", g=4)
        S.add("dve", lambda e: e.tensor_reduce(gmax, lgg, axis=AX.X, op=ALU.max), B_lg, R_)
        TT("dve", goh, lgg, bcl(gmax, 4), ALU.is_equal, B_lg + R_, R_)
        TT("dve", gex, lgg, bcl(gmax, 4), ALU.subtract, B_lg + R_, R_)
        ACT(gex, gex, AF.Exp, R_, R_)
        S.add("dve", lambda e: e.tensor_reduce(gsum, gex, axis=AX.X, op=ALU.add), R_, R_)
        RECIP(gw, gsum, R_, R_)
        TT("dve", prod4, le4, bcl(goh, 4), ALU.mult, B_lg + R_, R_)
        S.add("dve", lambda e: e.tensor_reduce(esel, prod4.rearrange("p t g e -> p t e g"), axis=AX.X, op=ALU.add), R_, R_)
        S.add("dve", lambda e: e.tensor_reduce(m1, esel, axis=AX.X, op=ALU.max), R_, R_)
        TT("dve", oh1, esel, bcl(m1, 4), ALU.is_equal, R_, R_)
        STT(es2, oh1, -1e30, esel, ALU.mult, ALU.add, R_, R_)
        S.add("dve", lambda e: e.tensor_reduce(m2, es2, axis=AX.X, op=ALU.max), R_, R_)
        TT("dve", oh2, es2, bcl(m2, 4), ALU.is_equal, R_, R_)
        TT("dve", e21, m2, m1, ALU.subtract, R_, R_)
        ACT(e21, e21, AF.Exp, R_, R_)
        TS("dve", den, e21, 1.0, None, ALU.add, None, R_, R_)
        RECIP(w1, den, R_, R_)
        TT("dve", w2, e21, w1, ALU.mult, R_, R_)
        TT("dve", inner, oh1, bcl(w1, 4), ALU.mult, R_, R_)
        TT("dve", oh2, oh2, bcl(w2, 4), ALU.mult, R_, R_)
        TT("dve", inner, inner, oh2, ALU.add, R_, R_)
        TT("dve", gsc, goh, bcl(gw, 4), ALU.mult, R_, R_)
        TT("dve", comb4, bcl(gsc, 4), bcm(inner, 4), ALU.mult, R_, R_)
        for q4 in range(4):
            pb_ = 2 + q4 % 2
            for t4 in range(4):
                tt = 4 * q4 + t4
                S.add("pe", lambda e, tt=tt, t4=t4, pb_=pb_: e.transpose(ps[pb_][0:16, t4 * 128:(t4 + 1) * 128], comb[:, tt, :], identf),
                      [B_rt, Bc], [Bp[pb_]])
            CP("dve", combT[0:16, q4 * 512:(q4 + 1) * 512], ps[pb_][0:16, :], [Bp[pb_]], B_comb[4 * q4:4 * q4 + 4])
            CP("dve", chi[0:16, q4 * 512:(q4 + 1) * 512], combT[0:16, q4 * 512:(q4 + 1) * 512], B_comb[4 * q4:4 * q4 + 4], B_comb[4 * q4:4 * q4 + 4])
            TT("dve", clo[0:16, q4 * 512:(q4 + 1) * 512], combT[0:16, q4 * 512:(q4 + 1) * 512], chi[0:16, q4 * 512:(q4 + 1) * 512], ALU.subtract,
               B_comb[4 * q4:4 * q4 + 4], B_comb[4 * q4:4 * q4 + 4])

        d_rot = [0]

        def gu_mm(u, s, e2, f):
            wg_, wu_, wd_, b1, b2, b3 = ring[u % NU][e2]
            bg = 2 * f
            bu = 2 * f + 1
            for c in range(8):
                MM(ps[bg][:, :], wg_[:, c, f * 128:(f + 1) * 128], hT[:, c, s * 512:(s + 1) * 512], c == 0, c == 7,
                   B_h3T[s] + [b1], [Bp[bg]])
            for c in range(8):
                MM(ps[bu][:, :], wu_[:, c, f * 128:(f + 1) * 128], hT[:, c, s * 512:(s + 1) * 512], c == 0, c == 7,
                   B_h3T[s] + [b2], [Bp[bu]])

        def gu_post(u, s, e2, f):
            e = 2 * u + e2
            bi = e2
            bg = 2 * f
            bu = 2 * f + 1
            if f == 0:
                MM(ps[6][:, :], selT[0:16, e * 128:(e + 1) * 128], chi[0:16, s * 512:(s + 1) * 512], True, False,
                   [B_sel] + B_comb[4 * s:4 * s + 4], [Bp[6]])
                MM(ps[6][:, :], selT[0:16, e * 128:(e + 1) * 128], clo[0:16, s * 512:(s + 1) * 512], False, True,
                   [B_sel] + B_comb[4 * s:4 * s + 4], [Bp[6]])
                CP("act", bcs[bi], ps[6][:, :], [Bp[6]], [B_bcs[bi]])
            ACT(sg[f], ps[bg][:, :], AF.Silu, [Bp[bg]], [B_sg[f]])
            TT("dve", tg[f], ps[bu][:, :], sg[f], ALU.mult, [Bp[bu], B_sg[f]], [B_tg[f]])
            TT("pool", actT[:, e2, f, :], tg[f], bcs[bi], ALU.mult, [B_tg[f], B_bcs[bi]], [B_act[e2][f]])

        def down(u, s):
            unit = ring[u % NU]
            for t4 in range(4):
                tt = 4 * s + t4
                for half in range(2):
                    b = 4 + d_rot[0] % 2
                    d_rot[0] += 1
                    k = 0
                    for e2 in range(2):
                        wd_, b3 = unit[e2][2], unit[e2][5]
                        for f in range(2):
                            MM(ps[b][:, :], actT[:, e2, f, t4 * 128:(t4 + 1) * 128], wd_[:, f, half * 512:(half + 1) * 512],
                               k == 0, k == 3, [B_act[e2][f], b3], [Bp[b]])
                            k += 1
                    TT("dve", hres[:, tt, half * 512:(half + 1) * 512], ps[b][:, :], hres[:, tt, half * 512:(half + 1) * 512], ALU.add,
                       [Bp[b], B_h[tt]], [B_h[tt]])
                if u == 7:
                    brs_f[tt] = rms_cols(hres[:, tt, :], B_h[tt], 34 + tt)

        steps = [(u, s) for u in range(8) for s in range(4)]
        gu_mm(0, 0, 0, 0)
        for k_, (u, s) in enumerate(steps):
            gu_post(u, s, 0, 0)
            gu_mm(u, s, 0, 1)
            gu_post(u, s, 0, 1)
            gu_mm(u, s, 1, 0)
            gu_post(u, s, 1, 0)
            gu_mm(u, s, 1, 1)
            gu_post(u, s, 1, 1)
            if k_ + 1 < len(steps):
                un, sn = steps[k_ + 1]
                gu_mm(un, sn, 0, 0)
            down(u, s)
            if s == 3 and u + NU < 8:
                load_unit(u + NU)
        dump_h("dbg_h3")

        S.barrier()
        R3.reset()
        yo = [R3.f32(1024) for _ in range(2)]
        B_yo = [Buf() for _ in range(2)]
        for tt in range(16):
            STT(yo[tt % 2], hres[:, tt, :], rstd_all[:, 34 + tt:35 + tt], fnorm_bc, ALU.mult, ALU.mult, [B_h[tt], brs_f[tt], Bc], [B_yo[tt % 2]])
            DMA("sp", y[tt * 128:(tt + 1) * 128, :], yo[tt % 2], [B_yo[tt % 2]], [])

        S.emit(nc, st)
    return nc


def _rel_bucket(rel):
    nb = 16
    max_exact = 8
    ret = (rel > 0).astype(np.int32) * nb
    n = np.abs(rel)
    nf = np.maximum(n, 1).astype(np.float32)
    large = max_exact + (np.log(nf / np.float32(max_exact)) / np.float32(math.log(128 / max_exact))
                         * np.float32(nb - max_exact)).astype(np.int32)
    large = np.minimum(large, nb - 1)
    return ret + np.where(n < max_exact, n, large)


_NC_CACHE = {}


def kernel(**inp):
    debug = int(inp.pop("_debug", 0)) if "_debug" in inp else 0
    f = lambda k: np.ascontiguousarray(np.asarray(inp[k], dtype=np.float32))
    x = f("x")
    mem = f("mem")
    i = np.arange(384)
    rel = 127 - i
    bk = _rel_bucket(rel.astype(np.int32))
    oh = np.zeros((32, 384), np.float32)
    oh[bk, i] = 1.0
    oh[15, :] -= 1.0
    kk = np.arange(128)[:, None]
    qq = np.arange(128)[None, :]
    maskT = np.where((kk < 64) | (qq >= 64), 0.0, NEG).astype(np.float32)
    ident = np.eye(128, dtype=np.float32)
    sel = np.zeros((16, 16, 128), np.float32)
    for e in range(16):
        sel[e, e, :] = 1.0
    sel = sel.reshape(16, 2048)
    gains = np.zeros((128, 40), np.float32)
    gains[:, 0:8] = f("attn_norm")[0].reshape(8, 128).T
    gains[:, 8:16] = f("cross_norm")[0].reshape(8, 128).T
    gains[:, 16:24] = f("mem_norm")[0].reshape(8, 128).T
    gains[:, 24:32] = f("ffn_norm")[0].reshape(8, 128).T
    gains[:, 32:36] = f("pool_scale")[0].reshape(4, 128).T
    gains[:, 36] = f("diff_subln")[0]
    wr = np.concatenate([f("router_group")[0], f("router_expert")[0].transpose(1, 0, 2).reshape(1024, 16)], axis=1)
    rbias = np.concatenate([f("router_group_bias")[0], f("router_expert_bias")[0].reshape(16)])[None, :]
    lamv = np.concatenate([f("lambda_q1")[0], f("lambda_k1")[0], f("lambda_q2")[0], f("lambda_k2")[0]])[None, :]
    shared = {
        "w_in": f("w_in")[0], "w_out": f("w_out")[0], "wq": f("wq_cross")[0], "wkv": f("wkv_cross")[0], "wo": f("wo_cross")[0],
        "w_gate": f("w_gate")[0], "w_up": f("w_up")[0], "w_down": f("w_down")[0], "pool_w": f("pool_w")[0],
        "wr": np.ascontiguousarray(wr), "rbias": np.ascontiguousarray(rbias), "rel_bias": f("rel_bias"),
        "lamv": np.ascontiguousarray(lamv), "gains": gains, "fnorm": f("final_norm")[None, :],
        "ident": ident, "oh": oh, "maskT": maskT, "sel": sel,
    }
    in_maps = []
    for c in range(8):
        b, j = c // 4, c % 4
        pad = 3 - j
        xs = np.zeros((8192, 1024), np.float32)
        xs[pad * 512:] = x[b, :(16 - pad) * 512]
        kvb = np.zeros((128, 16), np.float32)
        kvb[:, :pad] = NEG
        pinv = np.zeros((128, 4, 16), np.float32)
        for g in range(4):
            w = 2 ** (g + 1)
            if j == 0:
                pinv[:, g, :] = 1.0 / np.minimum(np.arange(1, 17), w)
            else:
                pinv[:, g, :] = 1.0 / w
        m = dict(shared)
        m.update({"x": xs, "mem": mem[b], "kvb": kvb, "pinv": pinv.reshape(128, 64)})
        in_maps.append(m)
    key = debug
    if key not in _NC_CACHE:
        _NC_CACHE[key] = build_nc(debug)
    nc = _NC_CACHE[key]
    res = run_bass_kernel_spmd(nc, in_maps, core_ids=list(range(8)))
    out = np.zeros((2, 8192, 1024), np.float32)
    extra = {}
    for c in range(8):
        b, j = c // 4, c % 4
        r = res.results[c]
        for s in range(4):
            t = 4 * s + j
            out[b, t * 512:(t + 1) * 512] = r["y"][s * 512:(s + 1) * 512]
        if debug:
            extra[c] = {k: v for k, v in r.items() if k.startswith("dbg")}
    if debug:
        return out, extra
    return out
```

```python
import math
import numpy as np
from contextlib import ExitStack
import concourse.bass as bass
import concourse.mybir as mybir
from concourse.bass_utils import run_bass_kernel_spmd

F32 = mybir.dt.float32
BF16 = mybir.dt.bfloat16
AF = mybir.ActivationFunctionType
ALU = mybir.AluOpType
AX = mybir.AxisListType

COMPUTE = ("pe", "act", "dve", "pool")
ENGS = ("pe", "act", "dve", "pool", "sp")
NEG = -30000.0


class Buf:
    __slots__ = ("name", "writer", "rd_eng", "rd_dma")

    def __init__(self, name=""):
        self.name = name
        self.writer = None
        self.rd_eng = {}
        self.rd_dma = []


class Op:
    __slots__ = ("eng", "idx", "fn", "waits", "signal", "num", "dma", "dma_i", "clock", "slotwait")


class Sched:
    def __init__(self, K=8):
        self.ops = {e: [] for e in ENGS}
        self.known = {e: {c: -1 for c in COMPUTE} for e in ENGS}
        self.dma_known = {e: set() for e in ENGS}
        self.ndma = {e: 0 for e in ENGS}
        self.dma_ops = {e: [] for e in ENGS}
        self.bar = {e: [] for e in ENGS}
        self.K = K

    def barrier(self):
        lasts = []
        for e in COMPUTE:
            for op in reversed(self.ops[e]):
                if not op.dma:
                    lasts.append(op)
                    break
        for e in ENGS:
            self.bar[e] = list(lasts)

    def add(self, eng, fn, reads=(), writes=(), dma=False):
        op = Op()
        op.eng = eng
        op.idx = len(self.ops[eng])
        op.fn = fn
        op.dma = dma
        op.signal = False
        op.num = None
        op.dma_i = None
        op.slotwait = None
        deps = []
        if self.bar[eng]:
            deps.extend(d for d in self.bar[eng] if not (d.eng == eng and eng == "pe"))
            self.bar[eng] = []
        for b in reads:
            if b.writer is not None:
                deps.append(b.writer)
        for b in writes:
            if b.writer is not None:
                deps.append(b.writer)
            deps.extend(b.rd_eng.values())
            deps.extend(b.rd_dma)
        known = self.known[eng]
        best = {}
        dwaits = []
        for d in deps:
            if d.dma:
                if id(d) not in self.dma_known[eng]:
                    self.dma_known[eng].add(id(d))
                    dwaits.append(d)
            else:
                if d.eng == eng and eng == "pe":
                    continue
                if known[d.eng] >= d.idx:
                    continue
                if d.eng not in best or best[d.eng].idx < d.idx:
                    best[d.eng] = d
        waits = list(best.values()) + dwaits
        for d in waits:
            d.signal = True
            for c in COMPUTE:
                if d.clock[c] > known[c]:
                    known[c] = d.clock[c]
            if not d.dma and d.idx > known[d.eng]:
                known[d.eng] = d.idx
        if dma:
            i = self.ndma[eng]
            op.dma_i = i
            self.ndma[eng] = i + 1
            if i >= self.K:
                prev = self.dma_ops[eng][i - self.K]
                op.slotwait = prev
                self.dma_known[eng].add(id(prev))
            self.dma_ops[eng].append(op)
        op.waits = waits
        op.clock = dict(known)
        if not dma and eng in COMPUTE:
            op.clock[eng] = op.idx
        for b in reads:
            if dma:
                b.rd_dma.append(op)
            else:
                b.rd_eng[eng] = op
        for b in writes:
            b.writer = op
            b.rd_eng = {}
            b.rd_dma = []
        self.ops[eng].append(op)
        return op

    def emit(self, nc, stack):
        sem_eng = {e: stack.enter_context(nc.semaphore("s_" + e)) for e in COMPUTE}
        sem_dma = {e: [stack.enter_context(nc.semaphore("d_%s%d" % (e, k))) for k in range(self.K)]
                   for e in ENGS if self.ndma[e] > 0}
        for e in COMPUTE:
            n = 0
            for op in self.ops[e]:
                if op.signal and not op.dma:
                    n += 1
                    op.num = n
        K = self.K

        def dma_target(d):
            return sem_dma[d.eng][d.dma_i % K], 16 * (d.dma_i // K + 1)

        def run(ename, e):
            for op in self.ops[ename]:
                if op.slotwait is not None:
                    s, v = dma_target(op.slotwait)
                    e.wait_ge(s, v)
                for d in op.waits:
                    if d.dma:
                        s, v = dma_target(d)
                        e.wait_ge(s, v)
                    else:
                        e.wait_ge(sem_eng[d.eng], d.num)
                ins = op.fn(e)
                if op.dma:
                    s, v = dma_target(op)
                    ins.then_inc(s, 16)
                elif op.signal:
                    ins.then_inc(sem_eng[ename], 1)
            for d in self.dma_ops[ename][-K:]:
                s, v = dma_target(d)
                e.wait_ge(s, v)

        block = stack.enter_context(nc.Block())

        @block.tensor
        def _(e):
            run("pe", e)

        @block.scalar
        def _(e):
            run("act", e)

        @block.vector
        def _(e):
            run("dve", e)

        @block.gpsimd
        def _(e):
            run("pool", e)

        @block.sync
        def _(e):
            run("sp", e)


class Arena:
    def __init__(self, nc, st, name, nbytes):
        self.t32 = st.enter_context(nc.sbuf_tensor(name, [128, nbytes // 4], F32))
        self.t16 = self.t32.bitcast(BF16)
        self.nbytes = nbytes
        self.off = 0

    def reset(self, off=0):
        self.off = off

    def f32(self, n):
        o = self.off
        self.off += 4 * n
        assert self.off <= self.nbytes, (self.off, self.nbytes)
        return self.t32[:, o // 4:o // 4 + n]

    def bf16(self, n):
        o = self.off
        self.off += 2 * n
        self.off = (self.off + 3) // 4 * 4
        assert self.off <= self.nbytes, (self.off, self.nbytes)
        return self.t16[:, o // 2:o // 2 + n]


def build_nc(debug=0):
    nc = bass.Bass("TRN2", target_bir_lowering=False)

    def din(name, shape):
        return nc.dram_tensor(name, shape, F32, kind="ExternalInput")

    x_t = din("x", [8192, 1024]); x = x_t.ap()
    mem = din("mem", [256, 1024]).ap()
    w_in = din("w_in", [1024, 2048]).ap()
    w_out = din("w_out", [1024, 1024]).ap()
    wq = din("wq", [1024, 1024]).ap()
    wkv = din("wkv", [1024, 2048]).ap()
    wo = din("wo", [1024, 1024]).ap()
    w_gate = din("w_gate", [16, 1024, 256]).ap()
    w_up = din("w_up", [16, 1024, 256]).ap()
    w_down = din("w_down", [16, 256, 1024]).ap()
    pool_w = din("pool_w", [4, 128, 128]).ap()
    wr = din("wr", [1024, 20]).ap()
    rbias_t = din("rbias", [1, 20])
    rel_bias = din("rel_bias", [32, 4]).ap()
    lamv_t = din("lamv", [1, 256])
    gains_d = din("gains", [128, 40]).ap()
    fnorm_t = din("fnorm", [1, 1024])
    ident_d = din("ident", [128, 128]).ap()
    oh_d = din("oh", [32, 384]).ap()
    maskT_d = din("maskT", [128, 128]).ap()
    kvb_d = din("kvb", [128, 16]).ap()
    pinv_d = din("pinv", [128, 64]).ap()
    sel_d = din("sel", [16, 2048]).ap()
    y = nc.dram_tensor("y", [2048, 1024], F32, kind="ExternalOutput").ap()
    E_t = nc.dram_tensor("Escr", [4, 128, 384], F32, kind="Internal")
    dbg = {}
    if debug:
        for nm in ("dbg_h1", "dbg_h2", "dbg_h3"):
            dbg[nm] = nc.dram_tensor(nm, [2048, 1024], F32, kind="ExternalOutput").ap()
        dbg["dbg_mix"] = nc.dram_tensor("dbg_mix", [128, 8 * 2048], BF16, kind="ExternalOutput").ap()

    S = Sched()
    st = ExitStack()
    with st:
        G = Arena(nc, st, "G", 18 * 1024)
        ident = G.bf16(128)
        ones_bf = G.bf16(128)
        ones_f = G.f32(128)
        gains = G.f32(40)
        gains_t = G.t32
        gains_off = gains.offset
        identf = G.f32(128)
        fnorm_bc = G.f32(1024)
        rbias_bc = G.f32(20)
        lam_sb = G.f32(256)
        small = G.f32(64)
        relb = G.f32(4)
        oh_sb = G.f32(384)
        g_sb = G.f32(384)
        g_off = g_sb.offset
        maskT = G.f32(128)
        kvb = G.f32(16)
        pinv = G.f32(64)
        biasT = G.f32(1024).rearrange("p (h t q) -> p h t q", h=4, t=2)
        rstd_all = G.f32(64)
        ssq_all = G.f32(64)
        lnv = G.f32(8)
        FP8 = mybir.dt.float8e4
        g8 = G.t32.bitcast(FP8)
        junks = [g8[:, G.off + 1024 * k:G.off + 1024 * (k + 1)] for k in range(2)]
        G.off += 2048
        B_junk = [Buf() for _ in range(2)]
        jctr = [0]

        def SQ(src, Bsrc, accum, wr_):
            k = jctr[0] % 2
            jctr[0] += 1
            return S.add("act", lambda e: e.activation(junks[k], src, AF.Square, accum_out=accum, saturate=False),
                         Bsrc, wr_ + [B_junk[k]])
        lnv_ctr = [0]
        R1 = Arena(nc, st, "R1", 32 * 1024)
        R2 = Arena(nc, st, "R2", 64 * 1024)
        R3 = Arena(nc, st, "R3", 92 * 1024)
        ps_all = st.enter_context(nc.psum_tensor("ps_all", [128, 4096], F32))
        psb_all = ps_all.bitcast(BF16)
        ps = [ps_all[:, i * 512:(i + 1) * 512] for i in range(8)]
        psb = [psb_all[:, i * 1024:(i + 1) * 1024] for i in range(8)]
        Bp = [Buf("ps%d" % i) for i in range(8)]

        mixT = R1.t16[:, 0:16384].rearrange("p (c n) -> p c n", c=8)
        hT = mixT
        KT = R2.t16[:, 0:16384].rearrange("p (h n) -> p h n", h=2)
        Vv = R2.t16[:, 16384:32768].rearrange("p (k n) -> p k n", k=64)
        hres = R2.t32[:, 0:16384].rearrange("p (t n) -> p t n", t=16)

        def MM(out, lhsT, rhs, start, stop, rd, wr_):
            return S.add("pe", lambda e: e.matmul(out, lhsT, rhs, start=start, stop=stop), rd, wr_)

        def TR(out, in_, rd, wr_):
            return S.add("pe", lambda e: e.transpose(out, in_, ident), rd, wr_)

        def ACT(out, in_, func, rd, wr_, **kw):
            return S.add("act", lambda e: e.activation(out, in_, func, **kw), rd, wr_)

        def TT(eng, out, in0, in1, op, rd, wr_):
            return S.add(eng, lambda e: e.tensor_tensor(out, in0, in1, op), rd, wr_)

        def TS(eng, out, in0, s1, s2, op0, op1, rd, wr_):
            if op1 is None:
                return S.add(eng, lambda e: e.tensor_scalar(out, in0, s1, None, op0), rd, wr_)
            return S.add(eng, lambda e: e.tensor_scalar(out, in0, s1, s2, op0, op1), rd, wr_)

        def STT(out, in0, scalar, in1, op0, op1, rd, wr_):
            return S.add("dve", lambda e: e.scalar_tensor_tensor(out, in0, scalar, in1, op0, op1), rd, wr_)

        def CP(eng, out, in_, rd, wr_):
            if eng == "act":
                return S.add("act", lambda e: e.copy(out, in_), rd, wr_)
            return S.add(eng, lambda e: e.tensor_copy(out, in_), rd, wr_)

        def RECIP(out, in_, rd, wr_):
            return S.add("dve", lambda e: e.reciprocal(out, in_), rd, wr_)

        def DMA(q, out, in_, rd, wr_):
            return S.add(q, lambda e: e.dma_start(out=out, in_=in_), rd, wr_, dma=True)

        def gain_bc(c0):
            return bass.AP(gains_t, gains_off + c0, [[G.nbytes // 4, 128], [1, 8], [0, 128]])

        def gcol(c):
            return gains[:, c:c + 1]

        Bc = Buf("consts")
        B_g = Buf("g_sb")
        B_E = Buf("E")
        B_bias = Buf("biasT")
        DMA("pool", ident, ident_d, [], [Bc])
        DMA("sp", identf, ident_d, [], [Bc])
        DMA("sp", gains, gains_d, [], [Bc])
        DMA("sp", fnorm_bc, bass.AP(fnorm_t, 0, [[0, 128], [1, 1024]]), [], [Bc])
        DMA("sp", rbias_bc, bass.AP(rbias_t, 0, [[0, 128], [1, 20]]), [], [Bc])
        DMA("sp", lam_sb, bass.AP(lamv_t, 0, [[0, 128], [1, 256]]), [], [Bc])
        DMA("sp", relb[0:32, :], rel_bias, [], [Bc])
        DMA("sp", oh_sb[0:32, :], oh_d, [], [Bc])
        DMA("sp", maskT, maskT_d, [], [Bc])
        DMA("sp", kvb, kvb_d, [], [Bc])
        DMA("sp", pinv, pinv_d, [], [Bc])
        S.add("dve", lambda e: e.memset(ones_bf, 1.0), [], [Bc])
        S.add("dve", lambda e: e.memset(ones_f, 1.0 / 128.0), [], [Bc])
        prod = G.f32(128)
        TT("dve", prod[:, 0:64], lam_sb[:, 0:64], lam_sb[:, 64:128], ALU.mult, [Bc], [Bc])
        TT("dve", prod[:, 64:128], lam_sb[:, 128:192], lam_sb[:, 192:256], ALU.mult, [Bc], [Bc])
        S.add("dve", lambda e: e.reduce_sum(small[:, 0:1], prod[:, 0:64], axis=AX.X), [Bc], [Bc])
        S.add("dve", lambda e: e.reduce_sum(small[:, 1:2], prod[:, 64:128], axis=AX.X), [Bc], [Bc])
        ACT(small[:, 2:4], small[:, 0:2], AF.Exp, [Bc], [Bc])
        TT("dve", small[:, 4:5], small[:, 3:4], small[:, 2:3], ALU.subtract, [Bc], [Bc])
        TS("dve", small[:, 4:5], small[:, 4:5], -0.2, None, ALU.add, None, [Bc], [Bc])
        TS("dve", small[:, 5:6], gcol(36), 0.8, None, ALU.mult, None, [Bc], [Bc])
        neg_lam = small[:, 4:5]
        subcol = small[:, 5:6]
        def bias_part1():
            MM(ps[7][0:4, 0:384], relb[0:32, 0:4], oh_sb[0:32, 0:384], True, True, [Bc], [Bp[7]])
            CP("dve", g_sb[0:4, :], ps[7][0:4, 0:384], [Bp[7]], [B_g])

        def bias_part1b():
            DMA("sp", E_t.ap(), bass.AP(G.t32, g_off, [[G.nbytes // 4, 4], [0, 128], [1, 384]]), [B_g], [B_E])

        def bias_part2():
            DMA("sp", biasT[:, :, 0, :], bass.AP(E_t, 127, [[383, 128], [128 * 384, 4], [1, 128]]), [B_E], [B_bias])
            DMA("sp", biasT[:, :, 1, :], bass.AP(E_t, 255, [[383, 128], [128 * 384, 4], [1, 128]]), [B_E], [B_bias])

        def bias_part3():
            mask_bc = bass.AP(G.t32, maskT.offset, [[G.nbytes // 4, 128], [0, 4], [1, 128]])
            TT("dve", biasT[:, :, 0, :], biasT[:, :, 0, :], mask_bc, ALU.add, [B_bias, Bc], [B_bias])

        def norm_transpose(src, Bsrc, rstd_col, gain_c0, dst, Bdst, xs, Bxs, bank, Brs):
            ACT(xs, src, AF.Copy, [Bsrc, Brs], [Bxs], scale=rstd_col)
            for c in range(8):
                TR(psb[bank][:, c * 128:(c + 1) * 128], xs[:, c * 128:(c + 1) * 128], [Bxs, Bc], [Bp[bank]])
            TT("dve", dst, psb[bank][:, 0:1024].rearrange("p (c n) -> p c n", c=8), gain_bc(gain_c0), ALU.mult,
               [Bp[bank], Bc], [Bdst])

        B_lnc = [Buf() for _ in range(8)]

        def rms_cols(src, Bsrc, col):
            k = lnv_ctr[0] % 8
            lnv_ctr[0] += 1
            b1 = Buf()
            b3 = Buf()
            SQ(src, [Bsrc], ssq_all[:, col:col + 1], [b1])
            ACT(lnv[:, k:k + 1], ssq_all[:, col:col + 1], AF.Ln, [b1], [B_lnc[k]], scale=1.0 / 1024.0, bias=1e-6)
            ACT(rstd_all[:, col:col + 1], lnv[:, k:k + 1], AF.Exp, [B_lnc[k]], [b3], scale=-0.5)
            return b3
        brs_c = [None] * 16
        brs_d = [None] * 16
        brs_f = [None] * 16
        B_ssq = [Buf() for _ in range(64)]
        B_ln = [Buf() for _ in range(2)]
        B_rslot = [Buf() for _ in range(16)]
        B_mix = [[Buf("mix%d_%d" % (c, s)) for s in range(4)] for c in range(8)]

        for pr in range(2):
            S.barrier()
            R3.reset()
            wA = R3.bf16(8 * 768).rearrange("p (c n) -> p c n", c=8)
            B_wA = [Buf() for _ in range(3)]
            for part, c0 in enumerate((512 + 256 * pr, 1024 + 256 * pr, 256 * pr)):
                DMA("pool", wA[:, :, part * 256:(part + 1) * 256],
                    w_in[:, c0:c0 + 256].rearrange("(c p) n -> p c n", p=128), [], [B_wA[part]])
            if pr == 0:
                wU = R3.bf16(8 * 512).rearrange("p (c n) -> p c n", c=8)
                B_wU = Buf()
                DMA("pool", wU, w_in[:, 1536:2048].rearrange("(c p) n -> p c n", p=128), [], [B_wU])
                pw = R3.bf16(512).rearrange("p (g n) -> p g n", g=4)
                B_pw = Buf()
                DMA("pool", pw, pool_w.rearrange("g c d -> c g d"), [], [B_pw])
            QT = R3.bf16(2 * 2 * 2048).rearrange("p (h m n) -> p h m n", h=2, m=2)
            B_QT = [[Buf() for _ in range(4)] for _ in range(2)]
            B_QTz = Buf()
            S.add("pool", lambda e, QT=QT: e.memset(QT.rearrange("p h m n -> p (h m n)"), 0.0), [], [B_QTz])
            mark = R3.off
            xst = [R3.f32(1024) for _ in range(4)]
            if pr == 0:
                xst += [R1.t32[:, k * 1024:(k + 1) * 1024] for k in range(4)]
            else:
                xst += [R3.f32(1024) for _ in range(4)]
            B_xst = [Buf() for _ in range(8)]
            nxs = 3 if pr == 0 else 4
            xs_t = [R3.bf16(1024) for _ in range(nxs)]
            B_xs = [Buf() for _ in range(nxs)]
            hnT = [R3.bf16(8 * 512).rearrange("p (c n) -> p c n", c=8) for _ in range(2)]
            B_hnT = [[Buf() for _ in range(4)] for _ in range(2)]
            if pr == 0:
                uT = R3.f32(4 * 528).rearrange("p (g n) -> p g n", g=4)
                B_uT = [Buf() for _ in range(4)]
                B_uTp = [Buf() for _ in range(4)]
                ptmp = [R3.f32(528) for _ in range(2)]
                B_pt = [Buf() for _ in range(2)]
                dT = R3.bf16(4 * 512).rearrange("p (g n) -> p g n", g=4)
                B_dT = [Buf() for _ in range(4)]
            kq = [0]

            def kbank():
                b_ = 4 + kq[0] % 3
                kq[0] += 1
                return b_

            def emit_V(g, hb, tt):
                vb = 2 + g % 2
                for c in range(8):
                    MM(ps[vb][:, 0:256], hnT[hb][:, c, tt * 128:(tt + 1) * 128], wA[:, c, 256:512], c == 0, c == 7,
                       [B_hnT[hb][tt], B_wA[1]], [Bp[vb]])
                CP("act" if pr == 1 else "dve", Vv[:, g, :], ps[vb][:, 0:256], [Bp[vb]], [])

            def emit_slot(i, hb):
                own = (i % 4 == 3)
                s_own = i // 4
                allh = B_hnT[hb]
                for hc in range(2):
                    kb_ = kbank()
                    for c in range(8):
                        MM(ps[kb_][:, :], wA[:, c, hc * 128:(hc + 1) * 128], hnT[hb][:, c, :], c == 0, c == 7,
                           allh + [B_wA[0]], [Bp[kb_]])
                    CP("dve" if (hc or pr == 0) else "act", KT[:, hc, i * 512:(i + 1) * 512], ps[kb_][:, :], [Bp[kb_]], [])
                if own:
                    for hc in range(2):
                        kb_ = kbank()
                        for c in range(8):
                            MM(ps[kb_][:, :], wA[:, c, 512 + hc * 128:512 + (hc + 1) * 128], hnT[hb][:, c, :], c == 0, c == 7,
                               allh + [B_wA[2]], [Bp[kb_]])
                        TS("dve", QT[0:64, hc, 0, s_own * 512:(s_own + 1) * 512], ps[kb_][0:64, :], 0.125, None, ALU.mult, None,
                           [Bp[kb_], B_QTz], [B_QT[hc][s_own]])
                        TS("dve", QT[64:128, hc, 1, s_own * 512:(s_own + 1) * 512], ps[kb_][64:128, :], 0.125, None, ALU.mult, None,
                           [Bp[kb_], B_QTz, B_QT[hc][s_own]], [B_QT[hc][s_own]])
                if pr == 0 and i % 4 == 2:
                    for gg in range(4):
                        kb_ = kbank()
                        for c in range(8):
                            MM(ps[kb_][:, 0:16], wU[:, c, gg * 128:(gg + 1) * 128], hnT[hb][:, c, 496:512], c == 0, c == 7,
                               [B_hnT[hb][3], B_wU], [Bp[kb_]])
                        CP("dve", uT[:, gg, 0:16], ps[kb_][:, 0:16], [Bp[kb_]], [B_uTp[gg]])
                if pr == 0 and own:
                    for gg in range(4):
                        w = 2 ** (gg + 1)
                        kb_ = kbank()
                        for c in range(8):
                            MM(ps[kb_][:, :], wU[:, c, gg * 128:(gg + 1) * 128], hnT[hb][:, c, :], c == 0, c == 7,
                               allh + [B_wU], [Bp[kb_]])
                        CP("act", uT[:, gg, 16:528], ps[kb_][:, :], [Bp[kb_]], [B_uT[gg]])
                        U = uT[:, gg, :]
                        ru = [B_uT[gg], B_uTp[gg]]
                        TT("pool", ptmp[0][:, 1:528], U[:, 1:528], U[:, 0:527], ALU.add, ru, [B_pt[0]])
                        cur = 0
                        sh = 2
                        lo = 1
                        while sh < w:
                            lo += sh
                            TT("pool", ptmp[1 - cur][:, lo:528], ptmp[cur][:, lo:528], ptmp[cur][:, lo - sh:528 - sh], ALU.add,
                               [B_pt[cur]], [B_pt[1 - cur]])
                            cur = 1 - cur
                            sh *= 2
                        STT(dT[:, gg, :], ptmp[cur][:, 16:528], 1.0 / w, U[:, 16:528], ALU.mult, ALU.subtract,
                            [B_pt[cur]] + ru, [B_dT[gg]])
                        if s_own == 0:
                            tmp16 = small[:, 16:32]
                            TT("dve", tmp16, ptmp[cur][:, 16:32], pinv[:, gg * 16:(gg + 1) * 16], ALU.mult, [B_pt[cur], Bc], [Bc])
                            TT("dve", dT[:, gg, 0:16], tmp16, U[:, 16:32], ALU.subtract, [Bc] + ru + [B_dT[gg]], [B_dT[gg], Bc])

                    def stageB(s_own=s_own):
                        for gg in range(4):
                            kb2 = kbank()
                            MM(ps[kb2][:, :], pw[:, gg, :], dT[:, gg, :], True, True, [B_pw, B_dT[gg]], [Bp[kb2]])
                            TS("dve", mixT[:, 4 + gg, s_own * 512:(s_own + 1) * 512], ps[kb2][:, :], gcol(32 + gg), None, ALU.mult, None,
                               [Bp[kb2], Bc], [B_mix[4 + gg][s_own]])
                    late.append(stageB)

            def emit_load(g):
                xb = 4 * ((g // 4) % 2) + g % 4
                DMA("sp", xst[xb], x[g * 128:(g + 1) * 128, :], [], [B_xst[xb]])

            def emit_sq1(g):
                if pr == 0:
                    xb = 4 * ((g // 4) % 2) + g % 4
                    SQ(xst[xb], [B_xst[xb]], ssq_all[:, g:g + 1], [B_ssq[g]])

            def emit_stats(i, squares=True):
                for tt in range(4):
                    if squares:
                        emit_sq1(4 * i + tt)
                if pr == 0:
                    lv = lnv[:, 4 * (i % 2):4 * (i % 2) + 4]
                    ACT(lv, ssq_all[:, 4 * i:4 * i + 4], AF.Ln, B_ssq[4 * i:4 * i + 4], [B_ln[i % 2]], scale=1.0 / 1024.0, bias=1e-6)
                    ACT(rstd_all[:, 4 * i:4 * i + 4], lv, AF.Exp, [B_ln[i % 2]], [B_rslot[i]], scale=-0.5)

            pending = []
            late = []
            TB = (0, 1, 7)

            def partA(g):
                i_, tt_ = g // 4, g % 4
                xb = 4 * (i_ % 2) + tt_
                ACT(xs_t[g % nxs], xst[xb], AF.Copy, [B_xst[xb], B_rslot[i_]], [B_xs[g % nxs]], scale=rstd_all[:, g:g + 1])

            def partB(g):
                i_, tt_ = g // 4, g % 4
                hb_ = i_ % 2
                bank = TB[g % 3]
                xs = xs_t[g % nxs]
                for c in range(8):
                    TR(psb[bank][:, c * 128:(c + 1) * 128], xs[:, c * 128:(c + 1) * 128], [B_xs[g % nxs], Bc], [Bp[bank]])
                TT("dve", hnT[hb_][:, :, tt_ * 128:(tt_ + 1) * 128], psb[bank][:, 0:1024].rearrange("p (c n) -> p c n", c=8),
                   gain_bc(0), ALU.mult, [Bp[bank], Bc], [B_hnT[hb_][tt_]])

            for g in range(8):
                emit_load(g)
            emit_stats(0)
            emit_stats(1)
            partA(0)
            partA(1)
            emit_load(8)
            emit_load(9)
            for g in range(64):
                i, tt = g // 4, g % 4
                hb = i % 2
                if tt == 0:
                    run_late = late[:]
                    del late[:]
                if pr == 0 and g == 8:
                    bias_part1()
                if pr == 0 and g == 16:
                    bias_part1b()
                if pr == 0 and g == 24:
                    bias_part2()
                if pr == 0 and g == 40:
                    bias_part3()
                if g + 2 < 64:
                    partA(g + 2)
                if g + 10 < 64:
                    emit_load(g + 10)
                if g + 8 < 64:
                    emit_sq1(g + 8)
                    if tt == 3:
                        emit_stats(i + 2, squares=False)
                partB(g)
                cur_p = [lambda g=g, hb=hb, tt=tt: emit_V(g, hb, tt)]
                if tt == 3:
                    cur_p.append(lambda i=i, hb=hb: emit_slot(i, hb))
                if tt == 2:
                    cur_p.extend(run_late)
                pending.append(cur_p)
                if len(pending) > 2:
                    for f_ in pending.pop(0):
                        f_()
            for grp_ in pending:
                for f_ in grp_:
                    f_()
            for f_ in late:
                f_()
            pending = []
            late = []

            S.barrier()
            R3.reset(mark)
            NP = 3
            Pt = [R3.bf16(1024) for _ in range(NP)]
            B_P = [Buf() for _ in range(NP)]
            rs = [R3.f32(512) for _ in range(2)]
            tq = [R3.f32(512) for _ in range(2)]
            ocp = [R3.f32(512) for _ in range(2)]
            B_ocp = [Buf() for _ in range(2)]
            o_sb = R3.f32(512)
            sq_sb = R3.f32(512)
            r2_sb = R3.f32(512)
            B_fin = [Buf() for _ in range(8)]
            if pr == 1:
                TOP = R3.nbytes - 32 * 1024
                assert R3.off <= TOP, R3.off
                R3.reset(TOP)
                wO = R3.bf16(8 * 1024).rearrange("p (c n) -> p c n", c=8)
                wQ = R3.bf16(8 * 1024).rearrange("p (c n) -> p c n", c=8)
                B_wO = Buf()
                B_wQ = Buf()
                DMA("pool", wO, w_out.rearrange("(c p) n -> p c n", p=128), [], [B_wO])
                DMA("pool", wQ, wq.rearrange("(c p) n -> p c n", p=128), [], [B_wQ])
            flat = []
            for s in range(4):
                for hc in range(2):
                    nkb_ = 4 * (4 * s + 3 + 1)
                    for kb in range(nkb_):
                        flat.append((s, hc, kb, nkb_))
            n = len(flat)
            sbank = {}
            pbuf = {}
            rot = {"s": 0, "p": 0}

            def geom(t):
                s, hc, kb, nkb = flat[t]
                r = kb - 4 * (4 * s + 3)
                return s, hc, kb, nkb, r, 128 * max(r, 0)

            def QK(t):
                s, hc, kb, nkb, r, c0 = geom(t)
                b0 = 2 * (rot["s"] % 2)
                rot["s"] += 1
                sbank[t] = b0
                for m in range(2):
                    MM(ps[b0 + m][:, c0:512], KT[:, hc, kb * 128:(kb + 1) * 128],
                       QT[:, hc, m, s * 512 + c0:(s + 1) * 512], True, True,
                       [B_QT[hc][s]], [Bp[b0 + m]])

            def SOFT(t):
                s, hc, kb, nkb, r, c0 = geom(t)
                h = 2 * pr + hc
                b0 = sbank[t]
                if r >= -1:
                    if r == -1:
                        cs, nb_, boff = 0, 128, 128
                    elif r == 3:
                        cs, nb_, boff = 384, 128, 0
                    else:
                        cs, nb_, boff = 128 * r, 256, 0
                    pv_ = ps_all[:, b0 * 512:(b0 + 2) * 512].rearrange("p (b n) -> p b n", b=2)[:, :, cs:cs + nb_]
                    bia = bass.AP(G.t32, biasT.offset + h * 256 + boff, [[G.nbytes // 4, 128], [0, 2], [1, nb_]])
                    TT("dve", pv_, pv_, bia, ALU.add, [Bp[b0], Bp[b0 + 1], B_bias], [Bp[b0], Bp[b0 + 1]])
                pb = rot["p"] % NP
                rot["p"] += 1
                pbuf[t] = pb
                kw = {}
                if kb // 4 <= 2:
                    kw["bias"] = kvb[:, kb // 4:kb // 4 + 1]
                src = ps_all[:, b0 * 512:(b0 + 2) * 512].rearrange("p (b n) -> p b n", b=2)[:, :, c0:512]
                dst = Pt[pb].rearrange("p (b n) -> p b n", b=2)[:, :, c0:512]
                ACT(dst, src, AF.Exp, [Bp[b0], Bp[b0 + 1], Bc], [B_P[pb]], **kw)

            def PV(t):
                s, hc, kb, nkb, r, c0 = geom(t)
                pb = pbuf[t]
                first = (kb == 0)
                last = (kb == nkb - 1)
                for m in range(2):
                    Pm = Pt[pb][:, m * 512 + c0:(m + 1) * 512]
                    MM(ps[4 + m][:, c0:512], Vv[:, kb, hc * 128:(hc + 1) * 128], Pm, first, last,
                       [B_P[pb]], [Bp[4 + m]])
                    MM(ps[6 + m][:, c0:512], ones_bf, Pm, first, last, [B_P[pb], Bc], [Bp[6 + m]])

            def FIN_a(s, hc):
                RECIP(rs[0], ps[6][:, :], [Bp[6]], [B_fin[0]])
                CP("act", ocp[0], ps[4][:, :], [Bp[4]], [B_ocp[0]])
                RECIP(rs[1], ps[7][:, :], [Bp[7]], [B_fin[1]])
                CP("act", ocp[1], ps[5][:, :], [Bp[5]], [B_ocp[1]])
                TT("dve", tq[0], ocp[0], rs[0], ALU.mult, [B_ocp[0], B_fin[0]], [B_fin[2]])
                TT("dve", tq[1], ocp[1], rs[1], ALU.mult, [B_ocp[1], B_fin[1]], [B_fin[3]])
                STT(o_sb, tq[1], neg_lam, tq[0], ALU.mult, ALU.add, [B_fin[2], B_fin[3], Bc], [B_fin[4]])
                ACT(sq_sb, o_sb, AF.Square, [B_fin[4]], [B_fin[5]])

            def FIN_b(s, hc, bm):
                h = 2 * pr + hc
                MM(ps[bm][:, :], ones_f, sq_sb, True, True, [B_fin[5], Bc], [Bp[bm]])
                ACT(r2_sb, ps[bm][:, :], AF.Ln, [Bp[bm]], [B_fin[6]], bias=1e-6)
                ACT(r2_sb, r2_sb, AF.Exp, [B_fin[6]], [B_fin[6]], scale=-0.5)
                STT(mixT[:, h, s * 512:(s + 1) * 512], o_sb, subcol, r2_sb, ALU.mult, ALU.mult,
                    [B_fin[4], B_fin[6], Bc], [B_mix[h][s]])

            QK(0)
            QK(1)
            pend_fin = None
            for t in range(n):
                SOFT(t)
                if pend_fin is not None and (t >= pend_fin[2] or t == n - 1):
                    FIN_b(pend_fin[0], pend_fin[1], sbank[t])
                    pend_fin = None
                if t + 2 < n:
                    QK(t + 2)
                PV(t)
                s_, hc_, kb_l, nkb_l = flat[t]
                if kb_l == nkb_l - 1:
                    FIN_a(s_, hc_)
                    pend_fin = (s_, hc_, t + 4)
            if pend_fin is not None:
                FIN_b(pend_fin[0], pend_fin[1], 0)

        S.barrier()
        R3.reset()
        if debug:
            DMA("sp", dbg["dbg_mix"], R1.t16[:, 0:16384], [b for row in B_mix for b in row], [])
        wKV = R3.bf16(8 * 2048).rearrange("p (c n) -> p c n", c=8)
        mark_b = R3.off
        B_wKV = Buf()
        DMA("pool", wKV[:, :, 0:1024], wkv[:, 0:1024].rearrange("(c p) n -> p c n", p=128), [], [B_wKV])
        DMA("pool", wKV[:, :, 1024:2048], wkv[:, 1024:2048].rearrange("(c p) n -> p c n", p=128), [], [B_wKV])
        xo = [R3.f32(1024) for _ in range(2)]
        assert R3.off <= TOP
        B_xo = [Buf() for _ in range(2)]
        B_h = [Buf("h%d" % t) for t in range(16)]
        allmix = [b for row in B_mix for b in row]
        for tt in range(16):
            s_, t4 = tt // 4, tt % 4
            row0 = (4 * s_ + 3) * 512 + t4 * 128
            DMA("sp", xo[tt % 2], x[row0:row0 + 128, :], [], [B_xo[tt % 2]])
            for half in range(2):
                b = 2 * (tt % 2) + half
                for c in range(8):
                    MM(ps[b][:, :], mixT[:, c, tt * 128:(tt + 1) * 128], wO[:, c, half * 512:(half + 1) * 512], c == 0, c == 7,
                       [B_mix[c][s_], B_wO], [Bp[b]])
                TT("dve", hres[:, tt, half * 512:(half + 1) * 512], ps[b][:, :], xo[tt % 2][:, half * 512:(half + 1) * 512], ALU.add,
                   [Bp[b], B_xo[tt % 2]], [B_h[tt]])
            brs_c[tt] = rms_cols(hres[:, tt, :], B_h[tt], tt)

        def dump_h(name):
            if debug:
                for tt in range(16):
                    DMA("sp", dbg[name][tt * 128:(tt + 1) * 128, :], hres[:, tt, :], [B_h[tt]], [])

        dump_h("dbg_h1")

        S.barrier()
        R3.reset(mark_b)
        xs_t = [R3.bf16(1024) for _ in range(2)]
        B_xs = [Buf() for _ in range(2)]
        mnT = R3.bf16(8 * 256).rearrange("p (c n) -> p c n", c=8)
        B_mnT = [Buf() for _ in range(2)]
        KcT = R3.bf16(8 * 256).rearrange("p (c n) -> p c n", c=8)
        B_Kc = Buf()
        Vc = R3.bf16(2 * 1024).rearrange("p (k n) -> p k n", k=2)
        B_Vc = Buf()
        mark_c = R3.off
        mst = [R3.f32(1024) for _ in range(2)]
        B_mst = [Buf() for _ in range(2)]

        for mk in range(2):
            DMA("sp", mst[mk], mem[mk * 128:(mk + 1) * 128, :], [], [B_mst[mk]])
            brs = rms_cols(mst[mk], B_mst[mk], 32 + mk)
            norm_transpose(mst[mk], B_mst[mk], rstd_all[:, 32 + mk:33 + mk], 16, mnT[:, :, mk * 128:(mk + 1) * 128], B_mnT[mk],
                           xs_t[mk], B_xs[mk], mk, brs)
        for j8 in range(8):
            b = 4 + j8 % 4
            for c in range(8):
                MM(ps[b][:, 0:256], wKV[:, c, j8 * 128:(j8 + 1) * 128], mnT[:, c, :], c == 0, c == 7, B_mnT + [B_wKV], [Bp[b]])
            CP("dve" if j8 % 2 else "act", KcT[:, j8, :], ps[b][:, 0:256], [Bp[b]], [B_Kc])
        for mk in range(2):
            for half in range(2):
                b = 4 + (2 * mk + half) % 4
                for c in range(8):
                    MM(ps[b][:, :], mnT[:, c, mk * 128:(mk + 1) * 128], wKV[:, c, 1024 + half * 512:1024 + (half + 1) * 512], c == 0, c == 7,
                       B_mnT + [B_wKV], [Bp[b]])
                CP("dve" if half else "act", Vc[:, mk, half * 512:(half + 1) * 512], ps[b][:, :], [Bp[b]], [B_Vc])
        B_hT = [[Buf() for _ in range(4)] for _ in range(4)]
        for tt in range(16):
            norm_transpose(hres[:, tt, :], B_h[tt], rstd_all[:, tt:tt + 1], 8, hT[:, :, tt * 128:(tt + 1) * 128], B_hT[tt // 4][tt % 4],
                           xs_t[tt % 2], B_xs[tt % 2], tt % 2, brs_c[tt])
        S.barrier()
        wOc = wKV[:, :, 0:1024]
        B_wOc = Buf()
        DMA("pool", wOc, wo.rearrange("(c p) n -> p c n", p=128), [], [B_wOc])
        R3.reset(mark_c)
        qT = R3.bf16(8 * 512).rearrange("p (c n) -> p c n", c=8)
        B_qT = [Buf() for _ in range(8)]
        ocT = R3.bf16(8 * 512).rearrange("p (c n) -> p c n", c=8)
        B_oc = [Buf() for _ in range(8)]
        Pc2 = [R3.bf16(1024) for _ in range(2)]
        B_Pc2 = [Buf() for _ in range(2)]
        rsc2 = [R3.f32(512) for _ in range(2)]
        B_rsc2 = [Buf() for _ in range(2)]
        pc_rot = 0
        for s in range(4):
            for j8 in range(8):
                b = j8 % 2
                for c in range(8):
                    MM(ps[b][:, :], wQ[:, c, j8 * 128:(j8 + 1) * 128], hT[:, c, s * 512:(s + 1) * 512], c == 0, c == 7,
                       B_hT[s] + [B_wQ], [Bp[b]])
                if j8 % 2:
                    TS("dve", qT[:, j8, :], ps[b][:, :], 1.0 / 16.0, None, ALU.mult, None, [Bp[b]], [B_qT[j8]])
                else:
                    ACT(qT[:, j8, :], ps[b][:, :], AF.Copy, [Bp[b]], [B_qT[j8]], scale=1.0 / 16.0)
            pcs_h = {}

            def c_scores(hh):
                nonlocal pc_rot
                for mk in range(2):
                    b = 2 + mk
                    for e2 in range(2):
                        MM(ps[b][:, :], KcT[:, 2 * hh + e2, mk * 128:(mk + 1) * 128], qT[:, 2 * hh + e2, :], e2 == 0, e2 == 1,
                           [B_Kc, B_qT[2 * hh + e2]], [Bp[b]])
                k_ = pc_rot % 2
                pc_rot += 1
                ACT(Pc2[k_].rearrange("p (b n) -> p b n", b=2), ps_all[:, 1024:2048].rearrange("p (b n) -> p b n", b=2), AF.Exp,
                    [Bp[2], Bp[3]], [B_Pc2[k_]])
                pcs_h[hh] = k_

            def c_pv(hh):
                k_ = pcs_h[hh]
                Pm = [Pc2[k_][:, 0:512], Pc2[k_][:, 512:1024]]
                st_ = hh % 2
                ob = (4, 5) if st_ == 0 else (0, 1)
                sb_ = 6 if st_ == 0 else 7
                for e2 in range(2):
                    b = ob[e2]
                    for mk in range(2):
                        MM(ps[b][:, :], Vc[:, mk, (2 * hh + e2) * 128:(2 * hh + e2 + 1) * 128], Pm[mk], mk == 0, mk == 1,
                           [B_Vc, B_Pc2[k_]], [Bp[b]])
                for mk in range(2):
                    MM(ps[sb_][:, :], ones_bf, Pm[mk], mk == 0, mk == 1, [Bc, B_Pc2[k_]], [Bp[sb_]])
                RECIP(rsc2[st_], ps[sb_][:, :], [Bp[sb_]], [B_rsc2[st_]])
                for e2 in range(2):
                    TT("dve", ocT[:, 2 * hh + e2, :], ps[ob[e2]][:, :], rsc2[st_], ALU.mult, [Bp[ob[e2]], B_rsc2[st_]], [B_oc[2 * hh + e2]])

            c_scores(0)
            for hh in range(4):
                if hh + 1 < 4:
                    c_scores(hh + 1)
                c_pv(hh)
            for t4 in range(4):
                tt = 4 * s + t4
                for half in range(2):
                    b = half
                    for c in range(8):
                        MM(ps[b][:, :], ocT[:, c, t4 * 128:(t4 + 1) * 128], wOc[:, c, half * 512:(half + 1) * 512], c == 0, c == 7,
                           [B_oc[c], B_wOc], [Bp[b]])
                    TT("dve", hres[:, tt, half * 512:(half + 1) * 512], ps[b][:, :], hres[:, tt, half * 512:(half + 1) * 512], ALU.add,
                       [Bp[b], B_h[tt]], [B_h[tt]])
                brs_d[tt] = rms_cols(hres[:, tt, :], B_h[tt], 16 + tt)
        dump_h("dbg_h2")

        S.barrier()
        R3.reset()
        NU = 2
        ring = []
        for k in range(NU):
            unit = []
            for _e in range(2):
                wg_ = R3.bf16(8 * 256).rearrange("p (c n) -> p c n", c=8)
                wu_ = R3.bf16(8 * 256).rearrange("p (c n) -> p c n", c=8)
                wd_ = R3.bf16(2 * 1024).rearrange("p (f n) -> p f n", f=2)
                unit.append((wg_, wu_, wd_, Buf(), Buf(), Buf()))
            ring.append(unit)

        def load_unit(u):
            for e2 in range(2):
                e = 2 * u + e2
                wg_, wu_, wd_, b1, b2, b3 = ring[u % NU][e2]
                DMA("pool", wg_, w_gate[e].rearrange("(c p) n -> p c n", p=128), [], [b1])
                DMA("pool", wu_, w_up[e].rearrange("(c p) n -> p c n", p=128), [], [b2])
                DMA("pool", wd_, w_down[e].rearrange("(f p) n -> p f n", p=128), [], [b3])

        wR = R3.bf16(8 * 20).rearrange("p (c n) -> p c n", c=8)
        B_wR = Buf()
        DMA("pool", wR, wr.rearrange("(c p) n -> p c n", p=128), [], [B_wR])
        for u in range(NU):
            load_unit(u)
        selT = R3.bf16(2048)
        B_sel = Buf()
        DMA("pool", selT[0:16, :], sel_d, [], [B_sel])
        chi = R3.bf16(2048)
        clo = R3.bf16(2048)
        combT = R3.f32(2048)
        B_comb = [Buf() for _ in range(16)]
        rt_n = 16 * (20 + 16 + 4 * 8 + 16 + 9)
        rt = R3.f32(rt_n)
        B_rt = Buf()
        B_lg = [Buf() for _ in range(16)]
        _o = [0]

        def rtv(n_inner):
            o = _o[0]
            _o[0] += 16 * n_inner
            v = rt[:, o:o + 16 * n_inner]
            return v if n_inner == 1 else v.rearrange("p (t k) -> p t k", t=16)

        lg = rtv(20); comb = rtv(16); goh = rtv(4); gex = rtv(4); esel = rtv(4); oh1 = rtv(4); es2 = rtv(4); oh2 = rtv(4)
        inner = rtv(4); gsc = rtv(4); prod16 = rtv(16)
        gmax = rtv(1); gsum = rtv(1); gw = rtv(1); m1 = rtv(1); m2 = rtv(1); e21 = rtv(1); den = rtv(1); w1 = rtv(1); w2 = rtv(1)
        prod4 = prod16.rearrange("p t (g e) -> p t g e", g=4)
        comb4 = comb.rearrange("p t (g e) -> p t g e", g=4)

        def bcl(v, n):
            return bass.AP(v.tensor, v.offset, [list(d) for d in v.ap] + [[0, n]])

        def bcm(v, n):
            dd = [list(d) for d in v.ap]
            return bass.AP(v.tensor, v.offset, dd[:-1] + [[0, n]] + dd[-1:])

        actT = R3.bf16(2 * 2 * 512).rearrange("p (e f n) -> p e f n", e=2, f=2)
        B_act = [[Buf() for _ in range(2)] for _ in range(2)]
        sg = [R3.f32(512) for _ in range(2)]
        B_sg = [Buf() for _ in range(2)]
        tg = [R3.f32(512) for _ in range(2)]
        B_tg = [Buf() for _ in range(2)]
        bcs = [R3.f32(512) for _ in range(2)]
        B_bcs = [Buf() for _ in range(2)]
        xs_t = [tg[0].bitcast(BF16), tg[1].bitcast(BF16)]
        B_xs = [Buf() for _ in range(2)]
        B_h3T = [[Buf() for _ in range(4)] for _ in range(4)]
        for tt in range(16):
            norm_transpose(hres[:, tt, :], B_h[tt], rstd_all[:, 16 + tt:17 + tt], 24, hT[:, :, tt * 128:(tt + 1) * 128], B_h3T[tt // 4][tt % 4],
                           xs_t[tt % 2], B_xs[tt % 2], tt % 2, brs_d[tt])
            for c in range(8):
                MM(ps[2 + tt % 2][:, 0:20], hT[:, c, tt * 128:(tt + 1) * 128], wR[:, c, :], c == 0, c == 7,
                   [B_h3T[tt // 4][tt % 4], B_wR], [Bp[2 + tt % 2]])
            TT("dve", lg[:, tt, :], ps[2 + tt % 2][:, 0:20], rbias_bc, ALU.add, [Bp[2 + tt % 2], Bc], [B_lg[tt]])

        R_ = [B_rt]
        lgg = lg[:, :, 0:4]
        le4 = lg[:, :, 4:20].rearrange("p t (g e) -> p t g e", g=4)
        S.add("dve", lambda e: e.tensor_reduce(gmax, lgg, axis=AX.X, op=ALU.max), B_lg, R_)
        TT("dve", goh, lgg, bcl(gmax, 4), ALU.is_equal, B_lg + R_, R_)
        TT("dve", gex, lgg, bcl(gmax, 4), ALU.subtract, B_lg + R_, R_)
        ACT(gex, gex, AF.Exp, R_, R_)
        S.add("dve", lambda e: e.tensor_reduce(gsum, gex, axis=AX.X, op=ALU.add), R_, R_)
        RECIP(gw, gsum, R_, R_)
        TT("dve", prod4, le4, bcl(goh, 4), ALU.mult, B_lg + R_, R_)
        S.add("dve", lambda e: e.tensor_reduce(esel, prod4.rearrange("p t g e -> p t e g"), axis=AX.X, op=ALU.add), R_, R_)
        S.add("dve", lambda e: e.tensor_reduce(m1, esel, axis=AX.X, op=ALU.max), R_, R_)
        TT("dve", oh1, esel, bcl(m1, 4), ALU.is_equal, R_, R_)
        STT(es2, oh1, -1e30, esel, ALU.mult, ALU.add, R_, R_)
        S.add("dve", lambda e: e.tensor_reduce(m2, es2, axis=AX.X, op=ALU.max), R_, R_)
        TT("dve", oh2, es2, bcl(m2, 4), ALU.is_equal, R_, R_)
        TT("dve", e21, m2, m1, ALU.subtract, R_, R_)
        ACT(e21, e21, AF.Exp, R_, R_)
        TS("dve", den, e21, 1.0, None, ALU.add, None, R_, R_)
        RECIP(w1, den, R_, R_)
        TT("dve", w2, e21, w1, ALU.mult, R_, R_)
        TT("dve", inner, oh1, bcl(w1, 4), ALU.mult, R_, R_)
        TT("dve", oh2, oh2, bcl(w2, 4), ALU.mult, R_, R_)
        TT("dve", inner, inner, oh2, ALU.add, R_, R_)
        TT("dve", gsc, goh, bcl(gw, 4), ALU.mult, R_, R_)
        TT("dve", comb4, bcl(gsc, 4), bcm(inner, 4), ALU.mult, R_, R_)
        for q4 in range(4):
            pb_ = 2 + q4 % 2
            for t4 in range(4):
                tt = 4 * q4 + t4
                S.add("pe", lambda e, tt=tt, t4=t4, pb_=pb_: e.transpose(ps[pb_][0:16, t4 * 128:(t4 + 1) * 128], comb[:, tt, :], identf),
                      [B_rt, Bc], [Bp[pb_]])
            CP("dve", combT[0:16, q4 * 512:(q4 + 1) * 512], ps[pb_][0:16, :], [Bp[pb_]], B_comb[4 * q4:4 * q4 + 4])
            CP("dve", chi[0:16, q4 * 512:(q4 + 1) * 512], combT[0:16, q4 * 512:(q4 + 1) * 512], B_comb[4 * q4:4 * q4 + 4], B_comb[4 * q4:4 * q4 + 4])
            TT("dve", clo[0:16, q4 * 512:(q4 + 1) * 512], combT[0:16, q4 * 512:(q4 + 1) * 512], chi[0:16, q4 * 512:(q4 + 1) * 512], ALU.subtract,
               B_comb[4 * q4:4 * q4 + 4], B_comb[4 * q4:4 * q4 + 4])

        d_rot = [0]

        def gu_mm(u, s, e2, f):
            wg_, wu_, wd_, b1, b2, b3 = ring[u % NU][e2]
            bg = 2 * f
            bu = 2 * f + 1
            for c in range(8):
                MM(ps[bg][:, :], wg_[:, c, f * 128:(f + 1) * 128], hT[:, c, s * 512:(s + 1) * 512], c == 0, c == 7,
                   B_h3T[s] + [b1], [Bp[bg]])
            for c in range(8):
                MM(ps[bu][:, :], wu_[:, c, f * 128:(f + 1) * 128], hT[:, c, s * 512:(s + 1) * 512], c == 0, c == 7,
                   B_h3T[s] + [b2], [Bp[bu]])

        def gu_post(u, s, e2, f):
            e = 2 * u + e2
            bi = e2
            bg = 2 * f
            bu = 2 * f + 1
            if f == 0:
                MM(ps[6][:, :], selT[0:16, e * 128:(e + 1) * 128], chi[0:16, s * 512:(s + 1) * 512], True, False,
                   [B_sel] + B_comb[4 * s:4 * s + 4], [Bp[6]])
                MM(ps[6][:, :], selT[0:16, e * 128:(e + 1) * 128], clo[0:16, s * 512:(s + 1) * 512], False, True,
                   [B_sel] + B_comb[4 * s:4 * s + 4], [Bp[6]])
                CP("act", bcs[bi], ps[6][:, :], [Bp[6]], [B_bcs[bi]])
            ACT(sg[f], ps[bg][:, :], AF.Silu, [Bp[bg]], [B_sg[f]])
            TT("dve", tg[f], ps[bu][:, :], sg[f], ALU.mult, [Bp[bu], B_sg[f]], [B_tg[f]])
            TT("pool", actT[:, e2, f, :], tg[f], bcs[bi], ALU.mult, [B_tg[f], B_bcs[bi]], [B_act[e2][f]])

        def down(u, s):
            unit = ring[u % NU]
            for t4 in range(4):
                tt = 4 * s + t4
                for half in range(2):
                    b = 4 + d_rot[0] % 2
                    d_rot[0] += 1
                    k = 0
                    for e2 in range(2):
                        wd_, b3 = unit[e2][2], unit[e2][5]
                        for f in range(2):
                            MM(ps[b][:, :], actT[:, e2, f, t4 * 128:(t4 + 1) * 128], wd_[:, f, half * 512:(half + 1) * 512],
                               k == 0, k == 3, [B_act[e2][f], b3], [Bp[b]])
                            k += 1
                    TT("dve", hres[:, tt, half * 512:(half + 1) * 512], ps[b][:, :], hres[:, tt, half * 512:(half + 1) * 512], ALU.add,
                       [Bp[b], B_h[tt]], [B_h[tt]])
                if u == 7:
                    brs_f[tt] = rms_cols(hres[:, tt, :], B_h[tt], 34 + tt)

        steps = [(u, s) for u in range(8) for s in range(4)]
        gu_mm(0, 0, 0, 0)
        for k_, (u, s) in enumerate(steps):
            gu_post(u, s, 0, 0)
            gu_mm(u, s, 0, 1)
            gu_post(u, s, 0, 1)
            gu_mm(u, s, 1, 0)
            gu_post(u, s, 1, 0)
            gu_mm(u, s, 1, 1)
            gu_post(u, s, 1, 1)
            if k_ + 1 < len(steps):
                un, sn = steps[k_ + 1]
                gu_mm(un, sn, 0, 0)
            down(u, s)
            if s == 3 and u + NU < 8:
                load_unit(u + NU)
        dump_h("dbg_h3")

        S.barrier()
        R3.reset()
        yo = [R3.f32(1024) for _ in range(2)]
        B_yo = [Buf() for _ in range(2)]
        for tt in range(16):
            STT(yo[tt % 2], hres[:, tt, :], rstd_all[:, 34 + tt:35 + tt], fnorm_bc, ALU.mult, ALU.mult, [B_h[tt], brs_f[tt], Bc], [B_yo[tt % 2]])
            DMA("sp", y[tt * 128:(tt + 1) * 128, :], yo[tt % 2], [B_yo[tt % 2]], [])

        S.emit(nc, st)
    return nc


def _rel_bucket(rel):
    nb = 16
    max_exact = 8
    ret = (rel > 0).astype(np.int32) * nb
    n = np.abs(rel)
    nf = np.maximum(n, 1).astype(np.float32)
    large = max_exact + (np.log(nf / np.float32(max_exact)) / np.float32(math.log(128 / max_exact))
                         * np.float32(nb - max_exact)).astype(np.int32)
    large = np.minimum(large, nb - 1)
    return ret + np.where(n < max_exact, n, large)


_NC_CACHE = {}


def kernel(**inp):
    debug = int(inp.pop("_debug", 0)) if "_debug" in inp else 0
    f = lambda k: np.ascontiguousarray(np.asarray(inp[k], dtype=np.float32))
    x = f("x")
    mem = f("mem")
    i = np.arange(384)
    rel = 127 - i
    bk = _rel_bucket(rel.astype(np.int32))
    oh = np.zeros((32, 384), np.float32)
    oh[bk, i] = 1.0
    oh[15, :] -= 1.0
    kk = np.arange(128)[:, None]
    qq = np.arange(128)[None, :]
    maskT = np.where((kk < 64) | (qq >= 64), 0.0, NEG).astype(np.float32)
    ident = np.eye(128, dtype=np.float32)
    sel = np.zeros((16, 16, 128), np.float32)
    for e in range(16):
        sel[e, e, :] = 1.0
    sel = sel.reshape(16, 2048)
    gains = np.zeros((128, 40), np.float32)
    gains[:, 0:8] = f("attn_norm")[0].reshape(8, 128).T
    gains[:, 8:16] = f("cross_norm")[0].reshape(8, 128).T
    gains[:, 16:24] = f("mem_norm")[0].reshape(8, 128).T
    gains[:, 24:32] = f("ffn_norm")[0].reshape(8, 128).T
    gains[:, 32:36] = f("pool_scale")[0].reshape(4, 128).T
    gains[:, 36] = f("diff_subln")[0]
    wr = np.concatenate([f("router_group")[0], f("router_expert")[0].transpose(1, 0, 2).reshape(1024, 16)], axis=1)
    rbias = np.concatenate([f("router_group_bias")[0], f("router_expert_bias")[0].reshape(16)])[None, :]
    lamv = np.concatenate([f("lambda_q1")[0], f("lambda_k1")[0], f("lambda_q2")[0], f("lambda_k2")[0]])[None, :]
    shared = {
        "w_in": f("w_in")[0], "w_out": f("w_out")[0], "wq": f("wq_cross")[0], "wkv": f("wkv_cross")[0], "wo": f("wo_cross")[0],
        "w_gate": f("w_gate")[0], "w_up": f("w_up")[0], "w_down": f("w_down")[0], "pool_w": f("pool_w")[0],
        "wr": np.ascontiguousarray(wr), "rbias": np.ascontiguousarray(rbias), "rel_bias": f("rel_bias"),
        "lamv": np.ascontiguousarray(lamv), "gains": gains, "fnorm": f("final_norm")[None, :],
        "ident": ident, "oh": oh, "maskT": maskT, "sel": sel,
    }
    in_maps = []
    for c in range(8):
        b, j = c // 4, c % 4
        pad = 3 - j
        xs = np.zeros((8192, 1024), np.float32)
        xs[pad * 512:] = x[b, :(16 - pad) * 512]
        kvb = np.zeros((128, 16), np.float32)
        kvb[:, :pad] = NEG
        pinv = np.zeros((128, 4, 16), np.float32)
        for g in range(4):
            w = 2 ** (g + 1)
            if j == 0:
                pinv[:, g, :] = 1.0 / np.minimum(np.arange(1, 17), w)
            else:
                pinv[:, g, :] = 1.0 / w
        m = dict(shared)
        m.update({"x": xs, "mem": mem[b], "kvb": kvb, "pinv": pinv.reshape(128, 64)})
        in_maps.append(m)
    key = debug
    if key not in _NC_CACHE:
        _NC_CACHE[key] = build_nc(debug)
    nc = _NC_CACHE[key]
    res = run_bass_kernel_spmd(nc, in_maps, core_ids=list(range(8)))
    out = np.zeros((2, 8192, 1024), np.float32)
    extra = {}
    for c in range(8):
        b, j = c // 4, c % 4
        r = res.results[c]
        for s in range(4):
            t = 4 * s + j
            out[b, t * 512:(t + 1) * 512] = r["y"][s * 512:(s + 1) * 512]
        if debug:
            extra[c] = {k: v for k, v in r.items() if k.startswith("dbg")}
    if debug:
        return out, extra
    return out
```
